# Optimizing a Trainium2 kernel written in Bass

```python
import jax, jax.numpy as jnp
from jax import lax
import numpy as np

D_MODEL = 1024
BATCH = 16
SEQ = 4096
DEPTH = 2

PLE_DIM = 256
HEAD_DIM = 128
GDN_HEADS = D_MODEL // 256
MOBA_HEADS = D_MODEL // 256
FOX_HEADS = D_MODEL // 128
GDN_W = GDN_HEADS * HEAD_DIM
MOBA_W = MOBA_HEADS * HEAD_DIM
FOX_W = FOX_HEADS * HEAD_DIM
MIX_W_AB = GDN_W + MOBA_W
MIX_W_C = FOX_W
CONV_K = 4
GDN_CHUNK = 64
MOBA_BLOCK = 256
MOBA_TOPK = 3
MOBA_QCHUNK = 16
FOX_QBLOCK = 128
IN_AB = 3 * GDN_W + 2 * GDN_HEADS + 3 * MOBA_W + MIX_W_AB
IN_C = 3 * FOX_W + FOX_HEADS + MIX_W_C
N_EVEN = (DEPTH + 1) // 2
N_ODD = DEPTH // 2
RMS_EPS = 1e-6
NEG = -1e30
F32 = jnp.float32

kernel_name = 'hybrid_gdn_moba_fox'


def rmsnorm(x, g):
    xf = x.astype(F32)
    y = xf * lax.rsqrt(jnp.mean(xf * xf, axis=-1, keepdims=True) + RMS_EPS)
    return (y * g.astype(F32)).astype(x.dtype)


def l2norm(t):
    return t * lax.rsqrt(jnp.sum(t * t, axis=-1, keepdims=True) + RMS_EPS)


def to_heads(t, n):
    b, s, _ = t.shape
    return t.reshape(b, s, n, -1).transpose(0, 2, 1, 3)


def from_heads(t):
    b, h, s, d = t.shape
    return t.transpose(0, 2, 1, 3).reshape(b, s, h * d)


def causal_depthwise_conv(u, w):
    c = u.shape[-1]
    return lax.conv_general_dilated(
        u, w[:, None, :].astype(u.dtype), window_strides=(1,),
        padding=[(w.shape[0] - 1, 0)], dimension_numbers=('NWC', 'WIO', 'NWC'),
        feature_group_count=c)


def alibi_slopes(n):
    return 2.0 ** (-8.0 * (jnp.arange(n, dtype=F32) + 1.0) / n)


def gated_delta_rule(q, k, v, beta, g):
    b, h, s, dk = q.shape
    dv = v.shape[-1]
    c = GDN_CHUNK
    n = s // c
    q, k, v = (t.reshape(b, h, n, c, -1) for t in (q, k, v))
    beta = beta.reshape(b, h, n, c)
    G = jnp.cumsum(g.reshape(b, h, n, c), axis=-1)
    incl = jnp.tril(jnp.ones((c, c), bool))
    strict = jnp.tril(jnp.ones((c, c), bool), -1)
    diff = G[..., :, None] - G[..., None, :]
    decay = jnp.where(incl, jnp.exp(jnp.where(incl, diff, 0.0)), 0.0)
    k_beta = k * beta[..., None]
    a_mat = jnp.where(strict, jnp.einsum('bhnid,bhnjd->bhnij', k_beta, k) * decay, 0.0)
    eye = jnp.eye(c, dtype=q.dtype)
    rhs = jnp.concatenate([v * beta[..., None], k_beta * jnp.exp(G)[..., None]], axis=-1)
    sol = lax.linalg.triangular_solve(eye + a_mat, rhs, left_side=True, lower=True,
                                      unit_diagonal=True)
    u, w = sol[..., :dv], sol[..., dv:]
    qk = jnp.where(incl, jnp.einsum('bhnid,bhnjd->bhnij', q, k) * decay, 0.0)
    q_dec = q * jnp.exp(G)[..., None]
    k_dec = k * jnp.exp(G[..., -1:] - G)[..., None]
    g_last = jnp.exp(G[..., -1])

    def step(state, xs):
        q_i, k_i, u_i, w_i, qk_i, gl_i = xs
        v_new = u_i - jnp.einsum('bhcd,bhde->bhce', w_i, state)
        o = jnp.einsum('bhcd,bhde->bhce', q_i, state) + jnp.einsum('bhij,bhje->bhie', qk_i, v_new)
        state = state * gl_i[..., None, None] + jnp.einsum('bhcd,bhce->bhde', k_i, v_new)
        return state, o

    xs = tuple(jnp.moveaxis(t, 2, 0) for t in (q_dec, k_dec, u, w, qk, g_last))
    state0 = jnp.zeros((b, h, dk, dv), q.dtype)
    _, o = lax.scan(step, state0, xs)
    return jnp.moveaxis(o, 0, 2).reshape(b, h, s, dv)


def moba_attention(q, k, v):
    b, h, s, d = q.shape
    L = MOBA_BLOCK
    QC = MOBA_QCHUNK
    nb = -(-s // L)
    s_pad = nb * L
    pad = [(0, 0), (0, 0), (0, s_pad - s), (0, 0)]
    q, k, v = (jnp.pad(t, pad) for t in (q, k, v))
    scale = d ** -0.5
    slopes = alibi_slopes(h)
    k_blk = k.reshape(b, h, nb, L, d)
    v_blk = v.reshape(b, h, nb, L, d)
    k_mean = jnp.mean(k_blk.astype(F32), axis=3)
    q_blk_id = jnp.arange(s_pad) // L
    gate = jnp.einsum('bhsd,bhnd->bhsn', q.astype(F32), k_mean)
    past = jnp.arange(nb)[None, :] < q_blk_id[:, None]
    gate = jnp.where(past, gate, NEG)
    topk = min(MOBA_TOPK, nb)
    _, idx = lax.top_k(gate, topk)
    nq = s_pad // QC
    q_c = q.reshape(b, h, nq, QC, d).transpose(2, 0, 1, 3, 4)
    idx_c = idx.reshape(b, h, nq, QC, topk).transpose(2, 0, 1, 3, 4)
    bi = jnp.arange(b)[:, None, None, None]
    hi = jnp.arange(h)[None, :, None, None]
    offs = jnp.arange(L)

    def chunk(args):
        ci, qq, ii = args
        t = ci * QC + jnp.arange(QC)
        own = (ci * QC) // L
        kg = k_blk[bi, hi, ii]
        vg = v_blk[bi, hi, ii]
        kpos = ii[..., None] * L + offs
        dist_p = (t[:, None, None] - kpos).astype(F32)
        lp = (jnp.einsum('bhqd,bhqkld->bhqkl', qq, kg).astype(F32) * scale
              - slopes[:, None, None, None] * dist_p)
        lp = jnp.where((ii < own)[..., None], lp, NEG).reshape(b, h, QC, topk * L)
        k_own = lax.dynamic_index_in_dim(k_blk, own, axis=2, keepdims=False)
        v_own = lax.dynamic_index_in_dim(v_blk, own, axis=2, keepdims=False)
        dist_o = (t[:, None] - (own * L + offs)[None, :]).astype(F32)
        lo = (jnp.einsum('bhqd,bhld->bhql', qq, k_own).astype(F32) * scale
              - slopes[:, None, None] * dist_o)
        lo = jnp.where(dist_o >= 0, lo, NEG)
        wts = jax.nn.softmax(jnp.concatenate([lp, lo], axis=-1), axis=-1).astype(qq.dtype)
        wp = wts[..., :topk * L].reshape(b, h, QC, topk, L)
        wo = wts[..., topk * L:]
        return (jnp.einsum('bhqkl,bhqkld->bhqd', wp, vg)
                + jnp.einsum('bhql,bhld->bhqd', wo, v_own))

    out = lax.map(chunk, (jnp.arange(nq), q_c, idx_c))
    return out.transpose(1, 2, 0, 3, 4).reshape(b, h, s_pad, d)[:, :, :s]


def forgetting_attention(q, k, v, log_f):
    b, h, s, d = q.shape
    scale = d ** -0.5
    c = jnp.cumsum(log_f, axis=-1)
    outs = []
    for i in range(s // FOX_QBLOCK):
        lo_, hi_ = i * FOX_QBLOCK, (i + 1) * FOX_QBLOCK
        logits = (jnp.einsum('bhqd,bhkd->bhqk', q[:, :, lo_:hi_], k[:, :, :hi_]).astype(F32) * scale
                  + c[:, :, lo_:hi_, None] - c[:, :, None, :hi_])
        causal = jnp.arange(lo_, hi_)[:, None] >= jnp.arange(hi_)[None, :]
        wts = jax.nn.softmax(jnp.where(causal, logits, NEG), axis=-1).astype(v.dtype)
        outs.append(jnp.einsum('bhqk,bhkd->bhqd', wts, v[:, :, :hi_]))
    return jnp.concatenate(outs, axis=2)


def ab_mixer(hn, w_in, conv_w, a_log, dt_bias, gdn_norm_g, w_out):
    proj = hn @ w_in
    cuts = [3 * GDN_W, 3 * GDN_W + GDN_HEADS, 3 * GDN_W + 2 * GDN_HEADS,
            3 * GDN_W + 2 * GDN_HEADS + 3 * MOBA_W]
    qkv_a, a_raw, b_raw, qkv_b, z = jnp.split(proj, cuts, axis=-1)
    qkv_a = jax.nn.silu(causal_depthwise_conv(qkv_a, conv_w))
    qa, ka, va = jnp.split(qkv_a, 3, axis=-1)
    qa = l2norm(to_heads(qa, GDN_HEADS).astype(F32)) * (HEAD_DIM ** -0.5)
    ka = l2norm(to_heads(ka, GDN_HEADS).astype(F32))
    va = to_heads(va, GDN_HEADS).astype(F32)
    beta = jax.nn.sigmoid(b_raw.astype(F32)).transpose(0, 2, 1)
    g = (-jnp.exp(a_log.astype(F32))
         * jax.nn.softplus(a_raw.astype(F32) + dt_bias.astype(F32))).transpose(0, 2, 1)
    oa = rmsnorm(gated_delta_rule(qa, ka, va, beta, g), gdn_norm_g).astype(hn.dtype)
    qb, kb, vb = (to_heads(t, MOBA_HEADS) for t in jnp.split(qkv_b, 3, axis=-1))
    ob = moba_attention(qb, kb, vb)
    y = jnp.concatenate([from_heads(oa), from_heads(ob)], axis=-1) * jax.nn.silu(z)
    return y @ w_out


def fox_mixer(hn, w_in, forget_b, w_out):
    proj = hn @ w_in
    qkv, f_raw, z = jnp.split(proj, [3 * FOX_W, 3 * FOX_W + FOX_HEADS], axis=-1)
    q, k, v = (to_heads(t, FOX_HEADS) for t in jnp.split(qkv, 3, axis=-1))
    log_f = jax.nn.log_sigmoid(f_raw.astype(F32) + forget_b.astype(F32)).transpose(0, 2, 1)
    o = forgetting_attention(q, k, v, log_f)
    return (from_heads(o) * jax.nn.silu(z)) @ w_out


def setup_inputs(seed: int = 0) -> dict:
    key = jax.random.key(seed)
    ks = jax.random.split(key, 18)
    nrm = jax.random.normal
    dt = jnp.exp(jax.random.uniform(ks[6], (N_EVEN, GDN_HEADS), F32, np.log(1e-3), np.log(1e-1)))
    return {
        'x': nrm(ks[0], (BATCH, SEQ, D_MODEL), F32),
        'p': nrm(ks[1], (DEPTH, BATCH, SEQ, PLE_DIM), F32),
        'norm_g': 1.0 + 0.02 * nrm(ks[2], (DEPTH, D_MODEL), F32),
        'w_in_ab': nrm(ks[3], (N_EVEN, D_MODEL, IN_AB), F32) * D_MODEL ** -0.5,
        'conv_w': nrm(ks[4], (N_EVEN, CONV_K, 3 * GDN_W), F32) * CONV_K ** -0.5,
        'a_log': jnp.log(jax.random.uniform(ks[5], (N_EVEN, GDN_HEADS), F32, 1.0, 16.0)),
        'dt_bias': dt + jnp.log(-jnp.expm1(-dt)),
        'gdn_norm_g': 1.0 + 0.02 * nrm(ks[7], (N_EVEN, HEAD_DIM), F32),
        'w_out_ab': nrm(ks[8], (N_EVEN, MIX_W_AB, D_MODEL), F32) * MIX_W_AB ** -0.5,
        'w_in_c': nrm(ks[9], (N_ODD, D_MODEL, IN_C), F32) * D_MODEL ** -0.5,
        'forget_b': jax.random.uniform(ks[10], (N_ODD, FOX_HEADS), F32, 1.0, 5.0),
        'w_out_c': nrm(ks[11], (N_ODD, MIX_W_C, D_MODEL), F32) * MIX_W_C ** -0.5,
        'ple_norm_g': 1.0 + 0.02 * nrm(ks[12], (DEPTH, D_MODEL), F32),
        'w_ple_gate': nrm(ks[13], (DEPTH, D_MODEL, D_MODEL), F32) * D_MODEL ** -0.5,
        'w_ple_proj': nrm(ks[14], (DEPTH, PLE_DIM, D_MODEL), F32) * PLE_DIM ** -0.5,
        'final_g': 1.0 + 0.02 * nrm(ks[15], (D_MODEL,), F32),
    }


def reference(x, p, norm_g, w_in_ab, conv_w, a_log, dt_bias, gdn_norm_g, w_out_ab,
              w_in_c, forget_b, w_out_c, ple_norm_g, w_ple_gate, w_ple_proj, final_g):
    for i in range(DEPTH):
        j = i // 2
        hn = rmsnorm(x, norm_g[i])
        if i % 2 == 0:
            x = x + ab_mixer(hn, w_in_ab[j], conv_w[j], a_log[j], dt_bias[j],
                             gdn_norm_g[j], w_out_ab[j])
        else:
            x = x + fox_mixer(hn, w_in_c[j], forget_b[j], w_out_c[j])
        gate = jax.nn.sigmoid(rmsnorm(x, ple_norm_g[i]) @ w_ple_gate[i])
        x = x + gate * (p[i] @ w_ple_proj[i])
    return rmsnorm(x, final_g)
```

```python
import contextlib
import numpy as np
import concourse.bass as bass
import concourse.mybir as mybir
from concourse.bass_utils import run_bass_kernel_spmd

F32 = mybir.dt.float32
BF16 = mybir.dt.bfloat16
AF = mybir.ActivationFunctionType
ALU = mybir.AluOpType
AX = mybir.AxisListType

D = 1024
HD = 128
EPS = 1e-6
BIG = 1.0e30
QSCALE = HD ** -0.5


class Buf:
    __slots__ = ("w", "r")

    def __init__(self):
        self.w = None
        self.r = []


class KS:
    ENGS = ("pe", "dve", "act", "pool", "sp")

    def __init__(self, nc, n_dma_chan=8):
        self.nc = nc
        self.streams = {e: [] for e in self.ENGS}
        self.cnt = {e: 0 for e in self.ENGS}
        self.seen = {e: {} for e in self.ENGS}
        self.nchan = n_dma_chan
        self.chan_cnt = {}
        self.chan_rr = {e: 0 for e in self.ENGS}
        self.sems = {}

    def _deps(self, eng, reads, writes):
        need = {}

        def add(tok):
            if tok is None:
                return
            k, v = tok
            if k == eng and eng == "pe":
                return
            if need.get(k, 0) < v:
                need[k] = v
        for b in reads:
            add(b.w)
        for b in writes:
            add(b.w)
            for t in b.r:
                add(t)
        out = []
        seen = self.seen[eng]
        for k, v in need.items():
            if seen.get(k, 0) >= v:
                continue
            seen[k] = v
            out.append((k, v))
        return out

    def _commit(self, tok, reads, writes):
        for b in reads:
            if len(b.r) > 24:
                m = {}
                for k, v in b.r:
                    if m.get(k, 0) < v:
                        m[k] = v
                b.r = list(m.items())
            b.r.append(tok)
        for b in writes:
            b.w = tok
            b.r = []

    def op(self, eng, fn, reads=(), writes=()):
        waits = self._deps(eng, reads, writes)
        self.cnt[eng] += 1
        tok = (eng, self.cnt[eng])
        self.streams[eng].append((waits, fn, (eng, 1)))
        self._commit(tok, reads, writes)
        return tok

    def dma(self, eng, fn, reads=(), writes=()):
        c = self.chan_rr[eng]
        self.chan_rr[eng] = (c + 1) % self.nchan
        key = ("dma", eng, c)
        prev = self.chan_cnt.get(key, 0)
        waits = self._deps(eng, reads, writes)
        if prev and self.seen[eng].get(key, 0) < prev:
            self.seen[eng][key] = prev
            waits.append((key, prev))
        self.chan_cnt[key] = prev + 16
        tok = (key, prev + 16)
        self.streams[eng].append((waits, fn, (key, 16)))
        self._commit(tok, reads, writes)
        return tok

    def fence(self):
        cur = dict(self.cnt)
        cur.update(self.chan_cnt)
        for e in self.ENGS:
            waits = []
            for k, v in cur.items():
                if v <= 0 or (k == e):
                    continue
                if self.seen[e].get(k, 0) < v:
                    self.seen[e][k] = v
                    waits.append((k, v))
            if waits:
                self.streams[e].append((waits, None, None))

    def emit(self):
        nc = self.nc
        keys = list(self.ENGS) + sorted(self.chan_cnt.keys(), key=str)
        with contextlib.ExitStack() as st:
            for k in keys:
                nm = "s_" + ("_".join(map(str, k)) if isinstance(k, tuple) else k)
                self.sems[k] = st.enter_context(nc.semaphore(nm))
            block = st.enter_context(nc.Block())
            finals = {k: v for k, v in self.chan_cnt.items()}
            finals.update({e: v for e, v in self.cnt.items() if v > 0})

            def run(eng, h):
                for waits, fn, inc in self.streams[eng]:
                    for k, v in waits:
                        h.wait_ge(self.sems[k], v)
                    if fn is not None:
                        fn(h).then_inc(self.sems[inc[0]], inc[1])
                if eng == "sp":
                    for k, v in finals.items():
                        if k != "sp":
                            h.wait_ge(self.sems[k], v)

            @block.tensor
            def _(h):
                run("pe", h)

            @block.vector
            def _(h):
                run("dve", h)

            @block.scalar
            def _(h):
                run("act", h)

            @block.gpsimd
            def _(h):
                run("pool", h)

            @block.sync
            def _(h):
                run("sp", h)


A_Q, A_K, A_V = 0, 512, 1024
A_A, A_B = 1536, 1540
B_Q, B_K, B_V = 1544, 2056, 2568
A_Z = 3080
C_Q, C_K, C_V, C_F, C_Z = 0, 1024, 2048, 3072, 3080
IN_W = 4104


def build(S, NSEQ, do_gdn=True, do_moba=True, do_fox=True):
    NT = S // 128
    NBQ = S // 512
    NB = S // 256
    nc = bass.Bass("TRN2", target_bir_lowering=False)
    dt = nc.dram_tensor
    x_in = dt("x", [NSEQ, S, D], F32, kind="ExternalInput").ap()
    pT_in = dt("pT", [2, NSEQ, 256, S], F32, kind="ExternalInput").ap()
    w_ab = dt("w_in_ab", [D, IN_W], F32, kind="ExternalInput").ap()
    w_c = dt("w_in_c", [D, IN_W], F32, kind="ExternalInput").ap()
    w_out = [dt("w_out_ab", [D, D], F32, kind="ExternalInput").ap(),
             dt("w_out_c", [D, D], F32, kind="ExternalInput").ap()]
    w_gate = dt("w_ple_gate", [2, D, D], F32, kind="ExternalInput").ap()
    w_pp = dt("w_ple_proj", [2, 256, D], F32, kind="ExternalInput").ap()
    gvec = dt("gvec", [5, 128, D], F32, kind="ExternalInput").ap()
    convw = dt("convw", [128, 12, 4], F32, kind="ExternalInput").ap()
    smallv = dt("smallv", [128, 144], F32, kind="ExternalInput").ap()
    wfc = dt("wfc", [8, 128, 8], F32, kind="ExternalInput").ap()
    out = dt("out", [NSEQ, S, D], F32, kind="ExternalOutput").ap()
    xres = dt("xres", [NSEQ, S, D], F32, kind="Internal").ap()
    yT = dt("yT", [NSEQ, D, S], BF16, kind="Internal").ap()

    ks = KS(nc)
    slopes = [2.0 ** (-8.0 * (h + 1) / 4) for h in range(4)]

    with contextlib.ExitStack() as top:
        uid = [0]

        def SB(st, name, shape, dtype):
            uid[0] += 1
            return st.enter_context(nc.sbuf_tensor("%s_%d" % (name, uid[0]), shape, dtype))

        def PS(st, name, shape, dtype):
            uid[0] += 1
            return st.enter_context(nc.psum_tensor("%s_%d" % (name, uid[0]), shape, dtype))

        hnT = SB(top, "hnT", [128, 8, S], BF16)
        B_hnT = [Buf() for _ in range(NT)]
        ident_f = SB(top, "ident_f", [128, 128], F32)
        ident_b = SB(top, "ident_b", [128, 128], BF16)
        ones_f = SB(top, "ones_f", [128, 128], F32)
        ones_b = SB(top, "ones_b", [128, 128], BF16)
        maskT_b = SB(top, "maskT_b", [128, 128], BF16)
        maskA = SB(top, "maskA", [128, 128], F32)
        maskQ = SB(top, "maskQ", [128, 128], F32)
        U_f = SB(top, "U_f", [128, 128], F32)
        e0col = SB(top, "e0col", [128, 2], F32)
        iq = SB(top, "iq", [128, 512], F32)
        ikcol = SB(top, "ikcol", [128, 1], F32)
        tmpf = SB(top, "tmpf", [128, 128], F32)
        smalls = SB(top, "smalls", [128, 144], F32)
        zero_c = SB(top, "zero_c", [128, 1], F32)
        B_const = Buf()

        PSM = {"A": [], "BA": [], "T": [], "BT": []}
        rr = {"A": 0, "T": 0}

        def set_psum(st, nA, nT):
            PSM["A"] = [PS(st, "psA", [128, 512], F32) for _ in range(nA)]
            PSM["BA"] = [Buf() for _ in range(nA)]
            PSM["T"] = [PS(st, "psT", [128, 1024], BF16) for _ in range(nT)]
            PSM["BT"] = [Buf() for _ in range(nT)]
            rr["A"] = 0
            rr["T"] = 0

        def nextA():
            i = rr["A"]
            rr["A"] = (i + 1) % len(PSM["A"])
            return PSM["A"][i], PSM["BA"][i]

        def nextT():
            i = rr["T"]
            rr["T"] = (i + 1) % len(PSM["T"])
            return PSM["T"][i], PSM["BT"][i]

        P = lambda fn, **kw: ks.op("pool", fn, **kw)
        V = lambda fn, **kw: ks.op("dve", fn, **kw)
        A = lambda fn, **kw: ks.op("act", fn, **kw)
        T = lambda fn, **kw: ks.op("pe", fn, **kw)

        C = [B_const]
        P(lambda e: e.memset(ident_f[:], 0.0), writes=C)
        P(lambda e: e.affine_select(out=ident_f[:], in_=ident_f[:], pattern=[[-1, 128]], compare_op=ALU.not_equal,
                                    fill=1.0, base=0, channel_multiplier=1), reads=C, writes=C)
        P(lambda e: e.tensor_copy(out=ident_b[:], in_=ident_f[:]), reads=C, writes=C)
        P(lambda e: e.memset(ones_f[:], 1.0), writes=C)
        P(lambda e: e.memset(ones_b[:], 1.0), writes=C)
        P(lambda e: e.memset(zero_c[:], 0.0), writes=C)
        P(lambda e: e.memset(maskQ[:], 0.0), writes=C)
        P(lambda e: e.affine_select(out=maskQ[:], in_=maskQ[:], pattern=[[1, 128]], compare_op=ALU.is_ge,
                                    fill=BIG, base=0, channel_multiplier=-1), reads=C, writes=C)
        P(lambda e: e.tensor_scalar(out=maskT_b[:], in0=maskQ[:], scalar1=-1.0, scalar2=None, op0=ALU.mult),
          reads=C, writes=C)
        P(lambda e: e.memset(maskA[:], 0.0), writes=C)
        P(lambda e: e.affine_select(out=maskA[:], in_=maskA[:], pattern=[[-1, 128]], compare_op=ALU.is_gt,
                                    fill=BIG, base=0, channel_multiplier=1), reads=C, writes=C)
        P(lambda e: e.memset(U_f[:], 1.0), writes=C)
        P(lambda e: e.affine_select(out=U_f[:], in_=U_f[:], pattern=[[1, 128]], compare_op=ALU.is_ge,
                                    fill=0.0, base=0, channel_multiplier=-1), reads=C, writes=C)
        P(lambda e: e.memset(e0col[:], 0.0), writes=C)
        P(lambda e: e.affine_select(out=e0col[:], in_=e0col[:], pattern=[[0, 2]], compare_op=ALU.not_equal,
                                    fill=1.0, base=0, channel_multiplier=1), reads=C, writes=C)
        P(lambda e: e.iota(iq[:], pattern=[[1, 512]], base=0, channel_multiplier=0,
                           allow_small_or_imprecise_dtypes=True), writes=C)
        P(lambda e: e.iota(ikcol[:], pattern=[[0, 1]], base=0, channel_multiplier=1,
                           allow_small_or_imprecise_dtypes=True), writes=C)
        ks.dma("sp", lambda e: e.dma_start(out=smalls[:], in_=smallv[:, :]), writes=C)

        def load_w(st, name, src_ap, ncols, eng="sp", kchunks=8):
            stg = SB(st, name + "_f", [128, kchunks, ncols], F32)
            wb = SB(st, name + "_b", [128, kchunks, ncols], BF16)
            bs, bw = Buf(), Buf()
            ks.dma(eng, lambda e: e.dma_start(out=stg[:], in_=src_ap.rearrange("(c p) n -> p c n", p=128)), writes=[bs])
            P(lambda e: e.tensor_copy(out=wb[:], in_=stg[:]), reads=[bs], writes=[bw])
            return wb, bw

        def rms_to_T(xt, bx, grep, bg, sc, dstT_fn, dst_bufs, extra_reads=()):
            sq, ss, rstd, hn = sc["sq"], sc["ss"], sc["rstd"], sc["hn"]
            bsc = sc["buf"]
            A(lambda e: e.activation(out=sq[:], in_=xt[:], func=AF.Square, accum_out=ss[:]),
              reads=[bx], writes=[bsc])
            V(lambda e: e.tensor_scalar(out=rstd[:], in0=ss[:], scalar1=1.0 / D, scalar2=EPS, op0=ALU.mult, op1=ALU.add),
              reads=[bsc], writes=[bsc])
            A(lambda e: e.activation(out=rstd[:], in_=rstd[:], func=AF.Sqrt), reads=[bsc], writes=[bsc])
            V(lambda e: e.reciprocal(out=rstd[:], in_=rstd[:]), reads=[bsc], writes=[bsc])
            V(lambda e: e.scalar_tensor_tensor(out=hn[:], in0=xt[:], scalar=rstd[:, 0:1], in1=grep[:],
                                               op0=ALU.mult, op1=ALU.mult), reads=[bx, bg, bsc], writes=[bsc])
            pst, bpst = nextT()
            for c in range(8):
                T(lambda e, c=c: e.transpose(out=pst[:, c * 128:(c + 1) * 128], in_=hn[:, c * 128:(c + 1) * 128],
                                             identity=ident_b[:]), reads=[bsc, B_const], writes=[bpst])
            dstT_fn(pst, bpst)

        def proj_fm(wb, bw, col0, blk, ps, bps, ncols=128):
            rd = [bw] + B_hnT[blk * 4:(blk + 1) * 4]
            for c in range(8):
                T(lambda e, c=c: e.matmul(ps[0:ncols, :], lhsT=wb[:, c, col0:col0 + ncols],
                                          rhs=hnT[:, c, blk * 512:(blk + 1) * 512], start=(c == 0), stop=(c == 7)),
                  reads=rd, writes=[bps])

        def proj_tm(wb, bw, col0, ncols, tt, ps, bps, pcol0=0):
            rd = [bw, B_hnT[tt]]
            for c in range(8):
                T(lambda e, c=c: e.matmul(ps[:, pcol0:pcol0 + ncols], lhsT=hnT[:, c, tt * 128:(tt + 1) * 128],
                                          rhs=wb[:, c, col0:col0 + ncols], start=(c == 0), stop=(c == 7)),
                  reads=rd, writes=[bps])

        def phase_A(sq_i):
            with contextlib.ExitStack() as st:
                set_psum(st, 0, 2)
                grep = SB(st, "gA", [128, D], F32)
                bg = Buf()
                ks.dma("sp", lambda e: e.dma_start(out=grep[:], in_=gvec[0, :, :]), writes=[bg])
                xts = [SB(st, "xtA%d" % i, [128, D], F32) for i in range(2)]
                bxs = [Buf(), Buf()]
                scs = [dict(sq=SB(st, "sqA%d" % i, [128, D], F32), ss=SB(st, "ssA%d" % i, [128, 1], F32),
                            rstd=SB(st, "rsA%d" % i, [128, 1], F32), hn=SB(st, "hnA%d" % i, [128, D], BF16),
                            buf=Buf()) for i in range(2)]
                for tt in range(NT):
                    xt, bx, sc = xts[tt % 2], bxs[tt % 2], scs[tt % 2]
                    ks.dma("sp", lambda e, tt=tt, xt=xt: e.dma_start(out=xt[:], in_=x_in[sq_i, tt * 128:(tt + 1) * 128, :]),
                           writes=[bx])

                    def dst(pst, bpst, tt=tt):
                        A(lambda e: e.copy(out=hnT[:, :, tt * 128:(tt + 1) * 128],
                                           in_=pst[:, :].rearrange("p (c n) -> p c n", c=8)),
                          reads=[bpst], writes=[B_hnT[tt]])
                    rms_to_T(xt, bx, grep, bg, sc, dst, None)
            ks.fence()

        def attn_head(sq_i, mode, h, wsrc, cols, yrow0):
            with contextlib.ExitStack() as st:
                set_psum(st, 3, 1)
                pso = [PS(st, "pso", [128, 512], F32) for _ in range(4)]
                bpso = [Buf() for _ in range(4)]
                wq, bwq = load_w(st, "wq", wsrc[:, cols["q"]:cols["q"] + 128], 128)
                wk, bwk = load_w(st, "wk", wsrc[:, cols["k"]:cols["k"] + 128], 128, eng="pool")
                wv, bwv = load_w(st, "wv", wsrc[:, cols["v"]:cols["v"] + 128], 128)
                wz, bwz = load_w(st, "wz", wsrc[:, cols["z"]:cols["z"] + 128], 128, eng="pool")
                qT = SB(st, "qT", [128, S], BF16)
                kT = SB(st, "kT", [128, S], BF16)
                szT = SB(st, "szT", [128, S], BF16)
                vtm = SB(st, "vtm", [128, NT, 130], BF16)
                b_q = [Buf() for _ in range(NBQ)]
                b_k = [Buf() for _ in range(NBQ)]
                b_z = [Buf() for _ in range(NBQ)]
                b_v = [Buf() for _ in range(NT)]
                b_vones = Buf()
                P(lambda e: e.memset(vtm[:, :, 128:130], 1.0), writes=[b_vones])
                for blk in range(NBQ):
                    ps, bps = nextA()
                    proj_fm(wq, bwq, 0, blk, ps, bps)
                    A(lambda e, ps=ps, blk=blk: e.activation(out=qT[:, blk * 512:(blk + 1) * 512], in_=ps[:, :],
                                                             func=AF.Copy, scale=QSCALE), reads=[bps], writes=[b_q[blk]])
                    ps, bps = nextA()
                    proj_fm(wk, bwk, 0, blk, ps, bps)
                    V(lambda e, ps=ps, blk=blk: e.tensor_copy(out=kT[:, blk * 512:(blk + 1) * 512], in_=ps[:, :]),
                      reads=[bps], writes=[b_k[blk]])
                    ps, bps = nextA()
                    proj_fm(wz, bwz, 0, blk, ps, bps)
                    A(lambda e, ps=ps, blk=blk: e.activation(out=szT[:, blk * 512:(blk + 1) * 512], in_=ps[:, :],
                                                             func=AF.Silu), reads=[bps], writes=[b_z[blk]])
                    ps, bps = nextA()
                    for s in range(4):
                        proj_tm(wv, bwv, 0, 128, blk * 4 + s, ps, bps, pcol0=s * 128)
                    V(lambda e, ps=ps, blk=blk: e.tensor_copy(
                        out=vtm[:, blk * 4:(blk + 1) * 4, 0:128], in_=ps[:, :].rearrange("p (s n) -> p s n", s=4)),
                      reads=[bps], writes=b_v[blk * 4:(blk + 1) * 4])

                if mode == "fox":
                    wf_s = SB(st, "wf_s", [128, 8, 1], F32)
                    wf_b = SB(st, "wf_b", [128, 8, 128], BF16)
                    bwf = Buf()
                    ks.dma("sp", lambda e: e.dma_start(out=wf_s[:, :, 0], in_=wfc[h, :, :]), writes=[bwf])
                    P(lambda e: e.tensor_copy(out=wf_b[:], in_=wf_s[:].to_broadcast([128, 8, 128])),
                      reads=[bwf], writes=[bwf])
                    crep = SB(st, "crep", [128, S], F32)
                    ltmp = SB(st, "ltmp", [128, 512], F32)
                    negb = SB(st, "negb", [128, 1], F32)
                    b_c = [Buf() for _ in range(NBQ)]
                    b_l = Buf()
                    V(lambda e: e.tensor_scalar(out=negb[:], in0=smalls[:, 8 + h:9 + h], scalar1=-1.0, scalar2=None,
                                                op0=ALU.mult), reads=[B_const], writes=[b_l])
                    for blk in range(NBQ):
                        ps, bps = nextA()
                        proj_fm(wf_b, bwf, 0, blk, ps, bps)
                        A(lambda e, ps=ps: e.activation(out=ltmp[:], in_=ps[:, :], func=AF.Exp, bias=negb[:, 0:1], scale=-1.0),
                          reads=[bps, b_l], writes=[b_l])
                        A(lambda e: e.activation(out=ltmp[:], in_=ltmp[:], func=AF.Ln, bias=1.0, scale=1.0),
                          reads=[b_l], writes=[b_l])
                        init = zero_c[:, 0:1] if blk == 0 else crep[:, blk * 512 - 1:blk * 512]
                        V(lambda e, blk=blk, init=init: e.tensor_tensor_scan(
                            out=crep[:, blk * 512:(blk + 1) * 512], data0=ones_f[:, 0:1].to_broadcast([128, 512]),
                            data1=ltmp[:], initial=init, op0=ALU.mult, op1=ALU.add),
                          reads=[b_l, B_const] + ([b_c[blk - 1]] if blk else []), writes=[b_c[blk]])
                    ckcol = SB(st, "ckcol", [128, NT], F32)
                    b_ck = Buf()
                    ps, bps = nextA()
                    for j in range(NT):
                        T(lambda e, j=j, ps=ps: e.matmul(ps[:, 2 * j:2 * j + 2], lhsT=crep[:, j * 128:(j + 1) * 128],
                                                         rhs=e0col[:, 0:2], start=True, stop=True),
                          reads=[b_c[j // 4], B_const], writes=[bps])
                    V(lambda e, ps=ps: e.tensor_copy(out=ckcol[:], in_=ps[:, 0:2 * NT].rearrange("p (j t) -> p j t", t=2)[:, :, 0]),
                      reads=[bps], writes=[b_ck])
                else:
                    slope = slopes[h]
                    nd = NT + 4
                    kbt = SB(st, "kbt", [128, nd], F32)
                    b_kb = Buf()
                    for m in range(nd):
                        V(lambda e, m=m: e.tensor_scalar(out=kbt[:, m:m + 1], in0=ikcol[:], scalar1=slope,
                                                         scalar2=-slope * 128.0 * (m - 3), op0=ALU.mult, op1=ALU.add),
                          reads=[B_const], writes=[b_kb])
                    kms = SB(st, "kms", [128, NB], F32)
                    kmb = SB(st, "kmb", [128, NB], BF16)
                    b_km = Buf()
                    V(lambda e: e.tensor_reduce(out=kms[:], in_=kT[:].rearrange("p (n l) -> p n l", l=256), axis=AX.X,
                                                op=ALU.add), reads=b_k, writes=[b_km])
                    V(lambda e: e.tensor_scalar(out=kmb[:], in0=kms[:], scalar1=1.0 / 256, scalar2=None, op0=ALU.mult),
                      reads=[b_km], writes=[b_km])
                    past01 = SB(st, "past01", [128, NB, NB], F32)
                    pastneg = SB(st, "pastneg", [128, NB, NB], F32)
                    ownb = SB(st, "ownb", [128, NB, NB], F32)
                    b_tab = Buf()
                    P(lambda e: e.memset(past01[:], 1.0), writes=[b_tab])
                    P(lambda e: e.affine_select(out=past01[:], in_=past01[:], pattern=[[1, NB], [-1, NB]],
                                                compare_op=ALU.is_gt, fill=0.0, base=0, channel_multiplier=0),
                      reads=[b_tab], writes=[b_tab])
                    P(lambda e: e.tensor_scalar(out=pastneg[:], in0=past01[:], scalar1=-1.0, scalar2=BIG,
                                                op0=ALU.add, op1=ALU.mult), reads=[b_tab], writes=[b_tab])
                    P(lambda e: e.memset(ownb[:], 0.0), writes=[b_tab])
                    P(lambda e: e.affine_select(out=ownb[:], in_=ownb[:], pattern=[[1, NB], [-1, NB]],
                                                compare_op=ALU.is_equal, fill=-BIG, base=0, channel_multiplier=0),
                      reads=[b_tab], writes=[b_tab])
                    E_f = SB(st, "E_f", [128, NB, 128], F32)
                    E_b = SB(st, "E_b", [128, NB, 128], BF16)
                    P(lambda e: e.memset(E_f[:], 1.0), writes=[b_tab])
                    P(lambda e: e.affine_select(out=E_f[:], in_=E_f[:], pattern=[[-1, NB], [0, 128]],
                                                compare_op=ALU.is_equal, fill=0.0, base=0, channel_multiplier=1),
                      reads=[b_tab], writes=[b_tab])
                    P(lambda e: e.tensor_copy(out=E_b[:], in_=E_f[:]), reads=[b_tab], writes=[b_tab])
                    selT = SB(st, "selT", [128, S], BF16)
                    b_sel = [Buf() for _ in range(NBQ)]
                    gm = SB(st, "gm", [128, NB], F32)
                    top8 = SB(st, "top8", [128, 8], F32)
                    t2 = SB(st, "t2", [128, NB], F32)
                    seln = SB(st, "seln", [128, NB], BF16)
                    b_g = Buf()
                    for blk in range(NBQ):
                        pst, bpst = nextT()
                        for s in range(4):
                            tt = blk * 4 + s
                            own = tt // 2
                            ps, bps = nextA()
                            T(lambda e, ps=ps, tt=tt: e.matmul(ps[:, 0:NB], lhsT=qT[:, tt * 128:(tt + 1) * 128], rhs=kmb[:, :],
                                                               start=True, stop=True), reads=[b_q[blk], b_km], writes=[bps])
                            V(lambda e, ps=ps, own=own: e.tensor_tensor(out=gm[:], in0=ps[:, 0:NB], in1=pastneg[:, own, :],
                                                                        op=ALU.add), reads=[bps, b_tab], writes=[b_g])
                            V(lambda e: e.max(out=top8[:], in_=gm[:]), reads=[b_g], writes=[b_g])
                            V(lambda e, own=own: e.scalar_tensor_tensor(out=t2[:], in0=gm[:], scalar=top8[:, 2:3],
                                                                        in1=past01[:, own, :], op0=ALU.is_ge, op1=ALU.mult),
                              reads=[b_g, b_tab], writes=[b_g])
                            V(lambda e, own=own: e.scalar_tensor_tensor(out=seln[:], in0=t2[:], scalar=BIG,
                                                                        in1=ownb[:, own, :], op0=ALU.mult, op1=ALU.add),
                              reads=[b_g, b_tab], writes=[b_g])
                            T(lambda e, pst=pst, s=s: e.transpose(out=pst[0:NB, s * 128:(s + 1) * 128], in_=seln[:, :],
                                                                  identity=ident_b[:]), reads=[b_g, B_const], writes=[bpst])
                        V(lambda e, pst=pst, blk=blk: e.tensor_copy(out=selT[0:NB, blk * 512:(blk + 1) * 512],
                                                                    in_=pst[0:NB, 0:512]), reads=[bpst], writes=[b_sel[blk]])

                LA = 2
                NBUF = LA + 1
                tmps = [SB(st, "atmp", [128, 512], F32) for i in range(NBUF)]
                pTs = [SB(st, "apT", [128, 512], BF16) for i in range(NBUF)]
                b_tmp = [Buf() for _ in range(NBUF)]
                b_pT = [Buf() for _ in range(NBUF)]
                on = [SB(st, "aon", [128, 128], BF16) for i in range(2)]
                rden = [SB(st, "ard", [128, 1], F32) for i in range(2)]
                b_on = [Buf(), Buf()]
                yblk = [SB(st, "ayb", [128, 512], BF16) for i in range(2)]
                b_yb = [Buf(), Buf()]
                its = [(I, j) for I in range(NBQ) for j in range(4 * I + 4)]

                def stage1(n):
                    I, j = its[n]
                    off = max(0, j - 4 * I)
                    c0 = off * 128
                    ps, bps = nextA()
                    diag = j >= 4 * I
                    T(lambda e: e.matmul(ps[:, c0:512], lhsT=kT[:, j * 128:(j + 1) * 128],
                                         rhs=qT[:, I * 512 + c0:(I + 1) * 512],
                                         start=True, stop=False, skip_group_check=True),
                      reads=[b_k[j // 4], b_q[I]], writes=[bps])
                    if diag:
                        T(lambda e: e.matmul(ps[:, c0:c0 + 128], lhsT=ident_b[:], rhs=maskT_b[:],
                                             start=False, stop=False, skip_group_check=True),
                          reads=[B_const], writes=[bps])
                    if mode == "moba":
                        T(lambda e: e.matmul(ps[:, c0:512], lhsT=E_b[0:NB, j // 2, :],
                                             rhs=selT[0:NB, I * 512 + c0:(I + 1) * 512],
                                             start=False, stop=True, skip_group_check=True),
                          reads=[b_tab, b_sel[I]], writes=[bps])
                    tmp, btm = tmps[n % NBUF], b_tmp[n % NBUF]
                    pT, bpT = pTs[n % NBUF], b_pT[n % NBUF]
                    if mode == "fox":
                        V(lambda e: e.tensor_tensor(out=tmp[:, c0:512], in0=ps[:, c0:512],
                                                    in1=crep[:, I * 512 + c0:(I + 1) * 512], op=ALU.subtract),
                          reads=[bps, b_c[I]], writes=[btm])
                        A(lambda e: e.activation(out=pT[:, c0:512], in_=tmp[:, c0:512], func=AF.Exp,
                                                 bias=ckcol[:, j:j + 1], scale=1.0),
                          reads=[btm, b_ck], writes=[bpT])
                    else:
                        m = (I * 4 - j) + 3
                        V(lambda e: e.scalar_tensor_tensor(out=tmp[:, c0:512], in0=iq[:, c0:512], scalar=-slope,
                                                           in1=ps[:, c0:512], op0=ALU.mult, op1=ALU.add),
                          reads=[bps, B_const], writes=[btm])
                        A(lambda e: e.activation(out=pT[:, c0:512], in_=tmp[:, c0:512], func=AF.Exp,
                                                 bias=kbt[:, m:m + 1], scale=1.0),
                          reads=[btm, b_kb], writes=[bpT])

                def stage2(n):
                    I, j = its[n]
                    off = max(0, j - 4 * I)
                    pT, bpT = pTs[n % NBUF], b_pT[n % NBUF]
                    pb = 2 * (I % 2)
                    for s in range(off, 4):
                        bank = pb + s // 2
                        oc = (s % 2) * 256
                        first = (j == 0 and s % 2 == 0)
                        T(lambda e, s=s, bank=bank, oc=oc, first=first: e.matmul(
                            pso[bank][:, oc:oc + 129], lhsT=pT[:, s * 128:(s + 1) * 128], rhs=vtm[:, j, 0:129],
                            start=first, stop=False, skip_group_check=True),
                          reads=[bpT, b_v[j], b_vones], writes=[bpso[bank]])
                    if j != 4 * I + 3:
                        return
                    yb, byb = yblk[I % 2], b_yb[I % 2]
                    for s in range(4):
                        bank = pb + s // 2
                        oc = (s % 2) * 256
                        o_n, rd_, bo = on[s % 2], rden[s % 2], b_on[s % 2]
                        V(lambda e, bank=bank, oc=oc, rd_=rd_: e.reciprocal(out=rd_[:], in_=pso[bank][:, oc + 128:oc + 129]),
                          reads=[bpso[bank]], writes=[bo])
                        V(lambda e, bank=bank, oc=oc, rd_=rd_, o_n=o_n: e.tensor_scalar(
                            out=o_n[:], in0=pso[bank][:, oc:oc + 128], scalar1=rd_[:, 0:1], scalar2=None, op0=ALU.mult),
                          reads=[bpso[bank], bo], writes=[bo])
                        pst, bpst = nextT()
                        T(lambda e, pst=pst, o_n=o_n: e.transpose(out=pst[:, 0:128], in_=o_n[:], identity=ident_b[:]),
                          reads=[bo, B_const], writes=[bpst])
                        V(lambda e, pst=pst, s=s: e.tensor_tensor(
                            out=yb[:, s * 128:(s + 1) * 128], in0=pst[:, 0:128],
                            in1=szT[:, I * 512 + s * 128:I * 512 + (s + 1) * 128], op=ALU.mult),
                          reads=[bpst, b_z[I]], writes=[byb])
                    ks.dma("pool", lambda e: e.dma_start(
                        out=yT[sq_i, yrow0:yrow0 + 128, I * 512:(I + 1) * 512], in_=yb[:]),
                           reads=[byb], writes=[B_yT])

                N_it = len(its)
                for n in range(N_it + LA):
                    if n < N_it:
                        stage1(n)
                    if n - LA >= 0:
                        stage2(n - LA)
            ks.fence()

        B_yT = Buf()
        B_xres = Buf()

        def gdn_prep(sq_i, st):
            wab, bwab = load_w(st, "wab", w_ab[:, A_A:A_A + 8], 8)
            names = ["g", "beta", "G", "eG", "eGlm", "gl", "bG"]
            tl = {n: SB(st, "gp_" + n, [128, NT, 4], F32) for n in names}
            b = Buf()
            negA = SB(st, "negA", [128, 4], F32)
            tl["negA"] = negA
            A(lambda e: e.activation(out=negA[:], in_=smalls[:, 0:4], func=AF.Exp), reads=[B_const], writes=[b])
            V(lambda e: e.tensor_scalar(out=negA[:], in0=negA[:], scalar1=-1.0, scalar2=None, op0=ALU.mult),
              reads=[b], writes=[b])
            t4 = SB(st, "gp_t4", [128, 4], F32)
            for tt in range(NT):
                ps, bps = nextA()
                proj_tm(wab, bwab, 0, 8, tt, ps, bps)
                V(lambda e, ps=ps: e.tensor_tensor(out=t4[:], in0=ps[:, 0:4], in1=smalls[:, 4:8], op=ALU.add),
                  reads=[bps, B_const], writes=[b])
                A(lambda e: e.activation(out=t4[:], in_=t4[:], func=AF.Exp), reads=[b], writes=[b])
                A(lambda e: e.activation(out=t4[:], in_=t4[:], func=AF.Ln, bias=1.0, scale=1.0), reads=[b], writes=[b])
                V(lambda e, tt=tt: e.tensor_tensor(out=tl["g"][:, tt, :], in0=t4[:], in1=negA[:], op=ALU.mult),
                  reads=[b], writes=[b])
                A(lambda e, ps=ps, tt=tt: e.activation(out=tl["beta"][:, tt, :], in_=ps[:, 4:8], func=AF.Sigmoid),
                  reads=[bps], writes=[b])
                ps2, bps2 = nextA()
                T(lambda e, ps2=ps2, tt=tt: e.matmul(ps2[:, 0:4], lhsT=U_f[:], rhs=tl["g"][:, tt, :], start=True, stop=True),
                  reads=[b, B_const], writes=[bps2])
                T(lambda e, ps2=ps2, tt=tt: e.matmul(ps2[:, 8:12], lhsT=ones_f[:], rhs=tl["g"][:, tt, :], start=True, stop=True),
                  reads=[b, B_const], writes=[bps2])
                V(lambda e, ps2=ps2, tt=tt: e.tensor_copy(out=tl["G"][:, tt, :], in_=ps2[:, 0:4]), reads=[bps2], writes=[b])
                A(lambda e, ps2=ps2, tt=tt: e.activation(out=tl["eG"][:, tt, :], in_=ps2[:, 0:4], func=AF.Exp),
                  reads=[bps2], writes=[b])
                A(lambda e, ps2=ps2, tt=tt: e.activation(out=tl["gl"][:, tt, :], in_=ps2[:, 8:12], func=AF.Exp),
                  reads=[bps2], writes=[b])
                V(lambda e, ps2=ps2, tt=tt: e.tensor_tensor(out=tl["eGlm"][:, tt, :], in0=ps2[:, 8:12], in1=tl["G"][:, tt, :],
                                                           op=ALU.subtract), reads=[bps2, b], writes=[b])
                A(lambda e, tt=tt: e.activation(out=tl["eGlm"][:, tt, :], in_=tl["eGlm"][:, tt, :], func=AF.Exp),
                  reads=[b], writes=[b])
                V(lambda e, tt=tt: e.tensor_tensor(out=tl["bG"][:, tt, :], in0=tl["beta"][:, tt, :], in1=tl["eG"][:, tt, :],
                                                   op=ALU.mult), reads=[b], writes=[b])
            tl["buf"] = b
            return tl

        def gdn_head(sq_i, h, tl, dg, b_dg):
            bsc = tl["buf"]
            with contextlib.ExitStack() as st:
                wq, bwq = load_w(st, "gwq", w_ab[:, A_Q + h * 128:A_Q + (h + 1) * 128], 128)
                wk, bwk = load_w(st, "gwk", w_ab[:, A_K + h * 128:A_K + (h + 1) * 128], 128, eng="pool")
                wv, bwv = load_w(st, "gwv", w_ab[:, A_V + h * 128:A_V + (h + 1) * 128], 128)
                wz, bwz = load_w(st, "gwz", w_ab[:, A_Z + h * 128:A_Z + (h + 1) * 128], 128, eng="pool")
                uT = SB(st, "uT", [128, S + 4], BF16)
                b_u = Buf()
                outT = {n: SB(st, "g_%sT" % n, [128, S], BF16) for n in ("q", "k", "v")}
                b_o = {n: [Buf() for _ in range(NBQ)] for n in ("q", "k", "v")}
                cs = SB(st, "g_cs", [128, 512], F32)
                sqb = SB(st, "g_sqb", [128, 512], BF16)
                rs = SB(st, "g_rs", [128, 512], F32)
                b_cs = Buf()
                P(lambda e: e.memset(uT[:, 0:4], 0.0), writes=[b_u])
                for ci, (n, wb, bw) in enumerate((("q", wq, bwq), ("k", wk, bwk), ("v", wv, bwv))):
                    chunk = ci * 4 + h
                    for blk in range(NBQ):
                        ps, bps = nextA()
                        proj_fm(wb, bw, 0, blk, ps, bps)
                        V(lambda e, ps=ps, blk=blk: e.tensor_copy(out=uT[:, 4 + blk * 512:4 + (blk + 1) * 512], in_=ps[:, :]),
                          reads=[bps], writes=[b_u])
                    for blk in range(NBQ):
                        ps, bps = nextA()
                        for j in range(4):
                            T(lambda e, ps=ps, j=j, blk=blk, chunk=chunk: e.matmul(
                                ps[:, :], lhsT=dg[:, chunk * 4 + j, :], rhs=uT[:, 1 + j + blk * 512:1 + j + (blk + 1) * 512],
                                start=(j == 0), stop=(j == 3)), reads=[b_u, b_dg], writes=[bps])
                        if n == "v":
                            A(lambda e, ps=ps, blk=blk: e.activation(out=outT["v"][:, blk * 512:(blk + 1) * 512], in_=ps[:, :],
                                                                     func=AF.Silu), reads=[bps], writes=[b_o["v"][blk]])
                        else:
                            A(lambda e, ps=ps: e.activation(out=cs[:], in_=ps[:, :], func=AF.Silu), reads=[bps], writes=[b_cs])
                            V(lambda e: e.tensor_tensor(out=sqb[:], in0=cs[:], in1=cs[:], op=ALU.mult), reads=[b_cs], writes=[b_cs])
                            ps2, bps2 = nextA()
                            T(lambda e, ps2=ps2: e.matmul(ps2[:, :], lhsT=ones_b[:], rhs=sqb[:], start=True, stop=True),
                              reads=[b_cs, B_const], writes=[bps2])
                            V(lambda e, ps2=ps2: e.tensor_scalar(out=rs[:], in0=ps2[:, :], scalar1=EPS, scalar2=None, op0=ALU.add),
                              reads=[bps2], writes=[b_cs])
                            A(lambda e: e.activation(out=rs[:], in_=rs[:], func=AF.Sqrt), reads=[b_cs], writes=[b_cs])
                            V(lambda e: e.reciprocal(out=rs[:], in_=rs[:]), reads=[b_cs], writes=[b_cs])
                            sc_ = QSCALE if n == "q" else 1.0
                            V(lambda e, blk=blk, n=n, sc_=sc_: e.scalar_tensor_tensor(
                                out=outT[n][:, blk * 512:(blk + 1) * 512], in0=cs[:], scalar=sc_, in1=rs[:],
                                op0=ALU.mult, op1=ALU.mult), reads=[b_cs], writes=[b_o[n][blk]])
                Sst = SB(st, "g_S", [128, 128], F32)
                b_S = Buf()
                V(lambda e: e.memset(Sst[:], 0.0), writes=[b_S])
                NPING = 2
                def mk(nm, shape, dtp):
                    return [SB(st, "g_%s%d" % (nm, i), shape, dtp) for i in range(NPING)]
                gb = mk("gb", [128, 128], F32)
                dec = mk("dec", [128, 128], F32)
                decT = mk("decT", [128, 128], F32)
                Am = [mk("A%d" % l, [128, 128], F32) for l in range(7)]
                AmT = [mk("AT%d" % l, [128, 128], F32) for l in range(7)]
                rr_ = mk("r", [128, 256], F32)
                kdec = mk("kdec", [128, 128], F32)
                qkT = mk("qkT", [128, 128], F32)
                wT = mk("wT", [128, 128], F32)
                vnew = mk("vnew", [128, 128], F32)
                qS = mk("qS", [128, 128], F32)
                osb = mk("o", [128, 128], F32)
                osq = mk("osq", [128, 128], F32)
                oss = mk("oss", [128, 1], F32)
                on_ = mk("on", [128, 128], F32)
                szt = mk("szt", [128, 128], F32)
                yb_ = mk("yb", [128, 128], BF16)
                ybT = [SB(st, "g_ybT%d" % i, [128, 512], BF16) for i in range(2)]
                b_ybT = [Buf(), Buf()]
                qnf = mk("qnf", [128, 128], F32)
                knf = mk("knf", [128, 128], F32)
                bt = [Buf() for _ in range(NPING)]
                for tt in range(NT):
                    pi = tt % NPING
                    b = bt[pi]
                    blk = tt // 4
                    sl = slice(tt * 128, (tt + 1) * 128)
                    col = lambda name: tl[name][:, tt, h:h + 1]
                    rdq, rdk, rdv = [b_o["q"][blk]], [b_o["k"][blk]], [b_o["v"][blk]]
                    V(lambda e, pi=pi, tt=tt: e.tensor_scalar(out=gb[pi][:], in0=ones_f[:], scalar1=tl["g"][:, tt, h:h + 1],
                                                              scalar2=None, op0=ALU.mult), reads=[bsc, B_const], writes=[b])
                    psG, bpsG = nextA()
                    T(lambda e, psG=psG, pi=pi: e.matmul(psG[:, 0:128], lhsT=gb[pi][:], rhs=U_f[:], start=True, stop=True),
                      reads=[b, B_const], writes=[bpsG])
                    V(lambda e, psG=psG, pi=pi, tt=tt: e.scalar_tensor_tensor(
                        out=dec[pi][:], in0=psG[:, 0:128], scalar=tl["G"][:, tt, h:h + 1], in1=maskA[:],
                        op0=ALU.subtract, op1=ALU.add), reads=[bpsG, bsc, B_const], writes=[b])
                    A(lambda e, pi=pi: e.activation(out=dec[pi][:], in_=dec[pi][:], func=AF.Exp, scale=-1.0), reads=[b], writes=[b])
                    V(lambda e, psG=psG, pi=pi, tt=tt: e.scalar_tensor_tensor(
                        out=decT[pi][:], in0=psG[:, 0:128], scalar=tl["G"][:, tt, h:h + 1], in1=maskQ[:],
                        op0=ALU.subtract, op1=ALU.subtract), reads=[bpsG, bsc, B_const], writes=[b])
                    A(lambda e, pi=pi: e.activation(out=decT[pi][:], in_=decT[pi][:], func=AF.Exp), reads=[b], writes=[b])
                    psK, bpsK = nextA()
                    T(lambda e, psK=psK, sl=sl: e.matmul(psK[:, 0:128], lhsT=outT["k"][:, sl], rhs=outT["k"][:, sl],
                                                         start=True, stop=True), reads=rdk, writes=[bpsK])
                    T(lambda e, psK=psK, sl=sl: e.matmul(psK[:, 128:256], lhsT=outT["k"][:, sl], rhs=outT["q"][:, sl],
                                                         start=True, stop=True), reads=rdk + rdq, writes=[bpsK])
                    V(lambda e, psK=psK, pi=pi, tt=tt: e.scalar_tensor_tensor(
                        out=Am[0][pi][:], in0=psK[:, 0:128], scalar=tl["beta"][:, tt, h:h + 1], in1=dec[pi][:],
                        op0=ALU.mult, op1=ALU.mult), reads=[bpsK, bsc, b], writes=[b])
                    V(lambda e, psK=psK, pi=pi: e.tensor_tensor(out=qkT[pi][:], in0=psK[:, 128:256], in1=decT[pi][:], op=ALU.mult),
                      reads=[bpsK, b], writes=[b])
                    pst, bpst = nextT()
                    T(lambda e, pst=pst, sl=sl: e.transpose(out=pst[:, 0:128], in_=outT["k"][:, sl], identity=ident_b[:]),
                      reads=rdk + [B_const], writes=[bpst])
                    T(lambda e, pst=pst, sl=sl: e.transpose(out=pst[:, 128:256], in_=outT["v"][:, sl], identity=ident_b[:]),
                      reads=rdv + [B_const], writes=[bpst])
                    V(lambda e, pst=pst, pi=pi, tt=tt: e.tensor_scalar(out=rr_[pi][:, 0:128], in0=pst[:, 128:256],
                                                                       scalar1=tl["beta"][:, tt, h:h + 1], scalar2=None, op0=ALU.mult),
                      reads=[bpst, bsc], writes=[b])
                    V(lambda e, pst=pst, pi=pi, tt=tt: e.tensor_scalar(out=rr_[pi][:, 128:256], in0=pst[:, 0:128],
                                                                       scalar1=tl["bG"][:, tt, h:h + 1], scalar2=None, op0=ALU.mult),
                      reads=[bpst, bsc], writes=[b])
                    V(lambda e, pst=pst, pi=pi, tt=tt: e.tensor_scalar(out=kdec[pi][:], in0=pst[:, 0:128],
                                                                       scalar1=tl["eGlm"][:, tt, h:h + 1], scalar2=None, op0=ALU.mult),
                      reads=[bpst, bsc], writes=[b])
                    psX, bpsX = nextA()
                    T(lambda e, psX=psX, pi=pi: e.matmul(psX[:, 0:128], lhsT=Am[0][pi][:], rhs=ident_f[:], start=True, stop=True),
                      reads=[b, B_const], writes=[bpsX])
                    V(lambda e, psX=psX, pi=pi: e.tensor_copy(out=AmT[0][pi][:], in_=psX[:, 0:128]), reads=[bpsX], writes=[b])
                    for l in range(6):
                        psX, bpsX = nextA()
                        T(lambda e, psX=psX, pi=pi, l=l: e.matmul(psX[:, 0:128], lhsT=Am[l][pi][:], rhs=AmT[l][pi][:],
                                                                  start=True, stop=True), reads=[b], writes=[bpsX])
                        if l < 5:
                            T(lambda e, psX=psX, pi=pi, l=l: e.matmul(psX[:, 128:256], lhsT=AmT[l][pi][:], rhs=Am[l][pi][:],
                                                                      start=True, stop=True), reads=[b], writes=[bpsX])
                            A(lambda e, psX=psX, pi=pi, l=l: e.copy(out=Am[l + 1][pi][:], in_=psX[:, 128:256]), reads=[bpsX], writes=[b])
                        V(lambda e, psX=psX, pi=pi, l=l: e.tensor_copy(out=AmT[l + 1][pi][:], in_=psX[:, 0:128]), reads=[bpsX], writes=[b])
                    for l in range(6, -1, -1):
                        psX, bpsX = nextA()
                        T(lambda e, psX=psX, pi=pi, l=l: e.matmul(psX[:, 0:256], lhsT=AmT[l][pi][:], rhs=rr_[pi][:],
                                                                  start=True, stop=True), reads=[b], writes=[bpsX])
                        op_ = ALU.add if l > 0 else ALU.subtract
                        V(lambda e, psX=psX, pi=pi, op_=op_: e.tensor_tensor(out=rr_[pi][:], in0=rr_[pi][:], in1=psX[:, 0:256], op=op_),
                          reads=[bpsX, b], writes=[b])
                    psX, bpsX = nextA()
                    T(lambda e, psX=psX, pi=pi: e.matmul(psX[:, 0:128], lhsT=rr_[pi][:, 128:256], rhs=ident_f[:], start=True, stop=True),
                      reads=[b, B_const], writes=[bpsX])
                    A(lambda e, psX=psX, pi=pi: e.copy(out=wT[pi][:], in_=psX[:, 0:128]), reads=[bpsX], writes=[b])
                    P(lambda e, pi=pi, sl=sl: e.tensor_copy(out=qnf[pi][:], in_=outT["q"][:, sl]), reads=rdq, writes=[b])
                    psR, bpsR = nextA()
                    T(lambda e, psR=psR, pi=pi: e.matmul(psR[:, 0:128], lhsT=wT[pi][:], rhs=Sst[:], start=True, stop=True),
                      reads=[b, b_S], writes=[bpsR])
                    T(lambda e, psR=psR, pi=pi: e.matmul(psR[:, 128:256], lhsT=qnf[pi][:], rhs=Sst[:], start=True, stop=True),
                      reads=[b, b_S], writes=[bpsR])
                    V(lambda e, psR=psR, pi=pi: e.tensor_tensor(out=vnew[pi][:], in0=rr_[pi][:, 0:128], in1=psR[:, 0:128],
                                                                op=ALU.subtract), reads=[bpsR, b], writes=[b])
                    A(lambda e, psR=psR, pi=pi, tt=tt: e.activation(out=qS[pi][:], in_=psR[:, 128:256], func=AF.Copy,
                                                                    scale=tl["eG"][:, tt, h:h + 1]), reads=[bpsR, bsc], writes=[b])
                    psO, bpsO = nextA()
                    T(lambda e, psO=psO, pi=pi: e.matmul(psO[:, 0:128], lhsT=qkT[pi][:], rhs=vnew[pi][:], start=True, stop=True),
                      reads=[b], writes=[bpsO])
                    T(lambda e, psO=psO, pi=pi: e.matmul(psO[:, 128:256], lhsT=kdec[pi][:], rhs=vnew[pi][:], start=True, stop=True),
                      reads=[b], writes=[bpsO])
                    V(lambda e, psO=psO, tt=tt: e.scalar_tensor_tensor(out=Sst[:], in0=Sst[:], scalar=tl["gl"][:, tt, h:h + 1],
                                                                       in1=psO[:, 128:256], op0=ALU.mult, op1=ALU.add),
                      reads=[bpsO, bsc, b_S], writes=[b_S])
                    V(lambda e, psO=psO, pi=pi: e.tensor_tensor(out=osb[pi][:], in0=qS[pi][:], in1=psO[:, 0:128], op=ALU.add),
                      reads=[bpsO, b], writes=[b])
                    A(lambda e, pi=pi: e.activation(out=osq[pi][:], in_=osb[pi][:], func=AF.Square, accum_out=oss[pi][:]),
                      reads=[b], writes=[b])
                    V(lambda e, pi=pi: e.tensor_scalar(out=oss[pi][:], in0=oss[pi][:], scalar1=1.0 / HD, scalar2=EPS,
                                                       op0=ALU.mult, op1=ALU.add), reads=[b], writes=[b])
                    A(lambda e, pi=pi: e.activation(out=oss[pi][:], in_=oss[pi][:], func=AF.Sqrt), reads=[b], writes=[b])
                    V(lambda e, pi=pi: e.reciprocal(out=oss[pi][:], in_=oss[pi][:]), reads=[b], writes=[b])
                    V(lambda e, pi=pi: e.scalar_tensor_tensor(out=on_[pi][:], in0=osb[pi][:], scalar=oss[pi][:, 0:1],
                                                              in1=smalls[:, 16:144], op0=ALU.mult, op1=ALU.mult),
                      reads=[b, B_const], writes=[b])
                    psZ, bpsZ = nextA()
                    proj_tm(wz, bwz, 0, 128, tt, psZ, bpsZ)
                    A(lambda e, psZ=psZ, pi=pi: e.activation(out=szt[pi][:], in_=psZ[:, 0:128], func=AF.Silu), reads=[bpsZ], writes=[b])
                    V(lambda e, pi=pi: e.tensor_tensor(out=yb_[pi][:], in0=on_[pi][:], in1=szt[pi][:], op=ALU.mult),
                      reads=[b], writes=[b])
                    pst, bpst = nextT()
                    T(lambda e, pst=pst, pi=pi: e.transpose(out=pst[:, 0:128], in_=yb_[pi][:], identity=ident_b[:]),
                      reads=[b, B_const], writes=[bpst])
                    ybt, bybt = ybT[blk % 2], b_ybT[blk % 2]
                    s = tt % 4
                    A(lambda e, pst=pst, ybt=ybt, s=s: e.copy(out=ybt[:, s * 128:(s + 1) * 128], in_=pst[:, 0:128]),
                      reads=[bpst], writes=[bybt])
                    if s == 3:
                        ks.dma("pool", lambda e, ybt=ybt, blk=blk: e.dma_start(
                            out=yT[sq_i, h * 128:(h + 1) * 128, blk * 512:(blk + 1) * 512], in_=ybt[:]),
                               reads=[bybt], writes=[B_yT])
            ks.fence()

        def gdn_all(sq_i):
            with contextlib.ExitStack() as st:
                set_psum(st, 6, 2)
                cw = SB(st, "cw", [128, 12, 4], F32)
                dg = SB(st, "dg", [128, 48, 128], BF16)
                b_dg = Buf()
                ks.dma("sp", lambda e: e.dma_start(out=cw[:], in_=convw[:, :, :]), writes=[b_dg])
                for c in range(12):
                    for j in range(4):
                        P(lambda e, c=c, j=j: e.tensor_scalar(out=dg[:, c * 4 + j, :], in0=ident_f[:], scalar1=cw[:, c, j:j + 1],
                                                              scalar2=None, op0=ALU.mult), reads=[b_dg, B_const], writes=[b_dg])
                tl = gdn_prep(sq_i, st)
                for h in range(4):
                    gdn_head(sq_i, h, tl, dg, b_dg)

        def phase_C(sq_i, layer):
            last = (layer == 1)
            xsrc = x_in if layer == 0 else xres
            with contextlib.ExitStack() as st:
                set_psum(st, 6, 2)
                wo, bwo = None, None
                stg = SB(st, "c_stg", [128, 8, 512], F32)
                b_stg = Buf()
                wo = SB(st, "c_wo", [128, 8, D], BF16)
                wg = SB(st, "c_wg", [128, 8, D], BF16)
                wp = SB(st, "c_wp", [128, 2, D], BF16)
                bwo, bwg, bwp = Buf(), Buf(), Buf()
                for (dst, bdst, src) in ((wo, bwo, w_out[layer]), (wg, bwg, w_gate[layer])):
                    for hh in range(2):
                        ks.dma("sp", lambda e, src=src, hh=hh: e.dma_start(
                            out=stg[:], in_=src[:, hh * 512:(hh + 1) * 512].rearrange("(c p) n -> p c n", p=128)),
                               writes=[b_stg])
                        P(lambda e, dst=dst, hh=hh: e.tensor_copy(out=dst[:, :, hh * 512:(hh + 1) * 512], in_=stg[:]),
                          reads=[b_stg], writes=[bdst])
                for hh in range(2):
                    ks.dma("sp", lambda e, hh=hh: e.dma_start(
                        out=stg[:, 0:2, :], in_=w_pp[layer, :, hh * 512:(hh + 1) * 512].rearrange("(c p) n -> p c n", p=128)),
                           writes=[b_stg])
                    P(lambda e, hh=hh: e.tensor_copy(out=wp[:, :, hh * 512:(hh + 1) * 512], in_=stg[:, 0:2, :]),
                      reads=[b_stg], writes=[bwp])
                gP = SB(st, "c_gP", [128, D], F32)
                gN = SB(st, "c_gN", [128, D], F32)
                bgP, bgN = Buf(), Buf()
                ks.dma("sp", lambda e: e.dma_start(out=gP[:], in_=gvec[2 + layer, :, :]), writes=[bgP])
                ks.dma("sp", lambda e: e.dma_start(out=gN[:], in_=gvec[4 if last else 1, :, :]), writes=[bgN])
                NP_ = 2
                xt = [SB(st, "c_x%d" % i, [128, D], F32) for i in range(NP_)]
                yl = [SB(st, "c_yl%d" % i, [128, 8, 128], BF16) for i in range(NP_)]
                ptf = [SB(st, "c_ptf%d" % i, [128, 2, 128], F32) for i in range(NP_)]
                ptb = [SB(st, "c_ptb%d" % i, [128, 2, 128], BF16) for i in range(NP_)]
                x1 = [SB(st, "c_x1%d" % i, [128, D], F32) for i in range(NP_)]
                hT = [SB(st, "c_hT%d" % i, [128, 8, 128], BF16) for i in range(NP_)]
                gt = [SB(st, "c_gt%d" % i, [128, D], F32) for i in range(NP_)]
                x2 = [SB(st, "c_x2%d" % i, [128, D], F32) for i in range(NP_)]
                ot = [SB(st, "c_ot%d" % i, [128, D], F32) for i in range(NP_)]
                scs = [dict(sq=SB(st, "c_sq%d" % i, [128, D], F32), ss=SB(st, "c_ss%d" % i, [128, 1], F32),
                            rstd=SB(st, "c_rs%d" % i, [128, 1], F32), hn=SB(st, "c_hn%d" % i, [128, D], BF16),
                            buf=Buf()) for i in range(NP_)]
                bx = [Buf() for _ in range(NP_)]
                byl = [Buf() for _ in range(NP_)]
                bpt = [Buf() for _ in range(NP_)]
                bx1 = [Buf() for _ in range(NP_)]
                bhT = [Buf() for _ in range(NP_)]
                bgt = [Buf() for _ in range(NP_)]
                bx2 = [Buf() for _ in range(NP_)]
                bot = [Buf() for _ in range(NP_)]
                for tt in range(NT):
                    i = tt % NP_
                    tsl = slice(tt * 128, (tt + 1) * 128)
                    ks.dma("sp", lambda e, i=i, tsl=tsl: e.dma_start(out=xt[i][:], in_=xsrc[sq_i, tsl, :]),
                           reads=[B_xres], writes=[bx[i]])
                    ks.dma("sp", lambda e, i=i, tsl=tsl: e.dma_start(
                        out=yl[i][:], in_=yT[sq_i, :, tsl].rearrange("(c p) n -> p c n", p=128)), reads=[B_yT], writes=[byl[i]])
                    ks.dma("sp", lambda e, i=i, tsl=tsl: e.dma_start(
                        out=ptf[i][:], in_=pT_in[layer, sq_i, :, tsl].rearrange("(c p) n -> p c n", p=128)), writes=[bpt[i]])
                    P(lambda e, i=i: e.tensor_copy(out=ptb[i][:], in_=ptf[i][:]), reads=[bpt[i]], writes=[bpt[i]])
                    for hh in range(2):
                        ps, bps = nextA()
                        for c in range(8):
                            T(lambda e, ps=ps, c=c, hh=hh, i=i: e.matmul(ps[:, :], lhsT=yl[i][:, c, :],
                                                                         rhs=wo[:, c, hh * 512:(hh + 1) * 512],
                                                                         start=(c == 0), stop=(c == 7)),
                              reads=[byl[i], bwo], writes=[bps])
                        V(lambda e, ps=ps, hh=hh, i=i: e.tensor_tensor(out=x1[i][:, hh * 512:(hh + 1) * 512], in0=ps[:, :],
                                                                       in1=xt[i][:, hh * 512:(hh + 1) * 512], op=ALU.add),
                          reads=[bps, bx[i]], writes=[bx1[i]])

                    def dstg(pst, bpst, i=i):
                        A(lambda e: e.copy(out=hT[i][:], in_=pst[:, :].rearrange("p (c n) -> p c n", c=8)),
                          reads=[bpst], writes=[bhT[i]])
                    rms_to_T(x1[i], bx1[i], gP, bgP, scs[i], dstg, None)
                    for hh in range(2):
                        ps, bps = nextA()
                        for c in range(8):
                            T(lambda e, ps=ps, c=c, hh=hh, i=i: e.matmul(ps[:, :], lhsT=hT[i][:, c, :],
                                                                         rhs=wg[:, c, hh * 512:(hh + 1) * 512],
                                                                         start=(c == 0), stop=(c == 7)),
                              reads=[bhT[i], bwg], writes=[bps])
                        A(lambda e, ps=ps, hh=hh, i=i: e.activation(out=gt[i][:, hh * 512:(hh + 1) * 512], in_=ps[:, :],
                                                                    func=AF.Sigmoid), reads=[bps], writes=[bgt[i]])
                        ps, bps = nextA()
                        for c in range(2):
                            T(lambda e, ps=ps, c=c, hh=hh, i=i: e.matmul(ps[:, :], lhsT=ptb[i][:, c, :],
                                                                         rhs=wp[:, c, hh * 512:(hh + 1) * 512],
                                                                         start=(c == 0), stop=(c == 1)),
                              reads=[bpt[i], bwp], writes=[bps])
                        V(lambda e, ps=ps, hh=hh, i=i: e.tensor_tensor(out=gt[i][:, hh * 512:(hh + 1) * 512],
                                                                       in0=gt[i][:, hh * 512:(hh + 1) * 512], in1=ps[:, :],
                                                                       op=ALU.mult), reads=[bps, bgt[i]], writes=[bgt[i]])
                    P(lambda e, i=i: e.tensor_tensor(out=x2[i][:], in0=gt[i][:], in1=x1[i][:], op=ALU.add),
                      reads=[bgt[i], bx1[i]], writes=[bx2[i]])
                    if not last:
                        ks.dma("pool", lambda e, i=i, tsl=tsl: e.dma_start(out=xres[sq_i, tsl, :], in_=x2[i][:]),
                               reads=[bx2[i]], writes=[B_xres])

                        def dstn(pst, bpst, tt=tt):
                            A(lambda e: e.copy(out=hnT[:, :, tt * 128:(tt + 1) * 128],
                                               in_=pst[:, :].rearrange("p (c n) -> p c n", c=8)),
                              reads=[bpst], writes=[B_hnT[tt]])
                        rms_to_T(x2[i], bx2[i], gN, bgN, scs[i], dstn, None)
                    else:
                        sc = scs[i]
                        bsc = sc["buf"]
                        A(lambda e, i=i, sc=sc: e.activation(out=sc["sq"][:], in_=x2[i][:], func=AF.Square, accum_out=sc["ss"][:]),
                          reads=[bx2[i]], writes=[bsc])
                        V(lambda e, sc=sc: e.tensor_scalar(out=sc["rstd"][:], in0=sc["ss"][:], scalar1=1.0 / D, scalar2=EPS,
                                                           op0=ALU.mult, op1=ALU.add), reads=[bsc], writes=[bsc])
                        A(lambda e, sc=sc: e.activation(out=sc["rstd"][:], in_=sc["rstd"][:], func=AF.Sqrt), reads=[bsc], writes=[bsc])
                        V(lambda e, sc=sc: e.reciprocal(out=sc["rstd"][:], in_=sc["rstd"][:]), reads=[bsc], writes=[bsc])
                        V(lambda e, i=i, sc=sc: e.scalar_tensor_tensor(out=ot[i][:], in0=x2[i][:], scalar=sc["rstd"][:, 0:1],
                                                                       in1=gN[:], op0=ALU.mult, op1=ALU.mult),
                          reads=[bx2[i], bgN, bsc], writes=[bot[i]])
                        ks.dma("pool", lambda e, i=i, tsl=tsl: e.dma_start(out=out[sq_i, tsl, :], in_=ot[i][:]),
                               reads=[bot[i]], writes=[B_out])
            ks.fence()

        B_out = Buf()

        def zero_yT(sq_i, r0, r1):
            with contextlib.ExitStack() as st:
                z = SB(st, "zz", [128, S], BF16)
                bz = Buf()
                P(lambda e: e.memset(z[:], 0.0), writes=[bz])
                for r in range(r0, r1, 128):
                    ks.dma("pool", lambda e, r=r: e.dma_start(out=yT[sq_i, r:r + 128, :], in_=z[:]), reads=[bz], writes=[B_yT])
            ks.fence()

        for sq_i in range(NSEQ):
            phase_A(sq_i)
            if do_gdn:
                gdn_all(sq_i)
                ks.fence()
            else:
                zero_yT(sq_i, 0, 512)
            if do_moba:
                for h in range(4):
                    attn_head(sq_i, "moba", h, w_ab,
                              dict(q=B_Q + h * 128, k=B_K + h * 128, v=B_V + h * 128, z=A_Z + 512 + h * 128), 512 + h * 128)
            else:
                zero_yT(sq_i, 512, 1024)
            phase_C(sq_i, 0)
            if do_fox:
                for h in range(8):
                    attn_head(sq_i, "fox", h, w_c,
                              dict(q=C_Q + h * 128, k=C_K + h * 128, v=C_V + h * 128, z=C_Z + h * 128, f=C_F + h), h * 128)
            else:
                zero_yT(sq_i, 0, 1024)
            phase_C(sq_i, 1)
        ks.emit()
    return nc


def prep_inputs(inp, nseq, ncores):
    f = lambda a: np.ascontiguousarray(np.asarray(a, dtype=np.float32))
    x = f(inp["x"])
    p = f(inp["p"])
    gv = np.stack([inp["norm_g"][0], inp["norm_g"][1], inp["ple_norm_g"][0], inp["ple_norm_g"][1], inp["final_g"]], 0)
    gv = f(np.broadcast_to(np.asarray(gv, np.float32)[:, None, :], (5, 128, D)))
    cw = np.asarray(inp["conv_w"], np.float32)[0]
    convw = f(cw.T.reshape(12, 128, 4).transpose(1, 0, 2))
    sv = np.concatenate([np.asarray(inp["a_log"], np.float32)[0], np.asarray(inp["dt_bias"], np.float32)[0],
                         np.asarray(inp["forget_b"], np.float32)[0], np.asarray(inp["gdn_norm_g"], np.float32)[0]])
    smallv = f(np.broadcast_to(sv[None, :], (128, 144)))
    shared = dict(w_in_ab=f(inp["w_in_ab"][0]), w_in_c=f(inp["w_in_c"][0]), w_out_ab=f(inp["w_out_ab"][0]),
                  w_out_c=f(inp["w_out_c"][0]), w_ple_gate=f(inp["w_ple_gate"]), w_ple_proj=f(inp["w_ple_proj"]),
                  gvec=gv, convw=convw, smallv=smallv,
                  wfc=f(np.asarray(inp["w_in_c"], np.float32)[0][:, C_F:C_F + 8].T.reshape(8, 8, 128).transpose(0, 2, 1)))
    maps = []
    for c in range(ncores):
        sl = slice(c * nseq, (c + 1) * nseq)
        m = dict(shared)
        m["x"] = f(x[sl])
        m["pT"] = f(p[:, sl].transpose(0, 1, 3, 2))
        maps.append(m)
    return maps


def kernel(**inputs):
    x = np.asarray(inputs["x"])
    Bsz, S, _ = x.shape
    ncores = 8
    nseq = Bsz // ncores
    nc = build(S, nseq)
    maps = prep_inputs(inputs, nseq, ncores)
    res = run_bass_kernel_spmd(nc, maps, core_ids=list(range(ncores)))
    return np.concatenate([np.asarray(r["out"], dtype=np.float32) for r in res.results], axis=0)
```

```python
import contextlib
import numpy as np
import concourse.bass as bass
import concourse.mybir as mybir
from concourse.bass_utils import run_bass_kernel_spmd

F32 = mybir.dt.float32
BF16 = mybir.dt.bfloat16
AF = mybir.ActivationFunctionType
ALU = mybir.AluOpType
AX = mybir.AxisListType

D = 1024
HD = 128
EPS = 1e-6
BIG = 1.0e30
QSCALE = HD ** -0.5


class Buf:
    __slots__ = ("w", "r", "excl")

    def __init__(self, excl=False):
        self.w = None
        self.r = []
        self.excl = excl


class KS:
    ENGS = ("pe", "dve", "act", "pool", "sp")

    def __init__(self, nc, n_dma_chan=8):
        self.nc = nc
        self.streams = {e: [] for e in self.ENGS}
        self.cnt = {e: 0 for e in self.ENGS}
        self.seen = {e: {} for e in self.ENGS}
        self.nchan = n_dma_chan
        self.chan_cnt = {}
        self.chan_rr = {e: 0 for e in self.ENGS}
        self.sems = {}

    def _deps(self, eng, reads, writes):
        need = {}

        def add(tok):
            if tok is None:
                return
            k, v = tok
            if k == eng and eng == "pe":
                return
            if need.get(k, 0) < v:
                need[k] = v
        for b in reads:
            add(b.w)
            if b.excl:
                for t in b.r:
                    if t[0] != eng:
                        add(t)
        for b in writes:
            add(b.w)
            for t in b.r:
                add(t)
        out = []
        seen = self.seen[eng]
        for k, v in need.items():
            if seen.get(k, 0) >= v:
                continue
            seen[k] = v
            out.append((k, v))
        return out

    def _commit(self, tok, reads, writes):
        for b in reads:
            if len(b.r) > 24:
                m = {}
                for k, v in b.r:
                    if m.get(k, 0) < v:
                        m[k] = v
                b.r = list(m.items())
            b.r.append(tok)
        for b in writes:
            b.w = tok
            b.r = []

    def op(self, eng, fn, reads=(), writes=()):
        waits = self._deps(eng, reads, writes)
        self.cnt[eng] += 1
        tok = (eng, self.cnt[eng])
        self.streams[eng].append((waits, fn, (eng, 1)))
        self._commit(tok, reads, writes)
        return tok

    def dma(self, eng, fn, reads=(), writes=()):
        c = self.chan_rr[eng]
        self.chan_rr[eng] = (c + 1) % self.nchan
        key = ("dma", eng, c)
        prev = self.chan_cnt.get(key, 0)
        waits = self._deps(eng, reads, writes)
        if prev and self.seen[eng].get(key, 0) < prev:
            self.seen[eng][key] = prev
            waits.append((key, prev))
        self.chan_cnt[key] = prev + 16
        tok = (key, prev + 16)
        self.streams[eng].append((waits, fn, (key, 16)))
        self._commit(tok, reads, writes)
        return tok

    def fence(self):
        cur = dict(self.cnt)
        cur.update(self.chan_cnt)
        for e in self.ENGS:
            waits = []
            for k, v in cur.items():
                if v <= 0 or (k == e):
                    continue
                if self.seen[e].get(k, 0) < v:
                    self.seen[e][k] = v
                    waits.append((k, v))
            if waits:
                self.streams[e].append((waits, None, None))

    def emit(self):
        nc = self.nc
        keys = list(self.ENGS) + sorted(self.chan_cnt.keys(), key=str)
        with contextlib.ExitStack() as st:
            for k in keys:
                nm = "s_" + ("_".join(map(str, k)) if isinstance(k, tuple) else k)
                self.sems[k] = st.enter_context(nc.semaphore(nm))
            block = st.enter_context(nc.Block())
            finals = {k: v for k, v in self.chan_cnt.items()}
            finals.update({e: v for e, v in self.cnt.items() if v > 0})

            def run(eng, h):
                for waits, fn, inc in self.streams[eng]:
                    for k, v in waits:
                        h.wait_ge(self.sems[k], v)
                    if fn is not None:
                        fn(h).then_inc(self.sems[inc[0]], inc[1])
                if eng == "sp":
                    for k, v in finals.items():
                        if k != "sp":
                            h.wait_ge(self.sems[k], v)

            @block.tensor
            def _(h):
                run("pe", h)

            @block.vector
            def _(h):
                run("dve", h)

            @block.scalar
            def _(h):
                run("act", h)

            @block.gpsimd
            def _(h):
                run("pool", h)

            @block.sync
            def _(h):
                run("sp", h)


A_Q, A_K, A_V = 0, 512, 1024
A_A, A_B = 1536, 1540
B_Q, B_K, B_V = 1544, 2056, 2568
A_Z = 3080
C_Q, C_K, C_V, C_F, C_Z = 0, 1024, 2048, 3072, 3080
IN_W = 4104


def build(S, NSEQ, do_gdn=True, do_moba=True, do_fox=True):
    NT = S // 128
    NBQ = S // 512
    NB = S // 256
    nc = bass.Bass("TRN2", target_bir_lowering=False)
    dt = nc.dram_tensor
    x_in = dt("x", [NSEQ, S, D], F32, kind="ExternalInput").ap()
    pT_in = dt("pT", [2, NSEQ, 256, S], F32, kind="ExternalInput").ap()
    w_ab = dt("w_in_ab", [D, IN_W], F32, kind="ExternalInput").ap()
    w_c = dt("w_in_c", [D, IN_W], F32, kind="ExternalInput").ap()
    w_out = [dt("w_out_ab", [D, D], F32, kind="ExternalInput").ap(),
             dt("w_out_c", [D, D], F32, kind="ExternalInput").ap()]
    w_gate = dt("w_ple_gate", [2, D, D], F32, kind="ExternalInput").ap()
    w_pp = dt("w_ple_proj", [2, 256, D], F32, kind="ExternalInput").ap()
    gvec = dt("gvec", [5, 128, D], F32, kind="ExternalInput").ap()
    convw = dt("convw", [128, 12, 4], F32, kind="ExternalInput").ap()
    smallv = dt("smallv", [128, 144], F32, kind="ExternalInput").ap()
    wfc = dt("wfc", [8, 128, 8], F32, kind="ExternalInput").ap()
    out = dt("out", [NSEQ, S, D], F32, kind="ExternalOutput").ap()
    xres = dt("xres", [NSEQ, S, D], F32, kind="Internal").ap()
    yT = dt("yT", [NSEQ, D, S], BF16, kind="Internal").ap()

    ks = KS(nc)
    slopes = [2.0 ** (-8.0 * (h + 1) / 4) for h in range(4)]

    with contextlib.ExitStack() as top:
        uid = [0]

        def SB(st, name, shape, dtype):
            uid[0] += 1
            return st.enter_context(nc.sbuf_tensor("%s_%d" % (name, uid[0]), shape, dtype))

        def PS(st, name, shape, dtype):
            uid[0] += 1
            return st.enter_context(nc.psum_tensor("%s_%d" % (name, uid[0]), shape, dtype))

        hnT = SB(top, "hnT", [128, 8, S], BF16)
        B_hnT = [Buf() for _ in range(NT)]
        ident_f = SB(top, "ident_f", [128, 128], F32)
        ident_b = SB(top, "ident_b", [128, 128], BF16)
        ones_f = SB(top, "ones_f", [128, 128], F32)
        ones_b = SB(top, "ones_b", [128, 128], BF16)
        maskT_b = SB(top, "maskT_b", [128, 128], BF16)
        maskA = SB(top, "maskA", [128, 128], F32)
        maskQ = SB(top, "maskQ", [128, 128], F32)
        U_f = SB(top, "U_f", [128, 128], F32)
        e0col = SB(top, "e0col", [128, 2], F32)
        iq = SB(top, "iq", [128, 512], F32)
        ikcol = SB(top, "ikcol", [128, 1], F32)
        tmpf = SB(top, "tmpf", [128, 128], F32)
        smalls = SB(top, "smalls", [128, 144], F32)
        zero_c = SB(top, "zero_c", [128, 1], F32)
        B_const = Buf()

        PSM = {"A": [], "BA": [], "T": [], "BT": []}
        rr = {"A": 0, "T": 0}

        def set_psum(st, nA, nT):
            PSM["A"] = [PS(st, "psA", [128, 512], F32) for _ in range(nA)]
            PSM["BA"] = [Buf(True) for _ in range(nA)]
            PSM["T"] = [PS(st, "psT", [128, 1024], BF16) for _ in range(nT)]
            PSM["BT"] = [Buf(True) for _ in range(nT)]
            rr["A"] = 0
            rr["T"] = 0

        def nextA():
            i = rr["A"]
            rr["A"] = (i + 1) % len(PSM["A"])
            return PSM["A"][i], PSM["BA"][i]

        def nextT():
            i = rr["T"]
            rr["T"] = (i + 1) % len(PSM["T"])
            return PSM["T"][i], PSM["BT"][i]

        P = lambda fn, **kw: ks.op("pool", fn, **kw)
        V = lambda fn, **kw: ks.op("dve", fn, **kw)
        A = lambda fn, **kw: ks.op("act", fn, **kw)
        T = lambda fn, **kw: ks.op("pe", fn, **kw)

        C = [B_const]
        P(lambda e: e.memset(ident_f[:], 0.0), writes=C)
        P(lambda e: e.affine_select(out=ident_f[:], in_=ident_f[:], pattern=[[-1, 128]], compare_op=ALU.not_equal,
                                    fill=1.0, base=0, channel_multiplier=1), reads=C, writes=C)
        P(lambda e: e.tensor_copy(out=ident_b[:], in_=ident_f[:]), reads=C, writes=C)
        P(lambda e: e.memset(ones_f[:], 1.0), writes=C)
        P(lambda e: e.memset(ones_b[:], 1.0), writes=C)
        P(lambda e: e.memset(zero_c[:], 0.0), writes=C)
        P(lambda e: e.memset(maskQ[:], 0.0), writes=C)
        P(lambda e: e.affine_select(out=maskQ[:], in_=maskQ[:], pattern=[[1, 128]], compare_op=ALU.is_ge,
                                    fill=BIG, base=0, channel_multiplier=-1), reads=C, writes=C)
        P(lambda e: e.tensor_scalar(out=maskT_b[:], in0=maskQ[:], scalar1=-1.0, scalar2=None, op0=ALU.mult),
          reads=C, writes=C)
        P(lambda e: e.memset(maskA[:], 0.0), writes=C)
        P(lambda e: e.affine_select(out=maskA[:], in_=maskA[:], pattern=[[-1, 128]], compare_op=ALU.is_gt,
                                    fill=BIG, base=0, channel_multiplier=1), reads=C, writes=C)
        P(lambda e: e.memset(U_f[:], 1.0), writes=C)
        P(lambda e: e.affine_select(out=U_f[:], in_=U_f[:], pattern=[[1, 128]], compare_op=ALU.is_ge,
                                    fill=0.0, base=0, channel_multiplier=-1), reads=C, writes=C)
        P(lambda e: e.memset(e0col[:], 0.0), writes=C)
        P(lambda e: e.affine_select(out=e0col[:], in_=e0col[:], pattern=[[0, 2]], compare_op=ALU.not_equal,
                                    fill=1.0, base=0, channel_multiplier=1), reads=C, writes=C)
        P(lambda e: e.iota(iq[:], pattern=[[1, 512]], base=0, channel_multiplier=0,
                           allow_small_or_imprecise_dtypes=True), writes=C)
        P(lambda e: e.iota(ikcol[:], pattern=[[0, 1]], base=0, channel_multiplier=1,
                           allow_small_or_imprecise_dtypes=True), writes=C)
        ks.dma("sp", lambda e: e.dma_start(out=smalls[:], in_=smallv[:, :]), writes=C)

        def load_w(st, name, src_ap, ncols, eng="sp", kchunks=8):
            stg = SB(st, name + "_f", [128, kchunks, ncols], F32)
            wb = SB(st, name + "_b", [128, kchunks, ncols], BF16)
            bs, bw = Buf(), Buf()
            ks.dma(eng, lambda e: e.dma_start(out=stg[:], in_=src_ap.rearrange("(c p) n -> p c n", p=128)), writes=[bs])
            P(lambda e: e.tensor_copy(out=wb[:], in_=stg[:]), reads=[bs], writes=[bw])
            return wb, bw

        def rms_to_T(xt, bx, grep, bg, sc, dstT_fn, dst_bufs, extra_reads=()):
            sq, ss, rstd, hn = sc["sq"], sc["ss"], sc["rstd"], sc["hn"]
            bsc = sc["buf"]
            A(lambda e: e.activation(out=sq[:], in_=xt[:], func=AF.Square, accum_out=ss[:]),
              reads=[bx], writes=[bsc])
            V(lambda e: e.tensor_scalar(out=rstd[:], in0=ss[:], scalar1=1.0 / D, scalar2=EPS, op0=ALU.mult, op1=ALU.add),
              reads=[bsc], writes=[bsc])
            A(lambda e: e.activation(out=rstd[:], in_=rstd[:], func=AF.Sqrt), reads=[bsc], writes=[bsc])
            V(lambda e: e.reciprocal(out=rstd[:], in_=rstd[:]), reads=[bsc], writes=[bsc])
            V(lambda e: e.scalar_tensor_tensor(out=hn[:], in0=xt[:], scalar=rstd[:, 0:1], in1=grep[:],
                                               op0=ALU.mult, op1=ALU.mult), reads=[bx, bg, bsc], writes=[bsc])
            pst, bpst = nextT()
            for c in range(8):
                T(lambda e, c=c: e.transpose(out=pst[:, c * 128:(c + 1) * 128], in_=hn[:, c * 128:(c + 1) * 128],
                                             identity=ident_b[:]), reads=[bsc, B_const], writes=[bpst])
            dstT_fn(pst, bpst)

        def proj_fm(wb, bw, col0, blk, ps, bps, ncols=128):
            rd = [bw] + B_hnT[blk * 4:(blk + 1) * 4]
            for c in range(8):
                T(lambda e, c=c: e.matmul(ps[0:ncols, :], lhsT=wb[:, c, col0:col0 + ncols],
                                          rhs=hnT[:, c, blk * 512:(blk + 1) * 512], start=(c == 0), stop=(c == 7)),
                  reads=rd, writes=[bps])

        def proj_tm(wb, bw, col0, ncols, tt, ps, bps, pcol0=0):
            rd = [bw, B_hnT[tt]]
            for c in range(8):
                T(lambda e, c=c: e.matmul(ps[:, pcol0:pcol0 + ncols], lhsT=hnT[:, c, tt * 128:(tt + 1) * 128],
                                          rhs=wb[:, c, col0:col0 + ncols], start=(c == 0), stop=(c == 7)),
                  reads=rd, writes=[bps])

        def phase_A(sq_i):
            with contextlib.ExitStack() as st:
                set_psum(st, 0, 2)
                grep = SB(st, "gA", [128, D], F32)
                bg = Buf()
                ks.dma("sp", lambda e: e.dma_start(out=grep[:], in_=gvec[0, :, :]), writes=[bg])
                xts = [SB(st, "xtA%d" % i, [128, D], F32) for i in range(2)]
                bxs = [Buf(), Buf()]
                scs = [dict(sq=SB(st, "sqA%d" % i, [128, D], F32), ss=SB(st, "ssA%d" % i, [128, 1], F32),
                            rstd=SB(st, "rsA%d" % i, [128, 1], F32), hn=SB(st, "hnA%d" % i, [128, D], BF16),
                            buf=Buf()) for i in range(2)]
                for tt in range(NT):
                    xt, bx, sc = xts[tt % 2], bxs[tt % 2], scs[tt % 2]
                    ks.dma("sp", lambda e, tt=tt, xt=xt: e.dma_start(out=xt[:], in_=x_in[sq_i, tt * 128:(tt + 1) * 128, :]),
                           writes=[bx])

                    def dst(pst, bpst, tt=tt):
                        A(lambda e: e.copy(out=hnT[:, :, tt * 128:(tt + 1) * 128],
                                           in_=pst[:, :].rearrange("p (c n) -> p c n", c=8)),
                          reads=[bpst], writes=[B_hnT[tt]])
                    rms_to_T(xt, bx, grep, bg, sc, dst, None)
            ks.fence()

        def attn_head(sq_i, mode, h, wsrc, cols, yrow0):
            with contextlib.ExitStack() as st:
                set_psum(st, 3, 1)
                pso = [PS(st, "pso", [128, 512], F32) for _ in range(4)]
                bpso = [Buf(True) for _ in range(4)]
                wq, bwq = load_w(st, "wq", wsrc[:, cols["q"]:cols["q"] + 128], 128)
                wk, bwk = load_w(st, "wk", wsrc[:, cols["k"]:cols["k"] + 128], 128, eng="pool")
                wv, bwv = load_w(st, "wv", wsrc[:, cols["v"]:cols["v"] + 128], 128)
                wz, bwz = load_w(st, "wz", wsrc[:, cols["z"]:cols["z"] + 128], 128, eng="pool")
                qT = SB(st, "qT", [128, S], BF16)
                kT = SB(st, "kT", [128, S], BF16)
                szT = SB(st, "szT", [128, S], BF16)
                vtm = SB(st, "vtm", [128, NT, 130], BF16)
                b_q = [Buf() for _ in range(NBQ)]
                b_k = [Buf() for _ in range(NBQ)]
                b_z = [Buf() for _ in range(NBQ)]
                b_v = [Buf() for _ in range(NT)]
                b_vones = Buf()
                P(lambda e: e.memset(vtm[:, :, 128:130], 1.0), writes=[b_vones])
                for blk in range(NBQ):
                    ps, bps = nextA()
                    proj_fm(wq, bwq, 0, blk, ps, bps)
                    A(lambda e, ps=ps, blk=blk: e.activation(out=qT[:, blk * 512:(blk + 1) * 512], in_=ps[:, :],
                                                             func=AF.Copy, scale=QSCALE), reads=[bps], writes=[b_q[blk]])
                    ps, bps = nextA()
                    proj_fm(wk, bwk, 0, blk, ps, bps)
                    V(lambda e, ps=ps, blk=blk: e.tensor_copy(out=kT[:, blk * 512:(blk + 1) * 512], in_=ps[:, :]),
                      reads=[bps], writes=[b_k[blk]])
                    ps, bps = nextA()
                    proj_fm(wz, bwz, 0, blk, ps, bps)
                    A(lambda e, ps=ps, blk=blk: e.activation(out=szT[:, blk * 512:(blk + 1) * 512], in_=ps[:, :],
                                                             func=AF.Silu), reads=[bps], writes=[b_z[blk]])
                    ps, bps = nextA()
                    for s in range(4):
                        proj_tm(wv, bwv, 0, 128, blk * 4 + s, ps, bps, pcol0=s * 128)
                    V(lambda e, ps=ps, blk=blk: e.tensor_copy(
                        out=vtm[:, blk * 4:(blk + 1) * 4, 0:128], in_=ps[:, :].rearrange("p (s n) -> p s n", s=4)),
                      reads=[bps], writes=b_v[blk * 4:(blk + 1) * 4])

                if mode == "fox":
                    wf_s = SB(st, "wf_s", [128, 8, 1], F32)
                    wf_b = SB(st, "wf_b", [128, 8, 128], BF16)
                    bwf = Buf()
                    ks.dma("sp", lambda e: e.dma_start(out=wf_s[:, :, 0], in_=wfc[h, :, :]), writes=[bwf])
                    P(lambda e: e.tensor_copy(out=wf_b[:], in_=wf_s[:].to_broadcast([128, 8, 128])),
                      reads=[bwf], writes=[bwf])
                    crep = SB(st, "crep", [128, S], F32)
                    ltmp = SB(st, "ltmp", [128, 512], F32)
                    negb = SB(st, "negb", [128, 1], F32)
                    b_c = [Buf() for _ in range(NBQ)]
                    b_l = Buf()
                    V(lambda e: e.tensor_scalar(out=negb[:], in0=smalls[:, 8 + h:9 + h], scalar1=-1.0, scalar2=None,
                                                op0=ALU.mult), reads=[B_const], writes=[b_l])
                    for blk in range(NBQ):
                        ps, bps = nextA()
                        proj_fm(wf_b, bwf, 0, blk, ps, bps)
                        A(lambda e, ps=ps: e.activation(out=ltmp[:], in_=ps[:, :], func=AF.Exp, bias=negb[:, 0:1], scale=-1.0),
                          reads=[bps, b_l], writes=[b_l])
                        A(lambda e: e.activation(out=ltmp[:], in_=ltmp[:], func=AF.Ln, bias=1.0, scale=1.0),
                          reads=[b_l], writes=[b_l])
                        init = zero_c[:, 0:1] if blk == 0 else crep[:, blk * 512 - 1:blk * 512]
                        V(lambda e, blk=blk, init=init: e.tensor_tensor_scan(
                            out=crep[:, blk * 512:(blk + 1) * 512], data0=ones_f[:, 0:1].to_broadcast([128, 512]),
                            data1=ltmp[:], initial=init, op0=ALU.mult, op1=ALU.add),
                          reads=[b_l, B_const] + ([b_c[blk - 1]] if blk else []), writes=[b_c[blk]])
                    ckcol = SB(st, "ckcol", [128, NT], F32)
                    b_ck = Buf()
                    ps, bps = nextA()
                    for j in range(NT):
                        T(lambda e, j=j, ps=ps: e.matmul(ps[:, 2 * j:2 * j + 2], lhsT=crep[:, j * 128:(j + 1) * 128],
                                                         rhs=e0col[:, 0:2], start=True, stop=True),
                          reads=[b_c[j // 4], B_const], writes=[bps])
                    V(lambda e, ps=ps: e.tensor_copy(out=ckcol[:], in_=ps[:, 0:2 * NT].rearrange("p (j t) -> p j t", t=2)[:, :, 0]),
                      reads=[bps], writes=[b_ck])
                else:
                    slope = slopes[h]
                    nd = NT + 4
                    kbt = SB(st, "kbt", [128, nd], F32)
                    b_kb = Buf()
                    for m in range(nd):
                        V(lambda e, m=m: e.tensor_scalar(out=kbt[:, m:m + 1], in0=ikcol[:], scalar1=slope,
                                                         scalar2=-slope * 128.0 * (m - 3), op0=ALU.mult, op1=ALU.add),
                          reads=[B_const], writes=[b_kb])
                    kms = SB(st, "kms", [128, NB], F32)
                    kmb = SB(st, "kmb", [128, NB], BF16)
                    b_km = Buf()
                    V(lambda e: e.tensor_reduce(out=kms[:], in_=kT[:].rearrange("p (n l) -> p n l", l=256), axis=AX.X,
                                                op=ALU.add), reads=b_k, writes=[b_km])
                    V(lambda e: e.tensor_scalar(out=kmb[:], in0=kms[:], scalar1=1.0 / 256, scalar2=None, op0=ALU.mult),
                      reads=[b_km], writes=[b_km])
                    past01 = SB(st, "past01", [128, NB, NB], F32)
                    pastneg = SB(st, "pastneg", [128, NB, NB], F32)
                    ownb = SB(st, "ownb", [128, NB, NB], F32)
                    b_tab = Buf()
                    P(lambda e: e.memset(past01[:], 1.0), writes=[b_tab])
                    P(lambda e: e.affine_select(out=past01[:], in_=past01[:], pattern=[[1, NB], [-1, NB]],
                                                compare_op=ALU.is_gt, fill=0.0, base=0, channel_multiplier=0),
                      reads=[b_tab], writes=[b_tab])
                    P(lambda e: e.tensor_scalar(out=pastneg[:], in0=past01[:], scalar1=-1.0, scalar2=BIG,
                                                op0=ALU.add, op1=ALU.mult), reads=[b_tab], writes=[b_tab])
                    P(lambda e: e.memset(ownb[:], 0.0), writes=[b_tab])
                    P(lambda e: e.affine_select(out=ownb[:], in_=ownb[:], pattern=[[1, NB], [-1, NB]],
                                                compare_op=ALU.is_equal, fill=-BIG, base=0, channel_multiplier=0),
                      reads=[b_tab], writes=[b_tab])
                    E_f = SB(st, "E_f", [128, NB, 128], F32)
                    E_b = SB(st, "E_b", [128, NB, 128], BF16)
                    P(lambda e: e.memset(E_f[:], 1.0), writes=[b_tab])
                    P(lambda e: e.affine_select(out=E_f[:], in_=E_f[:], pattern=[[-1, NB], [0, 128]],
                                                compare_op=ALU.is_equal, fill=0.0, base=0, channel_multiplier=1),
                      reads=[b_tab], writes=[b_tab])
                    P(lambda e: e.tensor_copy(out=E_b[:], in_=E_f[:]), reads=[b_tab], writes=[b_tab])
                    selT = SB(st, "selT", [128, S], BF16)
                    b_sel = [Buf() for _ in range(NBQ)]
                    gm = SB(st, "gm", [128, NB], F32)
                    top8 = SB(st, "top8", [128, 8], F32)
                    t2 = SB(st, "t2", [128, NB], F32)
                    seln = SB(st, "seln", [128, NB], BF16)
                    b_g = Buf()
                    for blk in range(NBQ):
                        pst, bpst = nextT()
                        for s in range(4):
                            tt = blk * 4 + s
                            own = tt // 2
                            ps, bps = nextA()
                            T(lambda e, ps=ps, tt=tt: e.matmul(ps[:, 0:NB], lhsT=qT[:, tt * 128:(tt + 1) * 128], rhs=kmb[:, :],
                                                               start=True, stop=True), reads=[b_q[blk], b_km], writes=[bps])
                            V(lambda e, ps=ps, own=own: e.tensor_tensor(out=gm[:], in0=ps[:, 0:NB], in1=pastneg[:, own, :],
                                                                        op=ALU.add), reads=[bps, b_tab], writes=[b_g])
                            V(lambda e: e.max(out=top8[:], in_=gm[:]), reads=[b_g], writes=[b_g])
                            V(lambda e, own=own: e.scalar_tensor_tensor(out=t2[:], in0=gm[:], scalar=top8[:, 2:3],
                                                                        in1=past01[:, own, :], op0=ALU.is_ge, op1=ALU.mult),
                              reads=[b_g, b_tab], writes=[b_g])
                            V(lambda e, own=own: e.scalar_tensor_tensor(out=seln[:], in0=t2[:], scalar=BIG,
                                                                        in1=ownb[:, own, :], op0=ALU.mult, op1=ALU.add),
                              reads=[b_g, b_tab], writes=[b_g])
                            T(lambda e, pst=pst, s=s: e.transpose(out=pst[0:NB, s * 128:(s + 1) * 128], in_=seln[:, :],
                                                                  identity=ident_b[:]), reads=[b_g, B_const], writes=[bpst])
                        V(lambda e, pst=pst, blk=blk: e.tensor_copy(out=selT[0:NB, blk * 512:(blk + 1) * 512],
                                                                    in_=pst[0:NB, 0:512]), reads=[bpst], writes=[b_sel[blk]])

                LA = 2
                NBUF = LA + 1
                tmps = [SB(st, "atmp", [128, 512], F32) for i in range(NBUF)]
                pTs = [SB(st, "apT", [128, 512], BF16) for i in range(NBUF)]
                b_tmp = [Buf() for _ in range(NBUF)]
                b_pT = [Buf() for _ in range(NBUF)]
                on = [SB(st, "aon", [128, 128], BF16) for i in range(2)]
                rden = [SB(st, "ard", [128, 1], F32) for i in range(2)]
                b_on = [Buf(), Buf()]
                yblk = [SB(st, "ayb", [128, 512], BF16) for i in range(2)]
                b_yb = [Buf(), Buf()]
                its = [(I, j) for I in range(NBQ) for j in range(4 * I + 4)]

                def stage1(n):
                    I, j = its[n]
                    off = max(0, j - 4 * I)
                    c0 = off * 128
                    ps, bps = nextA()
                    diag = j >= 4 * I
                    T(lambda e: e.matmul(ps[:, c0:512], lhsT=kT[:, j * 128:(j + 1) * 128],
                                         rhs=qT[:, I * 512 + c0:(I + 1) * 512],
                                         start=True, stop=False, skip_group_check=True),
                      reads=[b_k[j // 4], b_q[I]], writes=[bps])
                    if diag:
                        T(lambda e: e.matmul(ps[:, c0:c0 + 128], lhsT=ident_b[:], rhs=maskT_b[:],
                                             start=False, stop=False, skip_group_check=True),
                          reads=[B_const], writes=[bps])
                    if mode == "moba":
                        T(lambda e: e.matmul(ps[:, c0:512], lhsT=E_b[0:NB, j // 2, :],
                                             rhs=selT[0:NB, I * 512 + c0:(I + 1) * 512],
                                             start=False, stop=True, skip_group_check=True),
                          reads=[b_tab, b_sel[I]], writes=[bps])
                    tmp, btm = tmps[n % NBUF], b_tmp[n % NBUF]
                    pT, bpT = pTs[n % NBUF], b_pT[n % NBUF]
                    if mode == "fox":
                        V(lambda e: e.tensor_tensor(out=tmp[:, c0:512], in0=ps[:, c0:512],
                                                    in1=crep[:, I * 512 + c0:(I + 1) * 512], op=ALU.subtract),
                          reads=[bps, b_c[I]], writes=[btm])
                        A(lambda e: e.activation(out=pT[:, c0:512], in_=tmp[:, c0:512], func=AF.Exp,
                                                 bias=ckcol[:, j:j + 1], scale=1.0),
                          reads=[btm, b_ck], writes=[bpT])
                    else:
                        m = (I * 4 - j) + 3
                        V(lambda e: e.scalar_tensor_tensor(out=tmp[:, c0:512], in0=iq[:, c0:512], scalar=-slope,
                                                           in1=ps[:, c0:512], op0=ALU.mult, op1=ALU.add),
                          reads=[bps, B_const], writes=[btm])
                        A(lambda e: e.activation(out=pT[:, c0:512], in_=tmp[:, c0:512], func=AF.Exp,
                                                 bias=kbt[:, m:m + 1], scale=1.0),
                          reads=[btm, b_kb], writes=[bpT])

                def stage2(n):
                    I, j = its[n]
                    off = max(0, j - 4 * I)
                    pT, bpT = pTs[n % NBUF], b_pT[n % NBUF]
                    pb = 2 * (I % 2)
                    for s in range(off, 4):
                        bank = pb + s // 2
                        oc = (s % 2) * 256
                        first = (j == 0 and s % 2 == 0)
                        T(lambda e, s=s, bank=bank, oc=oc, first=first: e.matmul(
                            pso[bank][:, oc:oc + 129], lhsT=pT[:, s * 128:(s + 1) * 128], rhs=vtm[:, j, 0:129],
                            start=first, stop=False, skip_group_check=True),
                          reads=[bpT, b_v[j], b_vones], writes=[bpso[bank]])
                    if j != 4 * I + 3:
                        return
                    yb, byb = yblk[I % 2], b_yb[I % 2]
                    for s in range(4):
                        bank = pb + s // 2
                        oc = (s % 2) * 256
                        o_n, rd_, bo = on[s % 2], rden[s % 2], b_on[s % 2]
                        V(lambda e, bank=bank, oc=oc, rd_=rd_: e.reciprocal(out=rd_[:], in_=pso[bank][:, oc + 128:oc + 129]),
                          reads=[bpso[bank]], writes=[bo])
                        V(lambda e, bank=bank, oc=oc, rd_=rd_, o_n=o_n: e.tensor_scalar(
                            out=o_n[:], in0=pso[bank][:, oc:oc + 128], scalar1=rd_[:, 0:1], scalar2=None, op0=ALU.mult),
                          reads=[bpso[bank], bo], writes=[bo])
                        pst, bpst = nextT()
                        T(lambda e, pst=pst, o_n=o_n: e.transpose(out=pst[:, 0:128], in_=o_n[:], identity=ident_b[:]),
                          reads=[bo, B_const], writes=[bpst])
                        V(lambda e, pst=pst, s=s: e.tensor_tensor(
                            out=yb[:, s * 128:(s + 1) * 128], in0=pst[:, 0:128],
                            in1=szT[:, I * 512 + s * 128:I * 512 + (s + 1) * 128], op=ALU.mult),
                          reads=[bpst, b_z[I]], writes=[byb])
                    ks.dma("pool", lambda e: e.dma_start(
                        out=yT[sq_i, yrow0:yrow0 + 128, I * 512:(I + 1) * 512], in_=yb[:]),
                           reads=[byb], writes=[B_yT])

                N_it = len(its)
                for n in range(N_it + LA):
                    if n < N_it:
                        stage1(n)
                    if n - LA >= 0:
                        stage2(n - LA)
            ks.fence()

        B_yT = Buf()
        B_xres = Buf()

        def gdn_prep(sq_i, st):
            wab, bwab = load_w(st, "wab", w_ab[:, A_A:A_A + 8], 8)
            names = ["g", "beta", "G", "eG", "eGlm", "gl", "bG"]
            tl = {n: SB(st, "gp_" + n, [128, NT, 4], F32) for n in names}
            b = Buf()
            negA = SB(st, "negA", [128, 4], F32)
            tl["negA"] = negA
            A(lambda e: e.activation(out=negA[:], in_=smalls[:, 0:4], func=AF.Exp), reads=[B_const], writes=[b])
            V(lambda e: e.tensor_scalar(out=negA[:], in0=negA[:], scalar1=-1.0, scalar2=None, op0=ALU.mult),
              reads=[b], writes=[b])
            t4 = SB(st, "gp_t4", [128, 4], F32)
            for tt in range(NT):
                ps, bps = nextA()
                proj_tm(wab, bwab, 0, 8, tt, ps, bps)
                V(lambda e, ps=ps: e.tensor_tensor(out=t4[:], in0=ps[:, 0:4], in1=smalls[:, 4:8], op=ALU.add),
                  reads=[bps, B_const], writes=[b])
                A(lambda e: e.activation(out=t4[:], in_=t4[:], func=AF.Exp), reads=[b], writes=[b])
                A(lambda e: e.activation(out=t4[:], in_=t4[:], func=AF.Ln, bias=1.0, scale=1.0), reads=[b], writes=[b])
                V(lambda e, tt=tt: e.tensor_tensor(out=tl["g"][:, tt, :], in0=t4[:], in1=negA[:], op=ALU.mult),
                  reads=[b], writes=[b])
                A(lambda e, ps=ps, tt=tt: e.activation(out=tl["beta"][:, tt, :], in_=ps[:, 4:8], func=AF.Sigmoid),
                  reads=[bps], writes=[b])
                ps2, bps2 = nextA()
                T(lambda e, ps2=ps2, tt=tt: e.matmul(ps2[:, 0:4], lhsT=U_f[:], rhs=tl["g"][:, tt, :], start=True, stop=True),
                  reads=[b, B_const], writes=[bps2])
                T(lambda e, ps2=ps2, tt=tt: e.matmul(ps2[:, 8:12], lhsT=ones_f[:], rhs=tl["g"][:, tt, :], start=True, stop=True),
                  reads=[b, B_const], writes=[bps2])
                V(lambda e, ps2=ps2, tt=tt: e.tensor_copy(out=tl["G"][:, tt, :], in_=ps2[:, 0:4]), reads=[bps2], writes=[b])
                A(lambda e, ps2=ps2, tt=tt: e.activation(out=tl["eG"][:, tt, :], in_=ps2[:, 0:4], func=AF.Exp),
                  reads=[bps2], writes=[b])
                A(lambda e, ps2=ps2, tt=tt: e.activation(out=tl["gl"][:, tt, :], in_=ps2[:, 8:12], func=AF.Exp),
                  reads=[bps2], writes=[b])
                V(lambda e, ps2=ps2, tt=tt: e.tensor_tensor(out=tl["eGlm"][:, tt, :], in0=ps2[:, 8:12], in1=tl["G"][:, tt, :],
                                                           op=ALU.subtract), reads=[bps2, b], writes=[b])
                A(lambda e, tt=tt: e.activation(out=tl["eGlm"][:, tt, :], in_=tl["eGlm"][:, tt, :], func=AF.Exp),
                  reads=[b], writes=[b])
                V(lambda e, tt=tt: e.tensor_tensor(out=tl["bG"][:, tt, :], in0=tl["beta"][:, tt, :], in1=tl["eG"][:, tt, :],
                                                   op=ALU.mult), reads=[b], writes=[b])
            tl["buf"] = b
            return tl

        def gdn_head(sq_i, h, tl, dg, b_dg):
            bsc = tl["buf"]
            with contextlib.ExitStack() as st:
                wq, bwq = load_w(st, "gwq", w_ab[:, A_Q + h * 128:A_Q + (h + 1) * 128], 128)
                wk, bwk = load_w(st, "gwk", w_ab[:, A_K + h * 128:A_K + (h + 1) * 128], 128, eng="pool")
                wv, bwv = load_w(st, "gwv", w_ab[:, A_V + h * 128:A_V + (h + 1) * 128], 128)
                wz, bwz = load_w(st, "gwz", w_ab[:, A_Z + h * 128:A_Z + (h + 1) * 128], 128, eng="pool")
                uT = SB(st, "uT", [128, S + 4], BF16)
                b_u = Buf()
                outT = {n: SB(st, "g_%sT" % n, [128, S], BF16) for n in ("q", "k", "v")}
                b_o = {n: [Buf() for _ in range(NBQ)] for n in ("q", "k", "v")}
                cs = SB(st, "g_cs", [128, 512], F32)
                sqb = SB(st, "g_sqb", [128, 512], BF16)
                rs = SB(st, "g_rs", [128, 512], F32)
                b_cs = Buf()
                P(lambda e: e.memset(uT[:, 0:4], 0.0), writes=[b_u])
                for ci, (n, wb, bw) in enumerate((("q", wq, bwq), ("k", wk, bwk), ("v", wv, bwv))):
                    chunk = ci * 4 + h
                    for blk in range(NBQ):
                        ps, bps = nextA()
                        proj_fm(wb, bw, 0, blk, ps, bps)
                        V(lambda e, ps=ps, blk=blk: e.tensor_copy(out=uT[:, 4 + blk * 512:4 + (blk + 1) * 512], in_=ps[:, :]),
                          reads=[bps], writes=[b_u])
                    for blk in range(NBQ):
                        ps, bps = nextA()
                        for j in range(4):
                            T(lambda e, ps=ps, j=j, blk=blk, chunk=chunk: e.matmul(
                                ps[:, :], lhsT=dg[:, chunk * 4 + j, :], rhs=uT[:, 1 + j + blk * 512:1 + j + (blk + 1) * 512],
                                start=(j == 0), stop=(j == 3)), reads=[b_u, b_dg], writes=[bps])
                        if n == "v":
                            A(lambda e, ps=ps, blk=blk: e.activation(out=outT["v"][:, blk * 512:(blk + 1) * 512], in_=ps[:, :],
                                                                     func=AF.Silu), reads=[bps], writes=[b_o["v"][blk]])
                        else:
                            A(lambda e, ps=ps: e.activation(out=cs[:], in_=ps[:, :], func=AF.Silu), reads=[bps], writes=[b_cs])
                            V(lambda e: e.tensor_tensor(out=sqb[:], in0=cs[:], in1=cs[:], op=ALU.mult), reads=[b_cs], writes=[b_cs])
                            ps2, bps2 = nextA()
                            T(lambda e, ps2=ps2: e.matmul(ps2[:, :], lhsT=ones_b[:], rhs=sqb[:], start=True, stop=True),
                              reads=[b_cs, B_const], writes=[bps2])
                            V(lambda e, ps2=ps2: e.tensor_scalar(out=rs[:], in0=ps2[:, :], scalar1=EPS, scalar2=None, op0=ALU.add),
                              reads=[bps2], writes=[b_cs])
                            A(lambda e: e.activation(out=rs[:], in_=rs[:], func=AF.Sqrt), reads=[b_cs], writes=[b_cs])
                            V(lambda e: e.reciprocal(out=rs[:], in_=rs[:]), reads=[b_cs], writes=[b_cs])
                            sc_ = QSCALE if n == "q" else 1.0
                            V(lambda e, blk=blk, n=n, sc_=sc_: e.scalar_tensor_tensor(
                                out=outT[n][:, blk * 512:(blk + 1) * 512], in0=cs[:], scalar=sc_, in1=rs[:],
                                op0=ALU.mult, op1=ALU.mult), reads=[b_cs], writes=[b_o[n][blk]])
                class TB:
                    def __init__(self, name, shape, dtp):
                        self.t = SB(st, "g_" + name, shape, dtp)
                        self.b = Buf()
                G = 4
                Sst = TB("S", [128, 128], F32)
                V(lambda e: e.memset(Sst.t[:], 0.0), writes=[Sst.b])
                f128 = [128, 128]
                I_ = [dict(gb=TB("gb", f128, F32), dec=TB("dec", f128, F32), decT=TB("decT", f128, F32),
                           P=[TB("Pa", f128, F32), TB("Pb", f128, F32)], PT=[TB("PTa", f128, F32), TB("PTb", f128, F32)],
                           XT=[TB("XTa", f128, F32), TB("XTb", f128, F32)]) for _ in range(G)]
                O_ = [dict(rr=TB("rr", [128, 256], F32), qkT=TB("qkT", f128, F32), kdec=TB("kdec", f128, F32),
                           wT=TB("wT", f128, F32), qnf=TB("qnf", f128, F32)) for _ in range(2 * G)]
                Q_ = [dict(vnew=TB("vnew", f128, F32), qS=TB("qS", f128, F32), o=TB("o", f128, F32), osq=TB("osq", f128, F32),
                           oss=TB("oss", [128, 1], F32), on=TB("on", f128, F32), sz=TB("sz", f128, F32),
                           yb=TB("yb", f128, BF16)) for _ in range(2)]
                ybT = [TB("ybT", [128, 512], BF16) for _ in range(2)]
                kT_, qT_, vT_ = outT["k"], outT["q"], outT["v"]

                freeA = list(zip(PSM["A"], PSM["BA"]))
                freeT = list(zip(PSM["T"], PSM["BT"]))

                def acquire(nA=0, nT=0):
                    while len(freeA) < nA or len(freeT) < nT:
                        yield
                    ra = [freeA.pop(0) for _ in range(nA)]
                    rt = [freeT.pop(0) for _ in range(nT)]
                    return ra + rt

                def relA(*bk):
                    freeA.extend(bk)

                def relT(*bk):
                    freeT.extend(bk)

                def part1(tt, si, oi):
                    I, O = I_[si], O_[oi]
                    blk = tt // 4
                    sl = slice(tt * 128, (tt + 1) * 128)
                    cl = lambda nm: tl[nm][:, tt, h:h + 1]
                    rdq, rdk, rdv = [b_o["q"][blk]], [b_o["k"][blk]], [b_o["v"][blk]]
                    gb, dec, decT = I["gb"], I["dec"], I["decT"]
                    rr_, qkT, kdec, wT, qnf = O["rr"], O["qkT"], O["kdec"], O["wT"], O["qnf"]
                    V(lambda e: e.tensor_scalar(out=gb.t[:], in0=ones_f[:], scalar1=cl("g"), scalar2=None, op0=ALU.mult),
                      reads=[bsc, B_const], writes=[gb.b])
                    (psG, bpsG), (pst, bpst) = yield from acquire(1, 1)
                    T(lambda e: e.matmul(psG[:, 0:128], lhsT=gb.t[:], rhs=U_f[:], start=True, stop=True),
                      reads=[gb.b, B_const], writes=[bpsG])
                    T(lambda e: e.transpose(out=pst[:, 0:128], in_=kT_[:, sl], identity=ident_b[:]),
                      reads=rdk + [B_const], writes=[bpst])
                    T(lambda e: e.transpose(out=pst[:, 128:256], in_=vT_[:, sl], identity=ident_b[:]),
                      reads=rdv + [B_const], writes=[bpst])
                    yield
                    V(lambda e: e.scalar_tensor_tensor(out=dec.t[:], in0=psG[:, 0:128], scalar=cl("G"), in1=maskA[:],
                                                       op0=ALU.subtract, op1=ALU.add), reads=[bpsG, bsc, B_const], writes=[dec.b])
                    V(lambda e: e.scalar_tensor_tensor(out=decT.t[:], in0=psG[:, 0:128], scalar=cl("G"), in1=maskQ[:],
                                                       op0=ALU.subtract, op1=ALU.subtract), reads=[bpsG, bsc, B_const], writes=[decT.b])
                    V(lambda e: e.tensor_scalar(out=rr_.t[:, 0:128], in0=pst[:, 128:256], scalar1=cl("beta"), scalar2=None,
                                                op0=ALU.mult), reads=[bpst, bsc], writes=[rr_.b])
                    V(lambda e: e.tensor_scalar(out=rr_.t[:, 128:256], in0=pst[:, 0:128], scalar1=cl("bG"), scalar2=None,
                                                op0=ALU.mult), reads=[bpst, bsc], writes=[rr_.b])
                    V(lambda e: e.tensor_scalar(out=kdec.t[:], in0=pst[:, 0:128], scalar1=cl("eGlm"), scalar2=None,
                                                op0=ALU.mult), reads=[bpst, bsc], writes=[kdec.b])
                    P(lambda e: e.tensor_copy(out=qnf.t[:], in_=qT_[:, sl]), reads=rdq, writes=[qnf.b])
                    relA((psG, bpsG))
                    relT((pst, bpst))
                    yield
                    ((psK, bpsK),) = yield from acquire(1, 0)
                    T(lambda e: e.matmul(psK[:, 0:128], lhsT=kT_[:, sl], rhs=kT_[:, sl], start=True, stop=True),
                      reads=rdk, writes=[bpsK])
                    T(lambda e: e.matmul(psK[:, 128:256], lhsT=kT_[:, sl], rhs=qT_[:, sl], start=True, stop=True),
                      reads=rdk + rdq, writes=[bpsK])
                    A(lambda e: e.activation(out=dec.t[:], in_=dec.t[:], func=AF.Exp, scale=-1.0), reads=[dec.b], writes=[dec.b])
                    A(lambda e: e.activation(out=decT.t[:], in_=decT.t[:], func=AF.Exp), reads=[decT.b], writes=[decT.b])
                    yield
                    Pc, PTc, XTc = I["P"][0], I["PT"][0], I["XT"][0]
                    V(lambda e: e.scalar_tensor_tensor(out=Pc.t[:], in0=psK[:, 0:128], scalar=cl("beta"), in1=dec.t[:],
                                                       op0=ALU.mult, op1=ALU.mult), reads=[bpsK, bsc, dec.b], writes=[Pc.b])
                    V(lambda e: e.tensor_tensor(out=qkT.t[:], in0=psK[:, 128:256], in1=decT.t[:], op=ALU.mult),
                      reads=[bpsK, decT.b], writes=[qkT.b])
                    relA((psK, bpsK))
                    yield
                    ((psX, bpsX),) = yield from acquire(1, 0)
                    T(lambda e: e.matmul(psX[:, 0:128], lhsT=Pc.t[:], rhs=ident_f[:], start=True, stop=True),
                      reads=[Pc.b, B_const], writes=[bpsX])
                    yield
                    V(lambda e: e.tensor_copy(out=PTc.t[:], in_=psX[:, 0:128]), reads=[bpsX], writes=[PTc.b])
                    V(lambda e: e.scalar_tensor_tensor(out=XTc.t[:], in0=psX[:, 0:128], scalar=-1.0, in1=ident_f[:],
                                                       op0=ALU.mult, op1=ALU.add), reads=[bpsX, B_const], writes=[XTc.b])
                    relA((psX, bpsX))
                    yield
                    pend = None
                    for l in range(1, 8):
                        Pp, PTp = I["P"][(l - 1) % 2], I["PT"][(l - 1) % 2]
                        Pn, PTn = I["P"][l % 2], I["PT"][l % 2]
                        got = yield from acquire((1 if l <= 6 else 0) + (1 if l >= 2 else 0), 0)
                        if l <= 6:
                            psS, bpsS = got.pop(0)
                            T(lambda e, psS=psS, Pp=Pp, PTp=PTp: e.matmul(psS[:, 0:128], lhsT=PTp.t[:], rhs=Pp.t[:], start=True, stop=True),
                              reads=[Pp.b, PTp.b], writes=[bpsS])
                            if l < 6:
                                T(lambda e, psS=psS, Pp=Pp, PTp=PTp: e.matmul(psS[:, 128:256], lhsT=Pp.t[:], rhs=PTp.t[:], start=True, stop=True),
                                  reads=[Pp.b, PTp.b], writes=[bpsS])
                        if l >= 2:
                            XTo, XTn = I["XT"][(l - 2) % 2], I["XT"][(l - 1) % 2]
                            psM, bpsM = got.pop(0)
                            T(lambda e, psM=psM, Pp=Pp, XTo=XTo: e.matmul(psM[:, 0:128], lhsT=Pp.t[:], rhs=XTo.t[:], start=True, stop=True),
                              reads=[Pp.b, XTo.b], writes=[bpsM])
                            pend = (psM, bpsM, XTo, XTn)
                        yield
                        if l <= 6:
                            A(lambda e, psS=psS, Pn=Pn: e.copy(out=Pn.t[:], in_=psS[:, 0:128]), reads=[bpsS], writes=[Pn.b])
                            if l < 6:
                                V(lambda e, psS=psS, PTn=PTn: e.tensor_copy(out=PTn.t[:], in_=psS[:, 128:256]), reads=[bpsS], writes=[PTn.b])
                            relA((psS, bpsS))
                        if pend is not None:
                            psM, bpsM, XTo, XTn = pend
                            V(lambda e, psM=psM, XTo=XTo, XTn=XTn: e.tensor_tensor(out=XTn.t[:], in0=XTo.t[:], in1=psM[:, 0:128], op=ALU.add),
                              reads=[bpsM, XTo.b], writes=[XTn.b])
                            relA((psM, bpsM))
                            pend = None
                        yield
                    XTf = I["XT"][0]
                    ((psL, bpsL),) = yield from acquire(1, 0)
                    T(lambda e: e.matmul(psL[:, 0:256], lhsT=XTf.t[:], rhs=rr_.t[:], start=True, stop=True),
                      reads=[XTf.b, rr_.b], writes=[bpsL])
                    yield
                    V(lambda e: e.tensor_copy(out=rr_.t[:, 0:128], in_=psL[:, 0:128]), reads=[bpsL], writes=[rr_.b])
                    A(lambda e: e.copy(out=rr_.t[:, 128:256], in_=psL[:, 128:256]), reads=[bpsL], writes=[rr_.b])
                    relA((psL, bpsL))
                    yield
                    ((psW, bpsW),) = yield from acquire(1, 0)
                    T(lambda e: e.matmul(psW[:, 0:128], lhsT=rr_.t[:, 128:256], rhs=ident_f[:], start=True, stop=True),
                      reads=[rr_.b, B_const], writes=[bpsW])
                    yield
                    A(lambda e: e.copy(out=wT.t[:], in_=psW[:, 0:128]), reads=[bpsW], writes=[wT.b])
                    relA((psW, bpsW))
                    yield

                posts = []

                def post(tt, qi):
                    Q = Q_[qi]
                    blk = tt // 4
                    o, osq, oss, on_, sz, yb = Q["o"], Q["osq"], Q["oss"], Q["on"], Q["sz"], Q["yb"]
                    ((psZ, bpsZ),) = yield from acquire(1, 0)
                    proj_tm(wz, bwz, 0, 128, tt, psZ, bpsZ)
                    yield
                    A(lambda e: e.activation(out=osq.t[:], in_=o.t[:], func=AF.Square, accum_out=oss.t[:]),
                      reads=[o.b], writes=[osq.b, oss.b])
                    A(lambda e: e.activation(out=sz.t[:], in_=psZ[:, 0:128], func=AF.Silu), reads=[bpsZ], writes=[sz.b])
                    relA((psZ, bpsZ))
                    yield
                    V(lambda e: e.tensor_scalar(out=oss.t[:], in0=oss.t[:], scalar1=1.0 / HD, scalar2=EPS, op0=ALU.mult, op1=ALU.add),
                      reads=[oss.b], writes=[oss.b])
                    yield
                    A(lambda e: e.activation(out=oss.t[:], in_=oss.t[:], func=AF.Sqrt), reads=[oss.b], writes=[oss.b])
                    yield
                    V(lambda e: e.reciprocal(out=oss.t[:], in_=oss.t[:]), reads=[oss.b], writes=[oss.b])
                    V(lambda e: e.scalar_tensor_tensor(out=on_.t[:], in0=o.t[:], scalar=oss.t[:, 0:1], in1=smalls[:, 16:144],
                                                       op0=ALU.mult, op1=ALU.mult), reads=[o.b, oss.b, B_const], writes=[on_.b])
                    V(lambda e: e.tensor_tensor(out=yb.t[:], in0=on_.t[:], in1=sz.t[:], op=ALU.mult),
                      reads=[on_.b, sz.b], writes=[yb.b])
                    yield
                    ((pst, bpst),) = yield from acquire(0, 1)
                    T(lambda e: e.transpose(out=pst[:, 0:128], in_=yb.t[:], identity=ident_b[:]), reads=[yb.b, B_const], writes=[bpst])
                    yield
                    ybt = ybT[blk % 2]
                    s_ = tt % 4
                    A(lambda e: e.copy(out=ybt.t[:, s_ * 128:(s_ + 1) * 128], in_=pst[:, 0:128]), reads=[bpst], writes=[ybt.b])
                    relT((pst, bpst))
                    if s_ == 3:
                        ks.dma("pool", lambda e: e.dma_start(
                            out=yT[sq_i, h * 128:(h + 1) * 128, blk * 512:(blk + 1) * 512], in_=ybt.t[:]),
                               reads=[ybt.b], writes=[B_yT])
                    yield

                def chain(tiles, obase):
                    for k_, tt in enumerate(tiles):
                        O = O_[obase + k_]
                        Q = Q_[tt % 2]
                        cl = lambda nm, tt=tt: tl[nm][:, tt, h:h + 1]
                        ((psR, bpsR),) = yield from acquire(1, 0)
                        T(lambda e, psR=psR, O=O: e.matmul(psR[:, 0:128], lhsT=O["wT"].t[:], rhs=Sst.t[:], start=True, stop=True),
                          reads=[O["wT"].b, Sst.b], writes=[bpsR])
                        T(lambda e, psR=psR, O=O: e.matmul(psR[:, 128:256], lhsT=O["qnf"].t[:], rhs=Sst.t[:], start=True, stop=True),
                          reads=[O["qnf"].b, Sst.b], writes=[bpsR])
                        yield
                        V(lambda e, psR=psR, O=O, Q=Q: e.tensor_tensor(out=Q["vnew"].t[:], in0=O["rr"].t[:, 0:128], in1=psR[:, 0:128],
                                                                      op=ALU.subtract), reads=[bpsR, O["rr"].b], writes=[Q["vnew"].b])
                        A(lambda e, psR=psR, Q=Q, cl=cl: e.activation(out=Q["qS"].t[:], in_=psR[:, 128:256], func=AF.Copy, scale=cl("eG")),
                          reads=[bpsR, bsc], writes=[Q["qS"].b])
                        relA((psR, bpsR))
                        yield
                        ((psO, bpsO),) = yield from acquire(1, 0)
                        T(lambda e, psO=psO, O=O, Q=Q: e.matmul(psO[:, 0:128], lhsT=O["qkT"].t[:], rhs=Q["vnew"].t[:], start=True, stop=True),
                          reads=[O["qkT"].b, Q["vnew"].b], writes=[bpsO])
                        T(lambda e, psO=psO, O=O, Q=Q: e.matmul(psO[:, 128:256], lhsT=O["kdec"].t[:], rhs=Q["vnew"].t[:], start=True, stop=True),
                          reads=[O["kdec"].b, Q["vnew"].b], writes=[bpsO])
                        yield
                        V(lambda e, psO=psO, cl=cl: e.scalar_tensor_tensor(out=Sst.t[:], in0=Sst.t[:], scalar=cl("gl"), in1=psO[:, 128:256],
                                                                          op0=ALU.mult, op1=ALU.add), reads=[bpsO, bsc, Sst.b], writes=[Sst.b])
                        V(lambda e, psO=psO, Q=Q: e.tensor_tensor(out=Q["o"].t[:], in0=Q["qS"].t[:], in1=psO[:, 0:128], op=ALU.add),
                          reads=[bpsO, Q["qS"].b], writes=[Q["o"].b])
                        relA((psO, bpsO))
                        posts.append(post(tt, tt % 2))
                        yield

                _stop = 0

                def _lim(g_):
                    n_ = 0
                    for _ in g_:
                        n_ += 1
                        if _stop and n_ >= _stop:
                            return
                        yield

                def run_round(gens):
                    gens = [_lim(g_) for g_ in gens] if _stop else list(gens)
                    while gens or posts:
                        for g_ in list(gens):
                            try:
                                next(g_)
                            except StopIteration:
                                gens.remove(g_)
                        for g_ in list(posts):
                            try:
                                next(g_)
                            except StopIteration:
                                posts.remove(g_)

                NG = NT // G
                for g in range(NG + 1):
                    gens = []
                    if g < NG:
                        gens += [part1(g * G + k_, k_, (g % 2) * G + k_) for k_ in range(G)]
                    if g >= 1 and not _stop:
                        gens.append(chain([(g - 1) * G + k_ for k_ in range(G)], ((g - 1) % 2) * G))
                    run_round(gens)
            ks.fence()

        def gdn_all(sq_i):
            with contextlib.ExitStack() as st:
                set_psum(st, 6, 2)
                cw = SB(st, "cw", [128, 12, 4], F32)
                dg = SB(st, "dg", [128, 48, 128], BF16)
                b_dg = Buf()
                ks.dma("sp", lambda e: e.dma_start(out=cw[:], in_=convw[:, :, :]), writes=[b_dg])
                for c in range(12):
                    for j in range(4):
                        P(lambda e, c=c, j=j: e.tensor_scalar(out=dg[:, c * 4 + j, :], in0=ident_f[:], scalar1=cw[:, c, j:j + 1],
                                                              scalar2=None, op0=ALU.mult), reads=[b_dg, B_const], writes=[b_dg])
                tl = gdn_prep(sq_i, st)
                for h in range(4):
                    gdn_head(sq_i, h, tl, dg, b_dg)

        def phase_C(sq_i, layer):
            last = (layer == 1)
            xsrc = x_in if layer == 0 else xres
            with contextlib.ExitStack() as st:
                set_psum(st, 6, 2)
                wo, bwo = None, None
                stg = SB(st, "c_stg", [128, 8, 512], F32)
                b_stg = Buf()
                wo = SB(st, "c_wo", [128, 8, D], BF16)
                wg = SB(st, "c_wg", [128, 8, D], BF16)
                wp = SB(st, "c_wp", [128, 2, D], BF16)
                bwo, bwg, bwp = Buf(), Buf(), Buf()
                for (dst, bdst, src) in ((wo, bwo, w_out[layer]), (wg, bwg, w_gate[layer])):
                    for hh in range(2):
                        ks.dma("sp", lambda e, src=src, hh=hh: e.dma_start(
                            out=stg[:], in_=src[:, hh * 512:(hh + 1) * 512].rearrange("(c p) n -> p c n", p=128)),
                               writes=[b_stg])
                        P(lambda e, dst=dst, hh=hh: e.tensor_copy(out=dst[:, :, hh * 512:(hh + 1) * 512], in_=stg[:]),
                          reads=[b_stg], writes=[bdst])
                for hh in range(2):
                    ks.dma("sp", lambda e, hh=hh: e.dma_start(
                        out=stg[:, 0:2, :], in_=w_pp[layer, :, hh * 512:(hh + 1) * 512].rearrange("(c p) n -> p c n", p=128)),
                           writes=[b_stg])
                    P(lambda e, hh=hh: e.tensor_copy(out=wp[:, :, hh * 512:(hh + 1) * 512], in_=stg[:, 0:2, :]),
                      reads=[b_stg], writes=[bwp])
                gP = SB(st, "c_gP", [128, D], F32)
                gN = SB(st, "c_gN", [128, D], F32)
                bgP, bgN = Buf(), Buf()
                ks.dma("sp", lambda e: e.dma_start(out=gP[:], in_=gvec[2 + layer, :, :]), writes=[bgP])
                ks.dma("sp", lambda e: e.dma_start(out=gN[:], in_=gvec[4 if last else 1, :, :]), writes=[bgN])
                NP_ = 2
                xt = [SB(st, "c_x%d" % i, [128, D], F32) for i in range(NP_)]
                yl = [SB(st, "c_yl%d" % i, [128, 8, 128], BF16) for i in range(NP_)]
                ptf = [SB(st, "c_ptf%d" % i, [128, 2, 128], F32) for i in range(NP_)]
                ptb = [SB(st, "c_ptb%d" % i, [128, 2, 128], BF16) for i in range(NP_)]
                x1 = [SB(st, "c_x1%d" % i, [128, D], F32) for i in range(NP_)]
                hT = [SB(st, "c_hT%d" % i, [128, 8, 128], BF16) for i in range(NP_)]
                gt = [SB(st, "c_gt%d" % i, [128, D], F32) for i in range(NP_)]
                x2 = [SB(st, "c_x2%d" % i, [128, D], F32) for i in range(NP_)]
                ot = [SB(st, "c_ot%d" % i, [128, D], F32) for i in range(NP_)]
                scs = [dict(sq=SB(st, "c_sq%d" % i, [128, D], F32), ss=SB(st, "c_ss%d" % i, [128, 1], F32),
                            rstd=SB(st, "c_rs%d" % i, [128, 1], F32), hn=SB(st, "c_hn%d" % i, [128, D], BF16),
                            buf=Buf()) for i in range(NP_)]
                bx = [Buf() for _ in range(NP_)]
                byl = [Buf() for _ in range(NP_)]
                bpt = [Buf() for _ in range(NP_)]
                bx1 = [Buf() for _ in range(NP_)]
                bhT = [Buf() for _ in range(NP_)]
                bgt = [Buf() for _ in range(NP_)]
                bx2 = [Buf() for _ in range(NP_)]
                bot = [Buf() for _ in range(NP_)]
                for tt in range(NT):
                    i = tt % NP_
                    tsl = slice(tt * 128, (tt + 1) * 128)
                    ks.dma("sp", lambda e, i=i, tsl=tsl: e.dma_start(out=xt[i][:], in_=xsrc[sq_i, tsl, :]),
                           reads=[B_xres], writes=[bx[i]])
                    ks.dma("sp", lambda e, i=i, tsl=tsl: e.dma_start(
                        out=yl[i][:], in_=yT[sq_i, :, tsl].rearrange("(c p) n -> p c n", p=128)), reads=[B_yT], writes=[byl[i]])
                    ks.dma("sp", lambda e, i=i, tsl=tsl: e.dma_start(
                        out=ptf[i][:], in_=pT_in[layer, sq_i, :, tsl].rearrange("(c p) n -> p c n", p=128)), writes=[bpt[i]])
                    P(lambda e, i=i: e.tensor_copy(out=ptb[i][:], in_=ptf[i][:]), reads=[bpt[i]], writes=[bpt[i]])
                    for hh in range(2):
                        ps, bps = nextA()
                        for c in range(8):
                            T(lambda e, ps=ps, c=c, hh=hh, i=i: e.matmul(ps[:, :], lhsT=yl[i][:, c, :],
                                                                         rhs=wo[:, c, hh * 512:(hh + 1) * 512],
                                                                         start=(c == 0), stop=(c == 7)),
                              reads=[byl[i], bwo], writes=[bps])
                        V(lambda e, ps=ps, hh=hh, i=i: e.tensor_tensor(out=x1[i][:, hh * 512:(hh + 1) * 512], in0=ps[:, :],
                                                                       in1=xt[i][:, hh * 512:(hh + 1) * 512], op=ALU.add),
                          reads=[bps, bx[i]], writes=[bx1[i]])

                    def dstg(pst, bpst, i=i):
                        A(lambda e: e.copy(out=hT[i][:], in_=pst[:, :].rearrange("p (c n) -> p c n", c=8)),
                          reads=[bpst], writes=[bhT[i]])
                    rms_to_T(x1[i], bx1[i], gP, bgP, scs[i], dstg, None)
                    for hh in range(2):
                        ps, bps = nextA()
                        for c in range(8):
                            T(lambda e, ps=ps, c=c, hh=hh, i=i: e.matmul(ps[:, :], lhsT=hT[i][:, c, :],
                                                                         rhs=wg[:, c, hh * 512:(hh + 1) * 512],
                                                                         start=(c == 0), stop=(c == 7)),
                              reads=[bhT[i], bwg], writes=[bps])
                        A(lambda e, ps=ps, hh=hh, i=i: e.activation(out=gt[i][:, hh * 512:(hh + 1) * 512], in_=ps[:, :],
                                                                    func=AF.Sigmoid), reads=[bps], writes=[bgt[i]])
                        ps, bps = nextA()
                        for c in range(2):
                            T(lambda e, ps=ps, c=c, hh=hh, i=i: e.matmul(ps[:, :], lhsT=ptb[i][:, c, :],
                                                                         rhs=wp[:, c, hh * 512:(hh + 1) * 512],
                                                                         start=(c == 0), stop=(c == 1)),
                              reads=[bpt[i], bwp], writes=[bps])
                        V(lambda e, ps=ps, hh=hh, i=i: e.tensor_tensor(out=gt[i][:, hh * 512:(hh + 1) * 512],
                                                                       in0=gt[i][:, hh * 512:(hh + 1) * 512], in1=ps[:, :],
                                                                       op=ALU.mult), reads=[bps, bgt[i]], writes=[bgt[i]])
                    P(lambda e, i=i: e.tensor_tensor(out=x2[i][:], in0=gt[i][:], in1=x1[i][:], op=ALU.add),
                      reads=[bgt[i], bx1[i]], writes=[bx2[i]])
                    if not last:
                        ks.dma("pool", lambda e, i=i, tsl=tsl: e.dma_start(out=xres[sq_i, tsl, :], in_=x2[i][:]),
                               reads=[bx2[i]], writes=[B_xres])

                        def dstn(pst, bpst, tt=tt):
                            A(lambda e: e.copy(out=hnT[:, :, tt * 128:(tt + 1) * 128],
                                               in_=pst[:, :].rearrange("p (c n) -> p c n", c=8)),
                              reads=[bpst], writes=[B_hnT[tt]])
                        rms_to_T(x2[i], bx2[i], gN, bgN, scs[i], dstn, None)
                    else:
                        sc = scs[i]
                        bsc = sc["buf"]
                        A(lambda e, i=i, sc=sc: e.activation(out=sc["sq"][:], in_=x2[i][:], func=AF.Square, accum_out=sc["ss"][:]),
                          reads=[bx2[i]], writes=[bsc])
                        V(lambda e, sc=sc: e.tensor_scalar(out=sc["rstd"][:], in0=sc["ss"][:], scalar1=1.0 / D, scalar2=EPS,
                                                           op0=ALU.mult, op1=ALU.add), reads=[bsc], writes=[bsc])
                        A(lambda e, sc=sc: e.activation(out=sc["rstd"][:], in_=sc["rstd"][:], func=AF.Sqrt), reads=[bsc], writes=[bsc])
                        V(lambda e, sc=sc: e.reciprocal(out=sc["rstd"][:], in_=sc["rstd"][:]), reads=[bsc], writes=[bsc])
                        V(lambda e, i=i, sc=sc: e.scalar_tensor_tensor(out=ot[i][:], in0=x2[i][:], scalar=sc["rstd"][:, 0:1],
                                                                       in1=gN[:], op0=ALU.mult, op1=ALU.mult),
                          reads=[bx2[i], bgN, bsc], writes=[bot[i]])
                        ks.dma("pool", lambda e, i=i, tsl=tsl: e.dma_start(out=out[sq_i, tsl, :], in_=ot[i][:]),
                               reads=[bot[i]], writes=[B_out])
            ks.fence()

        B_out = Buf()

        def zero_yT(sq_i, r0, r1):
            with contextlib.ExitStack() as st:
                z = SB(st, "zz", [128, S], BF16)
                bz = Buf()
                P(lambda e: e.memset(z[:], 0.0), writes=[bz])
                for r in range(r0, r1, 128):
                    ks.dma("pool", lambda e, r=r: e.dma_start(out=yT[sq_i, r:r + 128, :], in_=z[:]), reads=[bz], writes=[B_yT])
            ks.fence()

        for sq_i in range(NSEQ):
            phase_A(sq_i)
            if do_gdn:
                gdn_all(sq_i)
                ks.fence()
            else:
                zero_yT(sq_i, 0, 512)
            if do_moba:
                for h in range(4):
                    attn_head(sq_i, "moba", h, w_ab,
                              dict(q=B_Q + h * 128, k=B_K + h * 128, v=B_V + h * 128, z=A_Z + 512 + h * 128), 512 + h * 128)
            else:
                zero_yT(sq_i, 512, 1024)
            phase_C(sq_i, 0)
            if do_fox:
                for h in range(8):
                    attn_head(sq_i, "fox", h, w_c,
                              dict(q=C_Q + h * 128, k=C_K + h * 128, v=C_V + h * 128, z=C_Z + h * 128, f=C_F + h), h * 128)
            else:
                zero_yT(sq_i, 0, 1024)
            phase_C(sq_i, 1)
        ks.emit()
    return nc


def prep_inputs(inp, nseq, ncores):
    f = lambda a: np.ascontiguousarray(np.asarray(a, dtype=np.float32))
    x = f(inp["x"])
    p = f(inp["p"])
    gv = np.stack([inp["norm_g"][0], inp["norm_g"][1], inp["ple_norm_g"][0], inp["ple_norm_g"][1], inp["final_g"]], 0)
    gv = f(np.broadcast_to(np.asarray(gv, np.float32)[:, None, :], (5, 128, D)))
    cw = np.asarray(inp["conv_w"], np.float32)[0]
    convw = f(cw.T.reshape(12, 128, 4).transpose(1, 0, 2))
    sv = np.concatenate([np.asarray(inp["a_log"], np.float32)[0], np.asarray(inp["dt_bias"], np.float32)[0],
                         np.asarray(inp["forget_b"], np.float32)[0], np.asarray(inp["gdn_norm_g"], np.float32)[0]])
    smallv = f(np.broadcast_to(sv[None, :], (128, 144)))
    shared = dict(w_in_ab=f(inp["w_in_ab"][0]), w_in_c=f(inp["w_in_c"][0]), w_out_ab=f(inp["w_out_ab"][0]),
                  w_out_c=f(inp["w_out_c"][0]), w_ple_gate=f(inp["w_ple_gate"]), w_ple_proj=f(inp["w_ple_proj"]),
                  gvec=gv, convw=convw, smallv=smallv,
                  wfc=f(np.asarray(inp["w_in_c"], np.float32)[0][:, C_F:C_F + 8].T.reshape(8, 8, 128).transpose(0, 2, 1)))
    maps = []
    for c in range(ncores):
        sl = slice(c * nseq, (c + 1) * nseq)
        m = dict(shared)
        m["x"] = f(x[sl])
        m["pT"] = f(p[:, sl].transpose(0, 1, 3, 2))
        maps.append(m)
    return maps


def kernel(**inputs):
    x = np.asarray(inputs["x"])
    Bsz, S, _ = x.shape
    ncores = 8
    nseq = Bsz // ncores
    nc = build(S, nseq)
    maps = prep_inputs(inputs, nseq, ncores)
    res = run_bass_kernel_spmd(nc, maps, core_ids=list(range(ncores)))
    return np.concatenate([np.asarray(r["out"], dtype=np.float32) for r in res.results], axis=0)
```

```python
import contextlib
import numpy as np
import concourse.bass as bass
import concourse.mybir as mybir
from concourse.bass_utils import run_bass_kernel_spmd

F32 = mybir.dt.float32
BF16 = mybir.dt.bfloat16
AF = mybir.ActivationFunctionType
ALU = mybir.AluOpType
AX = mybir.AxisListType

D = 1024
HD = 128
EPS = 1e-6
BIG = 1.0e30
QSCALE = HD ** -0.5


class Buf:
    __slots__ = ("w", "r", "excl")

    def __init__(self, excl=False):
        self.w = None
        self.r = []
        self.excl = excl


class KS:
    ENGS = ("pe", "dve", "act", "pool", "sp")

    def __init__(self, nc, n_dma_chan=8):
        self.nc = nc
        self.streams = {e: [] for e in self.ENGS}
        self.cnt = {e: 0 for e in self.ENGS}
        self.seen = {e: {} for e in self.ENGS}
        self.nchan = n_dma_chan
        self.chan_cnt = {}
        self.chan_rr = {e: 0 for e in self.ENGS}
        self.sems = {}

    def _deps(self, eng, reads, writes):
        need = {}

        def add(tok):
            if tok is None:
                return
            k, v = tok
            if k == eng and eng == "pe":
                return
            if need.get(k, 0) < v:
                need[k] = v
        for b in reads:
            add(b.w)
            if b.excl:
                for t in b.r:
                    if t[0] != eng:
                        add(t)
        for b in writes:
            add(b.w)
            for t in b.r:
                add(t)
        out = []
        seen = self.seen[eng]
        for k, v in need.items():
            if seen.get(k, 0) >= v:
                continue
            seen[k] = v
            out.append((k, v))
        return out

    def _commit(self, tok, reads, writes):
        for b in reads:
            if len(b.r) > 24:
                m = {}
                for k, v in b.r:
                    if m.get(k, 0) < v:
                        m[k] = v
                b.r = list(m.items())
            b.r.append(tok)
        for b in writes:
            b.w = tok
            b.r = []

    def op(self, eng, fn, reads=(), writes=()):
        waits = self._deps(eng, reads, writes)
        self.cnt[eng] += 1
        tok = (eng, self.cnt[eng])
        self.streams[eng].append((waits, fn, (eng, 1)))
        self._commit(tok, reads, writes)
        return tok

    def dma(self, eng, fn, reads=(), writes=()):
        c = self.chan_rr[eng]
        self.chan_rr[eng] = (c + 1) % self.nchan
        key = ("dma", eng, c)
        prev = self.chan_cnt.get(key, 0)
        waits = self._deps(eng, reads, writes)
        if prev and self.seen[eng].get(key, 0) < prev:
            self.seen[eng][key] = prev
            waits.append((key, prev))
        self.chan_cnt[key] = prev + 16
        tok = (key, prev + 16)
        self.streams[eng].append((waits, fn, (key, 16)))
        self._commit(tok, reads, writes)
        return tok

    def fence(self):
        cur = dict(self.cnt)
        cur.update(self.chan_cnt)
        for e in self.ENGS:
            waits = []
            for k, v in cur.items():
                if v <= 0 or (k == e):
                    continue
                if self.seen[e].get(k, 0) < v:
                    self.seen[e][k] = v
                    waits.append((k, v))
            if waits:
                self.streams[e].append((waits, None, None))

    def emit(self):
        nc = self.nc
        keys = list(self.ENGS) + sorted(self.chan_cnt.keys(), key=str)
        with contextlib.ExitStack() as st:
            for k in keys:
                nm = "s_" + ("_".join(map(str, k)) if isinstance(k, tuple) else k)
                self.sems[k] = st.enter_context(nc.semaphore(nm))
            block = st.enter_context(nc.Block())
            finals = {k: v for k, v in self.chan_cnt.items()}
            finals.update({e: v for e, v in self.cnt.items() if v > 0})

            def run(eng, h):
                for waits, fn, inc in self.streams[eng]:
                    for k, v in waits:
                        h.wait_ge(self.sems[k], v)
                    if fn is not None:
                        fn(h).then_inc(self.sems[inc[0]], inc[1])
                if eng == "sp":
                    for k, v in finals.items():
                        if k != "sp":
                            h.wait_ge(self.sems[k], v)

            @block.tensor
            def _(h):
                run("pe", h)

            @block.vector
            def _(h):
                run("dve", h)

            @block.scalar
            def _(h):
                run("act", h)

            @block.gpsimd
            def _(h):
                run("pool", h)

            @block.sync
            def _(h):
                run("sp", h)


A_Q, A_K, A_V = 0, 512, 1024
A_A, A_B = 1536, 1540
B_Q, B_K, B_V = 1544, 2056, 2568
A_Z = 3080
C_Q, C_K, C_V, C_F, C_Z = 0, 1024, 2048, 3072, 3080
IN_W = 4104


def build(S, NSEQ, do_gdn=True, do_moba=True, do_fox=True):
    NT = S // 128
    NBQ = S // 512
    NB = S // 256
    nc = bass.Bass("TRN2", target_bir_lowering=False)
    dt = nc.dram_tensor
    x_in = dt("x", [NSEQ, S, D], F32, kind="ExternalInput").ap()
    pT_in = dt("pT", [2, NSEQ, 256, S], F32, kind="ExternalInput").ap()
    w_ab = dt("w_in_ab", [D, IN_W], F32, kind="ExternalInput").ap()
    w_c = dt("w_in_c", [D, IN_W], F32, kind="ExternalInput").ap()
    w_out = [dt("w_out_ab", [D, D], F32, kind="ExternalInput").ap(),
             dt("w_out_c", [D, D], F32, kind="ExternalInput").ap()]
    w_gate = dt("w_ple_gate", [2, D, D], F32, kind="ExternalInput").ap()
    w_pp = dt("w_ple_proj", [2, 256, D], F32, kind="ExternalInput").ap()
    gvec = dt("gvec", [5, 128, D], F32, kind="ExternalInput").ap()
    convw = dt("convw", [128, 12, 4], F32, kind="ExternalInput").ap()
    smallv = dt("smallv", [128, 144], F32, kind="ExternalInput").ap()
    wfc = dt("wfc", [8, 128, 8], F32, kind="ExternalInput").ap()
    out = dt("out", [NSEQ, S, D], F32, kind="ExternalOutput").ap()
    xres = dt("xres", [NSEQ, S, D], F32, kind="Internal").ap()
    yT = dt("yT", [NSEQ, D, S], BF16, kind="Internal").ap()

    ks = KS(nc)
    slopes = [2.0 ** (-8.0 * (h + 1) / 4) for h in range(4)]

    with contextlib.ExitStack() as top:
        uid = [0]

        def SB(st, name, shape, dtype):
            uid[0] += 1
            return st.enter_context(nc.sbuf_tensor("%s_%d" % (name, uid[0]), shape, dtype))

        def PS(st, name, shape, dtype):
            uid[0] += 1
            return st.enter_context(nc.psum_tensor("%s_%d" % (name, uid[0]), shape, dtype))

        hnT = SB(top, "hnT", [128, 8, S], BF16)
        B_hnT = [Buf() for _ in range(NT)]
        ident_f = SB(top, "ident_f", [128, 128], F32)
        ident_b = SB(top, "ident_b", [128, 128], BF16)
        ones_f = SB(top, "ones_f", [128, 128], F32)
        ones_b = SB(top, "ones_b", [128, 128], BF16)
        maskT_b = SB(top, "maskT_b", [128, 128], BF16)
        maskA = SB(top, "maskA", [128, 128], F32)
        maskQ = SB(top, "maskQ", [128, 128], F32)
        U_f = SB(top, "U_f", [128, 128], F32)
        e0col = SB(top, "e0col", [128, 2], F32)
        iq = SB(top, "iq", [128, 512], F32)
        ikcol = SB(top, "ikcol", [128, 1], F32)
        tmpf = SB(top, "tmpf", [128, 128], F32)
        smalls = SB(top, "smalls", [128, 144], F32)
        zero_c = SB(top, "zero_c", [128, 1], F32)
        B_const = Buf()

        PSM = {"A": [], "BA": [], "T": [], "BT": []}
        rr = {"A": 0, "T": 0}

        def set_psum(st, nA, nT):
            PSM["A"] = [PS(st, "psA", [128, 512], F32) for _ in range(nA)]
            PSM["BA"] = [Buf(True) for _ in range(nA)]
            PSM["T"] = [PS(st, "psT", [128, 1024], BF16) for _ in range(nT)]
            PSM["BT"] = [Buf(True) for _ in range(nT)]
            rr["A"] = 0
            rr["T"] = 0

        def nextA():
            i = rr["A"]
            rr["A"] = (i + 1) % len(PSM["A"])
            return PSM["A"][i], PSM["BA"][i]

        def nextT():
            i = rr["T"]
            rr["T"] = (i + 1) % len(PSM["T"])
            return PSM["T"][i], PSM["BT"][i]

        P = lambda fn, **kw: ks.op("pool", fn, **kw)
        V = lambda fn, **kw: ks.op("dve", fn, **kw)
        A = lambda fn, **kw: ks.op("act", fn, **kw)
        T = lambda fn, **kw: ks.op("pe", fn, **kw)

        C = [B_const]
        P(lambda e: e.memset(ident_f[:], 0.0), writes=C)
        P(lambda e: e.affine_select(out=ident_f[:], in_=ident_f[:], pattern=[[-1, 128]], compare_op=ALU.not_equal,
                                    fill=1.0, base=0, channel_multiplier=1), reads=C, writes=C)
        P(lambda e: e.tensor_copy(out=ident_b[:], in_=ident_f[:]), reads=C, writes=C)
        P(lambda e: e.memset(ones_f[:], 1.0), writes=C)
        P(lambda e: e.memset(ones_b[:], 1.0), writes=C)
        P(lambda e: e.memset(zero_c[:], 0.0), writes=C)
        P(lambda e: e.memset(maskQ[:], 0.0), writes=C)
        P(lambda e: e.affine_select(out=maskQ[:], in_=maskQ[:], pattern=[[1, 128]], compare_op=ALU.is_ge,
                                    fill=BIG, base=0, channel_multiplier=-1), reads=C, writes=C)
        P(lambda e: e.tensor_scalar(out=maskT_b[:], in0=maskQ[:], scalar1=-1.0, scalar2=None, op0=ALU.mult),
          reads=C, writes=C)
        P(lambda e: e.memset(maskA[:], 0.0), writes=C)
        P(lambda e: e.affine_select(out=maskA[:], in_=maskA[:], pattern=[[-1, 128]], compare_op=ALU.is_gt,
                                    fill=BIG, base=0, channel_multiplier=1), reads=C, writes=C)
        P(lambda e: e.memset(U_f[:], 1.0), writes=C)
        P(lambda e: e.affine_select(out=U_f[:], in_=U_f[:], pattern=[[1, 128]], compare_op=ALU.is_ge,
                                    fill=0.0, base=0, channel_multiplier=-1), reads=C, writes=C)
        P(lambda e: e.memset(e0col[:], 0.0), writes=C)
        P(lambda e: e.affine_select(out=e0col[:], in_=e0col[:], pattern=[[0, 2]], compare_op=ALU.not_equal,
                                    fill=1.0, base=0, channel_multiplier=1), reads=C, writes=C)
        P(lambda e: e.iota(iq[:], pattern=[[1, 512]], base=0, channel_multiplier=0,
                           allow_small_or_imprecise_dtypes=True), writes=C)
        P(lambda e: e.iota(ikcol[:], pattern=[[0, 1]], base=0, channel_multiplier=1,
                           allow_small_or_imprecise_dtypes=True), writes=C)
        ks.dma("sp", lambda e: e.dma_start(out=smalls[:], in_=smallv[:, :]), writes=C)

        def cast_on(ceng, out_ap, in_ap, reads, writes):
            if ceng == "act":
                A(lambda e: e.copy(out=out_ap, in_=in_ap), reads=reads, writes=writes)
            elif ceng == "dve":
                V(lambda e: e.tensor_copy(out=out_ap, in_=in_ap), reads=reads, writes=writes)
            else:
                P(lambda e: e.tensor_copy(out=out_ap, in_=in_ap), reads=reads, writes=writes)

        def load_w(st, name, src_ap, ncols, eng="sp", kchunks=8, ceng="pool"):
            stg = SB(st, name + "_f", [128, kchunks, ncols], F32)
            wb = SB(st, name + "_b", [128, kchunks, ncols], BF16)
            bs, bw = Buf(), Buf()
            ks.dma(eng, lambda e: e.dma_start(out=stg[:], in_=src_ap.rearrange("(c p) n -> p c n", p=128)), writes=[bs])
            cast_on(ceng, wb[:], stg[:], [bs], [bw])
            return wb, bw

        def rms_to_T(xt, bx, grep, bg, sc, dstT_fn, dst_bufs, extra_reads=()):
            sq, ss, rstd, hn = sc["sq"], sc["ss"], sc["rstd"], sc["hn"]
            bsc = sc["buf"]
            A(lambda e: e.activation(out=sq[:], in_=xt[:], func=AF.Square, accum_out=ss[:]),
              reads=[bx], writes=[bsc])
            V(lambda e: e.tensor_scalar(out=rstd[:], in0=ss[:], scalar1=1.0 / D, scalar2=EPS, op0=ALU.mult, op1=ALU.add),
              reads=[bsc], writes=[bsc])
            A(lambda e: e.activation(out=rstd[:], in_=rstd[:], func=AF.Sqrt), reads=[bsc], writes=[bsc])
            V(lambda e: e.reciprocal(out=rstd[:], in_=rstd[:]), reads=[bsc], writes=[bsc])
            V(lambda e: e.scalar_tensor_tensor(out=hn[:], in0=xt[:], scalar=rstd[:, 0:1], in1=grep[:],
                                               op0=ALU.mult, op1=ALU.mult), reads=[bx, bg, bsc], writes=[bsc])
            pst, bpst = nextT()
            for c in range(8):
                T(lambda e, c=c: e.transpose(out=pst[:, c * 128:(c + 1) * 128], in_=hn[:, c * 128:(c + 1) * 128],
                                             identity=ident_b[:]), reads=[bsc, B_const], writes=[bpst])
            dstT_fn(pst, bpst)

        def proj_fm(wb, bw, col0, blk, ps, bps, ncols=128):
            rd = [bw] + B_hnT[blk * 4:(blk + 1) * 4]
            for c in range(8):
                T(lambda e, c=c: e.matmul(ps[0:ncols, :], lhsT=wb[:, c, col0:col0 + ncols],
                                          rhs=hnT[:, c, blk * 512:(blk + 1) * 512], start=(c == 0), stop=(c == 7)),
                  reads=rd, writes=[bps])

        def proj_tm(wb, bw, col0, ncols, tt, ps, bps, pcol0=0):
            rd = [bw, B_hnT[tt]]
            for c in range(8):
                T(lambda e, c=c: e.matmul(ps[:, pcol0:pcol0 + ncols], lhsT=hnT[:, c, tt * 128:(tt + 1) * 128],
                                          rhs=wb[:, c, col0:col0 + ncols], start=(c == 0), stop=(c == 7)),
                  reads=rd, writes=[bps])

        cw = SB(top, "cw", [128, 12, 4], F32)
        dg = SB(top, "dg", [128, 48, 128], BF16)
        b_dg = Buf()
        ks.dma("sp", lambda e: e.dma_start(out=cw[:], in_=convw[:, :, :]), writes=[b_dg])
        for c in range(12):
            for j in range(4):
                cast_eng = (P, V)[(c * 4 + j) % 2]
                cast_eng(lambda e, c=c, j=j: e.tensor_scalar(out=dg[:, c * 4 + j, :], in0=ident_f[:], scalar1=cw[:, c, j:j + 1],
                                                             scalar2=None, op0=ALU.mult), reads=[b_dg, B_const], writes=[b_dg])

        def phase_A(sq_i):
            with contextlib.ExitStack() as st:
                set_psum(st, 0, 2)
                grep = SB(st, "gA", [128, D], F32)
                bg = Buf()
                ks.dma("sp", lambda e: e.dma_start(out=grep[:], in_=gvec[0, :, :]), writes=[bg])
                xts = [SB(st, "xtA%d" % i, [128, D], F32) for i in range(2)]
                bxs = [Buf(), Buf()]
                scs = [dict(sq=SB(st, "sqA%d" % i, [128, D], F32), ss=SB(st, "ssA%d" % i, [128, 1], F32),
                            rstd=SB(st, "rsA%d" % i, [128, 1], F32), hn=SB(st, "hnA%d" % i, [128, D], BF16),
                            buf=Buf()) for i in range(2)]
                for tt in range(NT):
                    xt, bx, sc = xts[tt % 2], bxs[tt % 2], scs[tt % 2]
                    ks.dma("sp", lambda e, tt=tt, xt=xt: e.dma_start(out=xt[:], in_=x_in[sq_i, tt * 128:(tt + 1) * 128, :]),
                           writes=[bx])

                    def dst(pst, bpst, tt=tt):
                        A(lambda e: e.copy(out=hnT[:, :, tt * 128:(tt + 1) * 128],
                                           in_=pst[:, :].rearrange("p (c n) -> p c n", c=8)),
                          reads=[bpst], writes=[B_hnT[tt]])
                    rms_to_T(xt, bx, grep, bg, sc, dst, None)
            ks.fence()

        def attn_head(sq_i, mode, h, wsrc, cols, yrow0):
            with contextlib.ExitStack() as st:
                set_psum(st, 3, 1)
                pso = [PS(st, "pso", [128, 512], F32) for _ in range(4)]
                bpso = [Buf(True) for _ in range(4)]
                wq, bwq = load_w(st, "wq", wsrc[:, cols["q"]:cols["q"] + 128], 128, ceng="dve")
                wk, bwk = load_w(st, "wk", wsrc[:, cols["k"]:cols["k"] + 128], 128, eng="pool", ceng="act")
                wv, bwv = load_w(st, "wv", wsrc[:, cols["v"]:cols["v"] + 128], 128, ceng="pool")
                wz, bwz = load_w(st, "wz", wsrc[:, cols["z"]:cols["z"] + 128], 128, eng="pool", ceng="dve")
                qT = SB(st, "qT", [128, S], BF16)
                kT = SB(st, "kT", [128, S], BF16)
                szT = SB(st, "szT", [128, S], BF16)
                vtm = SB(st, "vtm", [128, NT, 130], BF16)
                b_q = [Buf() for _ in range(NBQ)]
                b_k = [Buf() for _ in range(NBQ)]
                b_z = [Buf() for _ in range(NBQ)]
                b_v = [Buf() for _ in range(NT)]
                b_vones = Buf()
                P(lambda e: e.memset(vtm[:, :, 128:130], 1.0), writes=[b_vones])
                for blk in range(NBQ):
                    ps, bps = nextA()
                    proj_fm(wq, bwq, 0, blk, ps, bps)
                    A(lambda e, ps=ps, blk=blk: e.activation(out=qT[:, blk * 512:(blk + 1) * 512], in_=ps[:, :],
                                                             func=AF.Copy, scale=QSCALE), reads=[bps], writes=[b_q[blk]])
                    ps, bps = nextA()
                    proj_fm(wk, bwk, 0, blk, ps, bps)
                    V(lambda e, ps=ps, blk=blk: e.tensor_copy(out=kT[:, blk * 512:(blk + 1) * 512], in_=ps[:, :]),
                      reads=[bps], writes=[b_k[blk]])
                    ps, bps = nextA()
                    proj_fm(wz, bwz, 0, blk, ps, bps)
                    A(lambda e, ps=ps, blk=blk: e.activation(out=szT[:, blk * 512:(blk + 1) * 512], in_=ps[:, :],
                                                             func=AF.Silu), reads=[bps], writes=[b_z[blk]])
                    ps, bps = nextA()
                    for s in range(4):
                        proj_tm(wv, bwv, 0, 128, blk * 4 + s, ps, bps, pcol0=s * 128)
                    V(lambda e, ps=ps, blk=blk: e.tensor_copy(
                        out=vtm[:, blk * 4:(blk + 1) * 4, 0:128], in_=ps[:, :].rearrange("p (s n) -> p s n", s=4)),
                      reads=[bps], writes=b_v[blk * 4:(blk + 1) * 4])

                if mode == "fox":
                    wf_s = SB(st, "wf_s", [128, 8, 1], F32)
                    wf_b = SB(st, "wf_b", [128, 8, 128], BF16)
                    bwf = Buf()
                    ks.dma("sp", lambda e: e.dma_start(out=wf_s[:, :, 0], in_=wfc[h, :, :]), writes=[bwf])
                    P(lambda e: e.tensor_copy(out=wf_b[:], in_=wf_s[:].to_broadcast([128, 8, 128])),
                      reads=[bwf], writes=[bwf])
                    crep = SB(st, "crep", [128, S], F32)
                    ltmp = SB(st, "ltmp", [128, 512], F32)
                    negb = SB(st, "negb", [128, 1], F32)
                    b_c = [Buf() for _ in range(NBQ)]
                    b_l = Buf()
                    V(lambda e: e.tensor_scalar(out=negb[:], in0=smalls[:, 8 + h:9 + h], scalar1=-1.0, scalar2=None,
                                                op0=ALU.mult), reads=[B_const], writes=[b_l])
                    for blk in range(NBQ):
                        ps, bps = nextA()
                        proj_fm(wf_b, bwf, 0, blk, ps, bps)
                        A(lambda e, ps=ps: e.activation(out=ltmp[:], in_=ps[:, :], func=AF.Exp, bias=negb[:, 0:1], scale=-1.0),
                          reads=[bps, b_l], writes=[b_l])
                        A(lambda e: e.activation(out=ltmp[:], in_=ltmp[:], func=AF.Ln, bias=1.0, scale=1.0),
                          reads=[b_l], writes=[b_l])
                        init = zero_c[:, 0:1] if blk == 0 else crep[:, blk * 512 - 1:blk * 512]
                        V(lambda e, blk=blk, init=init: e.tensor_tensor_scan(
                            out=crep[:, blk * 512:(blk + 1) * 512], data0=ones_f[:, 0:1].to_broadcast([128, 512]),
                            data1=ltmp[:], initial=init, op0=ALU.mult, op1=ALU.add),
                          reads=[b_l, B_const] + ([b_c[blk - 1]] if blk else []), writes=[b_c[blk]])
                    ckcol = SB(st, "ckcol", [128, NT], F32)
                    b_ck = Buf()
                    ps, bps = nextA()
                    for j in range(NT):
                        T(lambda e, j=j, ps=ps: e.matmul(ps[:, 2 * j:2 * j + 2], lhsT=crep[:, j * 128:(j + 1) * 128],
                                                         rhs=e0col[:, 0:2], start=True, stop=True),
                          reads=[b_c[j // 4], B_const], writes=[bps])
                    V(lambda e, ps=ps: e.tensor_copy(out=ckcol[:], in_=ps[:, 0:2 * NT].rearrange("p (j t) -> p j t", t=2)[:, :, 0]),
                      reads=[bps], writes=[b_ck])
                else:
                    slope = slopes[h]
                    nd = NT + 4
                    kbt = SB(st, "kbt", [128, nd], F32)
                    b_kb = Buf()
                    for m in range(nd):
                        V(lambda e, m=m: e.tensor_scalar(out=kbt[:, m:m + 1], in0=ikcol[:], scalar1=slope,
                                                         scalar2=-slope * 128.0 * (m - 3), op0=ALU.mult, op1=ALU.add),
                          reads=[B_const], writes=[b_kb])
                    kms = SB(st, "kms", [128, NB], F32)
                    kmb = SB(st, "kmb", [128, NB], BF16)
                    b_km = Buf()
                    V(lambda e: e.tensor_reduce(out=kms[:], in_=kT[:].rearrange("p (n l) -> p n l", l=256), axis=AX.X,
                                                op=ALU.add), reads=b_k, writes=[b_km])
                    V(lambda e: e.tensor_scalar(out=kmb[:], in0=kms[:], scalar1=1.0 / 256, scalar2=None, op0=ALU.mult),
                      reads=[b_km], writes=[b_km])
                    past01 = SB(st, "past01", [128, NB, NB], F32)
                    pastneg = SB(st, "pastneg", [128, NB, NB], F32)
                    ownb = SB(st, "ownb", [128, NB, NB], F32)
                    b_tab = Buf()
                    P(lambda e: e.memset(past01[:], 1.0), writes=[b_tab])
                    P(lambda e: e.affine_select(out=past01[:], in_=past01[:], pattern=[[1, NB], [-1, NB]],
                                                compare_op=ALU.is_gt, fill=0.0, base=0, channel_multiplier=0),
                      reads=[b_tab], writes=[b_tab])
                    P(lambda e: e.tensor_scalar(out=pastneg[:], in0=past01[:], scalar1=-1.0, scalar2=BIG,
                                                op0=ALU.add, op1=ALU.mult), reads=[b_tab], writes=[b_tab])
                    P(lambda e: e.memset(ownb[:], 0.0), writes=[b_tab])
                    P(lambda e: e.affine_select(out=ownb[:], in_=ownb[:], pattern=[[1, NB], [-1, NB]],
                                                compare_op=ALU.is_equal, fill=-BIG, base=0, channel_multiplier=0),
                      reads=[b_tab], writes=[b_tab])
                    E_f = SB(st, "E_f", [128, NB, 128], F32)
                    E_b = SB(st, "E_b", [128, NB, 128], BF16)
                    P(lambda e: e.memset(E_f[:], 1.0), writes=[b_tab])
                    P(lambda e: e.affine_select(out=E_f[:], in_=E_f[:], pattern=[[-1, NB], [0, 128]],
                                                compare_op=ALU.is_equal, fill=0.0, base=0, channel_multiplier=1),
                      reads=[b_tab], writes=[b_tab])
                    P(lambda e: e.tensor_copy(out=E_b[:], in_=E_f[:]), reads=[b_tab], writes=[b_tab])
                    selT = SB(st, "selT", [128, S], BF16)
                    b_sel = [Buf() for _ in range(NBQ)]
                    gm = SB(st, "gm", [128, NB], F32)
                    top8 = SB(st, "top8", [128, 8], F32)
                    t2 = SB(st, "t2", [128, NB], F32)
                    seln = SB(st, "seln", [128, NB], BF16)
                    b_g = Buf()
                    for blk in range(NBQ):
                        pst, bpst = nextT()
                        for s in range(4):
                            tt = blk * 4 + s
                            own = tt // 2
                            ps, bps = nextA()
                            T(lambda e, ps=ps, tt=tt: e.matmul(ps[:, 0:NB], lhsT=qT[:, tt * 128:(tt + 1) * 128], rhs=kmb[:, :],
                                                               start=True, stop=True), reads=[b_q[blk], b_km], writes=[bps])
                            V(lambda e, ps=ps, own=own: e.tensor_tensor(out=gm[:], in0=ps[:, 0:NB], in1=pastneg[:, own, :],
                                                                        op=ALU.add), reads=[bps, b_tab], writes=[b_g])
                            V(lambda e: e.max(out=top8[:], in_=gm[:]), reads=[b_g], writes=[b_g])
                            V(lambda e, own=own: e.scalar_tensor_tensor(out=t2[:], in0=gm[:], scalar=top8[:, 2:3],
                                                                        in1=past01[:, own, :], op0=ALU.is_ge, op1=ALU.mult),
                              reads=[b_g, b_tab], writes=[b_g])
                            V(lambda e, own=own: e.scalar_tensor_tensor(out=seln[:], in0=t2[:], scalar=BIG,
                                                                        in1=ownb[:, own, :], op0=ALU.mult, op1=ALU.add),
                              reads=[b_g, b_tab], writes=[b_g])
                            T(lambda e, pst=pst, s=s: e.transpose(out=pst[0:NB, s * 128:(s + 1) * 128], in_=seln[:, :],
                                                                  identity=ident_b[:]), reads=[b_g, B_const], writes=[bpst])
                        V(lambda e, pst=pst, blk=blk: e.tensor_copy(out=selT[0:NB, blk * 512:(blk + 1) * 512],
                                                                    in_=pst[0:NB, 0:512]), reads=[bpst], writes=[b_sel[blk]])

                LA = 2
                NBUF = LA + 1
                tmps = [SB(st, "atmp", [128, 512], F32) for i in range(NBUF)]
                pTs = [SB(st, "apT", [128, 512], BF16) for i in range(NBUF)]
                b_tmp = [Buf() for _ in range(NBUF)]
                b_pT = [Buf() for _ in range(NBUF)]
                on = [SB(st, "aon", [128, 128], BF16) for i in range(2)]
                rden = [SB(st, "ard", [128, 1], F32) for i in range(2)]
                b_on = [Buf(), Buf()]
                yblk = [SB(st, "ayb", [128, 512], BF16) for i in range(2)]
                b_yb = [Buf(), Buf()]
                its = [(I, j) for I in range(NBQ) for j in range(4 * I + 4)]

                def stage1(n):
                    I, j = its[n]
                    off = max(0, j - 4 * I)
                    c0 = off * 128
                    ps, bps = nextA()
                    diag = j >= 4 * I
                    T(lambda e: e.matmul(ps[:, c0:512], lhsT=kT[:, j * 128:(j + 1) * 128],
                                         rhs=qT[:, I * 512 + c0:(I + 1) * 512],
                                         start=True, stop=False, skip_group_check=True),
                      reads=[b_k[j // 4], b_q[I]], writes=[bps])
                    if diag:
                        T(lambda e: e.matmul(ps[:, c0:c0 + 128], lhsT=ident_b[:], rhs=maskT_b[:],
                                             start=False, stop=False, skip_group_check=True),
                          reads=[B_const], writes=[bps])
                    if mode == "moba":
                        T(lambda e: e.matmul(ps[:, c0:512], lhsT=E_b[0:NB, j // 2, :],
                                             rhs=selT[0:NB, I * 512 + c0:(I + 1) * 512],
                                             start=False, stop=True, skip_group_check=True),
                          reads=[b_tab, b_sel[I]], writes=[bps])
                    tmp, btm = tmps[n % NBUF], b_tmp[n % NBUF]
                    pT, bpT = pTs[n % NBUF], b_pT[n % NBUF]
                    if mode == "fox":
                        V(lambda e: e.tensor_tensor(out=tmp[:, c0:512], in0=ps[:, c0:512],
                                                    in1=crep[:, I * 512 + c0:(I + 1) * 512], op=ALU.subtract),
                          reads=[bps, b_c[I]], writes=[btm])
                        A(lambda e: e.activation(out=pT[:, c0:512], in_=tmp[:, c0:512], func=AF.Exp,
                                                 bias=ckcol[:, j:j + 1], scale=1.0),
                          reads=[btm, b_ck], writes=[bpT])
                    else:
                        m = (I * 4 - j) + 3
                        V(lambda e: e.scalar_tensor_tensor(out=tmp[:, c0:512], in0=iq[:, c0:512], scalar=-slope,
                                                           in1=ps[:, c0:512], op0=ALU.mult, op1=ALU.add),
                          reads=[bps, B_const], writes=[btm])
                        A(lambda e: e.activation(out=pT[:, c0:512], in_=tmp[:, c0:512], func=AF.Exp,
                                                 bias=kbt[:, m:m + 1], scale=1.0),
                          reads=[btm, b_kb], writes=[bpT])

                def stage2(n):
                    I, j = its[n]
                    off = max(0, j - 4 * I)
                    pT, bpT = pTs[n % NBUF], b_pT[n % NBUF]
                    pb = 2 * (I % 2)
                    for s in range(off, 4):
                        bank = pb + s // 2
                        oc = (s % 2) * 256
                        first = (j == 0 and s % 2 == 0)
                        T(lambda e, s=s, bank=bank, oc=oc, first=first: e.matmul(
                            pso[bank][:, oc:oc + 129], lhsT=pT[:, s * 128:(s + 1) * 128], rhs=vtm[:, j, 0:129],
                            start=first, stop=False, skip_group_check=True),
                          reads=[bpT, b_v[j], b_vones], writes=[bpso[bank]])
                    if j != 4 * I + 3:
                        return
                    yb, byb = yblk[I % 2], b_yb[I % 2]
                    for s in range(4):
                        bank = pb + s // 2
                        oc = (s % 2) * 256
                        o_n, rd_, bo = on[s % 2], rden[s % 2], b_on[s % 2]
                        V(lambda e, bank=bank, oc=oc, rd_=rd_: e.reciprocal(out=rd_[:], in_=pso[bank][:, oc + 128:oc + 129]),
                          reads=[bpso[bank]], writes=[bo])
                        V(lambda e, bank=bank, oc=oc, rd_=rd_, o_n=o_n: e.tensor_scalar(
                            out=o_n[:], in0=pso[bank][:, oc:oc + 128], scalar1=rd_[:, 0:1], scalar2=None, op0=ALU.mult),
                          reads=[bpso[bank], bo], writes=[bo])
                        pst, bpst = nextT()
                        T(lambda e, pst=pst, o_n=o_n: e.transpose(out=pst[:, 0:128], in_=o_n[:], identity=ident_b[:]),
                          reads=[bo, B_const], writes=[bpst])
                        V(lambda e, pst=pst, s=s: e.tensor_tensor(
                            out=yb[:, s * 128:(s + 1) * 128], in0=pst[:, 0:128],
                            in1=szT[:, I * 512 + s * 128:I * 512 + (s + 1) * 128], op=ALU.mult),
                          reads=[bpst, b_z[I]], writes=[byb])
                    ks.dma("pool", lambda e: e.dma_start(
                        out=yT[sq_i, yrow0:yrow0 + 128, I * 512:(I + 1) * 512], in_=yb[:]),
                           reads=[byb], writes=[B_yT])

                N_it = len(its)
                for n in range(N_it + LA):
                    if n < N_it:
                        stage1(n)
                    if n - LA >= 0:
                        stage2(n - LA)
            ks.fence()

        B_yT = Buf()
        B_xres = Buf()

        def gdn_prep(sq_i, st):
            wab, bwab = load_w(st, "wab", w_ab[:, A_A:A_A + 8], 8)
            names = ["g", "beta", "G", "eG", "eGlm", "gl", "bG"]
            tl = {n: SB(st, "gp_" + n, [128, NT, 4], F32) for n in names}
            b = Buf()
            negA = SB(st, "negA", [128, 4], F32)
            tl["negA"] = negA
            A(lambda e: e.activation(out=negA[:], in_=smalls[:, 0:4], func=AF.Exp), reads=[B_const], writes=[b])
            V(lambda e: e.tensor_scalar(out=negA[:], in0=negA[:], scalar1=-1.0, scalar2=None, op0=ALU.mult),
              reads=[b], writes=[b])
            t4 = SB(st, "gp_t4", [128, 4], F32)
            for tt in range(NT):
                ps, bps = nextA()
                proj_tm(wab, bwab, 0, 8, tt, ps, bps)
                V(lambda e, ps=ps: e.tensor_tensor(out=t4[:], in0=ps[:, 0:4], in1=smalls[:, 4:8], op=ALU.add),
                  reads=[bps, B_const], writes=[b])
                A(lambda e: e.activation(out=t4[:], in_=t4[:], func=AF.Exp), reads=[b], writes=[b])
                A(lambda e: e.activation(out=t4[:], in_=t4[:], func=AF.Ln, bias=1.0, scale=1.0), reads=[b], writes=[b])
                V(lambda e, tt=tt: e.tensor_tensor(out=tl["g"][:, tt, :], in0=t4[:], in1=negA[:], op=ALU.mult),
                  reads=[b], writes=[b])
                A(lambda e, ps=ps, tt=tt: e.activation(out=tl["beta"][:, tt, :], in_=ps[:, 4:8], func=AF.Sigmoid),
                  reads=[bps], writes=[b])
                ps2, bps2 = nextA()
                T(lambda e, ps2=ps2, tt=tt: e.matmul(ps2[:, 0:4], lhsT=U_f[:], rhs=tl["g"][:, tt, :], start=True, stop=True),
                  reads=[b, B_const], writes=[bps2])
                T(lambda e, ps2=ps2, tt=tt: e.matmul(ps2[:, 8:12], lhsT=ones_f[:], rhs=tl["g"][:, tt, :], start=True, stop=True),
                  reads=[b, B_const], writes=[bps2])
                V(lambda e, ps2=ps2, tt=tt: e.tensor_copy(out=tl["G"][:, tt, :], in_=ps2[:, 0:4]), reads=[bps2], writes=[b])
                A(lambda e, ps2=ps2, tt=tt: e.activation(out=tl["eG"][:, tt, :], in_=ps2[:, 0:4], func=AF.Exp),
                  reads=[bps2], writes=[b])
                A(lambda e, ps2=ps2, tt=tt: e.activation(out=tl["gl"][:, tt, :], in_=ps2[:, 8:12], func=AF.Exp),
                  reads=[bps2], writes=[b])
                V(lambda e, ps2=ps2, tt=tt: e.tensor_tensor(out=tl["eGlm"][:, tt, :], in0=ps2[:, 8:12], in1=tl["G"][:, tt, :],
                                                           op=ALU.subtract), reads=[bps2, b], writes=[b])
                A(lambda e, tt=tt: e.activation(out=tl["eGlm"][:, tt, :], in_=tl["eGlm"][:, tt, :], func=AF.Exp),
                  reads=[b], writes=[b])
                V(lambda e, tt=tt: e.tensor_tensor(out=tl["bG"][:, tt, :], in0=tl["beta"][:, tt, :], in1=tl["eG"][:, tt, :],
                                                   op=ALU.mult), reads=[b], writes=[b])
            tl["buf"] = b
            return tl

        def gdn_head(sq_i, h, tl, dg, b_dg):
            bsc = tl["buf"]
            with contextlib.ExitStack() as st:
                wq, bwq = load_w(st, "gwq", w_ab[:, A_Q + h * 128:A_Q + (h + 1) * 128], 128, ceng="dve")
                wk, bwk = load_w(st, "gwk", w_ab[:, A_K + h * 128:A_K + (h + 1) * 128], 128, eng="pool", ceng="act")
                wv, bwv = load_w(st, "gwv", w_ab[:, A_V + h * 128:A_V + (h + 1) * 128], 128, ceng="pool")
                wz, bwz = load_w(st, "gwz", w_ab[:, A_Z + h * 128:A_Z + (h + 1) * 128], 128, eng="pool", ceng="dve")
                uT = SB(st, "uT", [128, S + 4], BF16)
                b_u = Buf()
                outT = {n: SB(st, "g_%sT" % n, [128, S], BF16) for n in ("q", "k", "v")}
                b_o = {n: [Buf() for _ in range(NBQ)] for n in ("q", "k", "v")}
                cs = SB(st, "g_cs", [128, 512], F32)
                sqb = SB(st, "g_sqb", [128, 512], BF16)
                rs = SB(st, "g_rs", [128, 512], F32)
                b_cs = Buf()
                P(lambda e: e.memset(uT[:, 0:4], 0.0), writes=[b_u])
                for ci, (n, wb, bw) in enumerate((("q", wq, bwq), ("k", wk, bwk), ("v", wv, bwv))):
                    chunk = ci * 4 + h
                    for blk in range(NBQ):
                        ps, bps = nextA()
                        proj_fm(wb, bw, 0, blk, ps, bps)
                        V(lambda e, ps=ps, blk=blk: e.tensor_copy(out=uT[:, 4 + blk * 512:4 + (blk + 1) * 512], in_=ps[:, :]),
                          reads=[bps], writes=[b_u])
                    for blk in range(NBQ):
                        ps, bps = nextA()
                        for j in range(4):
                            T(lambda e, ps=ps, j=j, blk=blk, chunk=chunk: e.matmul(
                                ps[:, :], lhsT=dg[:, chunk * 4 + j, :], rhs=uT[:, 1 + j + blk * 512:1 + j + (blk + 1) * 512],
                                start=(j == 0), stop=(j == 3)), reads=[b_u, b_dg], writes=[bps])
                        if n == "v":
                            A(lambda e, ps=ps, blk=blk: e.activation(out=outT["v"][:, blk * 512:(blk + 1) * 512], in_=ps[:, :],
                                                                     func=AF.Silu), reads=[bps], writes=[b_o["v"][blk]])
                        else:
                            A(lambda e, ps=ps: e.activation(out=cs[:], in_=ps[:, :], func=AF.Silu), reads=[bps], writes=[b_cs])
                            V(lambda e: e.tensor_tensor(out=sqb[:], in0=cs[:], in1=cs[:], op=ALU.mult), reads=[b_cs], writes=[b_cs])
                            ps2, bps2 = nextA()
                            T(lambda e, ps2=ps2: e.matmul(ps2[:, :], lhsT=ones_b[:], rhs=sqb[:], start=True, stop=True),
                              reads=[b_cs, B_const], writes=[bps2])
                            V(lambda e, ps2=ps2: e.tensor_scalar(out=rs[:], in0=ps2[:, :], scalar1=EPS, scalar2=None, op0=ALU.add),
                              reads=[bps2], writes=[b_cs])
                            A(lambda e: e.activation(out=rs[:], in_=rs[:], func=AF.Sqrt), reads=[b_cs], writes=[b_cs])
                            V(lambda e: e.reciprocal(out=rs[:], in_=rs[:]), reads=[b_cs], writes=[b_cs])
                            sc_ = QSCALE if n == "q" else 1.0
                            V(lambda e, blk=blk, n=n, sc_=sc_: e.scalar_tensor_tensor(
                                out=outT[n][:, blk * 512:(blk + 1) * 512], in0=cs[:], scalar=sc_, in1=rs[:],
                                op0=ALU.mult, op1=ALU.mult), reads=[b_cs], writes=[b_o[n][blk]])
                class TB:
                    def __init__(self, name, shape, dtp):
                        self.t = SB(st, "g_" + name, shape, dtp)
                        self.b = Buf()
                G = 4
                Sst = TB("S", [128, 128], F32)
                V(lambda e: e.memset(Sst.t[:], 0.0), writes=[Sst.b])
                f128 = [128, 128]
                I_ = [dict(gb=TB("gb", f128, F32), dec=TB("dec", f128, F32), decT=TB("decT", f128, F32),
                           P=[TB("Pa", f128, F32), TB("Pb", f128, F32)], PT=[TB("PTa", f128, F32), TB("PTb", f128, F32)],
                           XT=[TB("XTa", f128, F32), TB("XTb", f128, F32)]) for _ in range(G)]
                O_ = [dict(rr=TB("rr", [128, 256], F32), qkT=TB("qkT", f128, F32), kdec=TB("kdec", f128, F32),
                           wT=TB("wT", f128, F32), qnf=TB("qnf", f128, F32)) for _ in range(2 * G)]
                Q_ = [dict(vnew=TB("vnew", f128, F32), qS=TB("qS", f128, F32), o=TB("o", f128, F32), osq=TB("osq", f128, F32),
                           oss=TB("oss", [128, 1], F32), on=TB("on", f128, F32), sz=TB("sz", f128, F32),
                           yb=TB("yb", f128, BF16)) for _ in range(2)]
                ybT = [TB("ybT", [128, 512], BF16) for _ in range(2)]
                kT_, qT_, vT_ = outT["k"], outT["q"], outT["v"]

                freeA = list(zip(PSM["A"], PSM["BA"]))
                freeT = list(zip(PSM["T"], PSM["BT"]))

                def acquire(nA=0, nT=0):
                    while len(freeA) < nA or len(freeT) < nT:
                        yield
                    ra = [freeA.pop(0) for _ in range(nA)]
                    rt = [freeT.pop(0) for _ in range(nT)]
                    return ra + rt

                def relA(*bk):
                    freeA.extend(bk)

                def relT(*bk):
                    freeT.extend(bk)

                def part1(tt, si, oi):
                    I, O = I_[si], O_[oi]
                    blk = tt // 4
                    sl = slice(tt * 128, (tt + 1) * 128)
                    cl = lambda nm: tl[nm][:, tt, h:h + 1]
                    rdq, rdk, rdv = [b_o["q"][blk]], [b_o["k"][blk]], [b_o["v"][blk]]
                    gb, dec, decT = I["gb"], I["dec"], I["decT"]
                    rr_, qkT, kdec, wT, qnf = O["rr"], O["qkT"], O["kdec"], O["wT"], O["qnf"]
                    V(lambda e: e.tensor_scalar(out=gb.t[:], in0=ones_f[:], scalar1=cl("g"), scalar2=None, op0=ALU.mult),
                      reads=[bsc, B_const], writes=[gb.b])
                    (psG, bpsG), (pst, bpst) = yield from acquire(1, 1)
                    T(lambda e: e.matmul(psG[:, 0:128], lhsT=gb.t[:], rhs=U_f[:], start=True, stop=True),
                      reads=[gb.b, B_const], writes=[bpsG])
                    T(lambda e: e.transpose(out=pst[:, 0:128], in_=kT_[:, sl], identity=ident_b[:]),
                      reads=rdk + [B_const], writes=[bpst])
                    T(lambda e: e.transpose(out=pst[:, 128:256], in_=vT_[:, sl], identity=ident_b[:]),
                      reads=rdv + [B_const], writes=[bpst])
                    yield
                    V(lambda e: e.scalar_tensor_tensor(out=dec.t[:], in0=psG[:, 0:128], scalar=cl("G"), in1=maskA[:],
                                                       op0=ALU.subtract, op1=ALU.add), reads=[bpsG, bsc, B_const], writes=[dec.b])
                    V(lambda e: e.scalar_tensor_tensor(out=decT.t[:], in0=psG[:, 0:128], scalar=cl("G"), in1=maskQ[:],
                                                       op0=ALU.subtract, op1=ALU.subtract), reads=[bpsG, bsc, B_const], writes=[decT.b])
                    V(lambda e: e.tensor_scalar(out=rr_.t[:, 0:128], in0=pst[:, 128:256], scalar1=cl("beta"), scalar2=None,
                                                op0=ALU.mult), reads=[bpst, bsc], writes=[rr_.b])
                    V(lambda e: e.tensor_scalar(out=rr_.t[:, 128:256], in0=pst[:, 0:128], scalar1=cl("bG"), scalar2=None,
                                                op0=ALU.mult), reads=[bpst, bsc], writes=[rr_.b])
                    V(lambda e: e.tensor_scalar(out=kdec.t[:], in0=pst[:, 0:128], scalar1=cl("eGlm"), scalar2=None,
                                                op0=ALU.mult), reads=[bpst, bsc], writes=[kdec.b])
                    P(lambda e: e.tensor_copy(out=qnf.t[:], in_=qT_[:, sl]), reads=rdq, writes=[qnf.b])
                    relA((psG, bpsG))
                    relT((pst, bpst))
                    yield
                    ((psK, bpsK),) = yield from acquire(1, 0)
                    T(lambda e: e.matmul(psK[:, 0:128], lhsT=kT_[:, sl], rhs=kT_[:, sl], start=True, stop=True),
                      reads=rdk, writes=[bpsK])
                    T(lambda e: e.matmul(psK[:, 128:256], lhsT=kT_[:, sl], rhs=qT_[:, sl], start=True, stop=True),
                      reads=rdk + rdq, writes=[bpsK])
                    A(lambda e: e.activation(out=dec.t[:], in_=dec.t[:], func=AF.Exp, scale=-1.0), reads=[dec.b], writes=[dec.b])
                    A(lambda e: e.activation(out=decT.t[:], in_=decT.t[:], func=AF.Exp), reads=[decT.b], writes=[decT.b])
                    yield
                    Pc, PTc, XTc = I["P"][0], I["PT"][0], I["XT"][0]
                    V(lambda e: e.scalar_tensor_tensor(out=Pc.t[:], in0=psK[:, 0:128], scalar=cl("beta"), in1=dec.t[:],
                                                       op0=ALU.mult, op1=ALU.mult), reads=[bpsK, bsc, dec.b], writes=[Pc.b])
                    V(lambda e: e.tensor_tensor(out=qkT.t[:], in0=psK[:, 128:256], in1=decT.t[:], op=ALU.mult),
                      reads=[bpsK, decT.b], writes=[qkT.b])
                    relA((psK, bpsK))
                    yield
                    ((psX, bpsX),) = yield from acquire(1, 0)
                    T(lambda e: e.matmul(psX[:, 0:128], lhsT=Pc.t[:], rhs=ident_f[:], start=True, stop=True),
                      reads=[Pc.b, B_const], writes=[bpsX])
                    yield
                    V(lambda e: e.tensor_copy(out=PTc.t[:], in_=psX[:, 0:128]), reads=[bpsX], writes=[PTc.b])
                    V(lambda e: e.scalar_tensor_tensor(out=XTc.t[:], in0=psX[:, 0:128], scalar=-1.0, in1=ident_f[:],
                                                       op0=ALU.mult, op1=ALU.add), reads=[bpsX, B_const], writes=[XTc.b])
                    relA((psX, bpsX))
                    yield
                    pend = None
                    for l in range(1, 8):
                        Pp, PTp = I["P"][(l - 1) % 2], I["PT"][(l - 1) % 2]
                        Pn, PTn = I["P"][l % 2], I["PT"][l % 2]
                        got = yield from acquire((1 if l <= 6 else 0) + (1 if l >= 2 else 0), 0)
                        if l <= 6:
                            psS, bpsS = got.pop(0)
                            T(lambda e, psS=psS, Pp=Pp, PTp=PTp: e.matmul(psS[:, 0:128], lhsT=PTp.t[:], rhs=Pp.t[:], start=True, stop=True),
                              reads=[Pp.b, PTp.b], writes=[bpsS])
                            if l < 6:
                                T(lambda e, psS=psS, Pp=Pp, PTp=PTp: e.matmul(psS[:, 128:256], lhsT=Pp.t[:], rhs=PTp.t[:], start=True, stop=True),
                                  reads=[Pp.b, PTp.b], writes=[bpsS])
                        if l >= 2:
                            XTo, XTn = I["XT"][(l - 2) % 2], I["XT"][(l - 1) % 2]
                            psM, bpsM = got.pop(0)
                            T(lambda e, psM=psM, Pp=Pp, XTo=XTo: e.matmul(psM[:, 0:128], lhsT=Pp.t[:], rhs=XTo.t[:], start=True, stop=True),
                              reads=[Pp.b, XTo.b], writes=[bpsM])
                            pend = (psM, bpsM, XTo, XTn)
                        yield
                        if l <= 6:
                            A(lambda e, psS=psS, Pn=Pn: e.copy(out=Pn.t[:], in_=psS[:, 0:128]), reads=[bpsS], writes=[Pn.b])
                            if l < 6:
                                V(lambda e, psS=psS, PTn=PTn: e.tensor_copy(out=PTn.t[:], in_=psS[:, 128:256]), reads=[bpsS], writes=[PTn.b])
                            relA((psS, bpsS))
                        if pend is not None:
                            psM, bpsM, XTo, XTn = pend
                            V(lambda e, psM=psM, XTo=XTo, XTn=XTn: e.tensor_tensor(out=XTn.t[:], in0=XTo.t[:], in1=psM[:, 0:128], op=ALU.add),
                              reads=[bpsM, XTo.b], writes=[XTn.b])
                            relA((psM, bpsM))
                            pend = None
                        yield
                    XTf = I["XT"][0]
                    ((psL, bpsL),) = yield from acquire(1, 0)
                    T(lambda e: e.matmul(psL[:, 0:256], lhsT=XTf.t[:], rhs=rr_.t[:], start=True, stop=True),
                      reads=[XTf.b, rr_.b], writes=[bpsL])
                    yield
                    V(lambda e: e.tensor_copy(out=rr_.t[:, 0:128], in_=psL[:, 0:128]), reads=[bpsL], writes=[rr_.b])
                    A(lambda e: e.copy(out=rr_.t[:, 128:256], in_=psL[:, 128:256]), reads=[bpsL], writes=[rr_.b])
                    relA((psL, bpsL))
                    yield
                    ((psW, bpsW),) = yield from acquire(1, 0)
                    T(lambda e: e.matmul(psW[:, 0:128], lhsT=rr_.t[:, 128:256], rhs=ident_f[:], start=True, stop=True),
                      reads=[rr_.b, B_const], writes=[bpsW])
                    yield
                    A(lambda e: e.copy(out=wT.t[:], in_=psW[:, 0:128]), reads=[bpsW], writes=[wT.b])
                    relA((psW, bpsW))
                    yield

                posts = []

                def post(tt, qi):
                    Q = Q_[qi]
                    blk = tt // 4
                    o, osq, oss, on_, sz, yb = Q["o"], Q["osq"], Q["oss"], Q["on"], Q["sz"], Q["yb"]
                    ((psZ, bpsZ),) = yield from acquire(1, 0)
                    proj_tm(wz, bwz, 0, 128, tt, psZ, bpsZ)
                    yield
                    A(lambda e: e.activation(out=osq.t[:], in_=o.t[:], func=AF.Square, accum_out=oss.t[:]),
                      reads=[o.b], writes=[osq.b, oss.b])
                    A(lambda e: e.activation(out=sz.t[:], in_=psZ[:, 0:128], func=AF.Silu), reads=[bpsZ], writes=[sz.b])
                    relA((psZ, bpsZ))
                    yield
                    V(lambda e: e.tensor_scalar(out=oss.t[:], in0=oss.t[:], scalar1=1.0 / HD, scalar2=EPS, op0=ALU.mult, op1=ALU.add),
                      reads=[oss.b], writes=[oss.b])
                    yield
                    A(lambda e: e.activation(out=oss.t[:], in_=oss.t[:], func=AF.Sqrt), reads=[oss.b], writes=[oss.b])
                    yield
                    V(lambda e: e.reciprocal(out=oss.t[:], in_=oss.t[:]), reads=[oss.b], writes=[oss.b])
                    V(lambda e: e.scalar_tensor_tensor(out=on_.t[:], in0=o.t[:], scalar=oss.t[:, 0:1], in1=smalls[:, 16:144],
                                                       op0=ALU.mult, op1=ALU.mult), reads=[o.b, oss.b, B_const], writes=[on_.b])
                    V(lambda e: e.tensor_tensor(out=yb.t[:], in0=on_.t[:], in1=sz.t[:], op=ALU.mult),
                      reads=[on_.b, sz.b], writes=[yb.b])
                    yield
                    ((pst, bpst),) = yield from acquire(0, 1)
                    T(lambda e: e.transpose(out=pst[:, 0:128], in_=yb.t[:], identity=ident_b[:]), reads=[yb.b, B_const], writes=[bpst])
                    yield
                    ybt = ybT[blk % 2]
                    s_ = tt % 4
                    A(lambda e: e.copy(out=ybt.t[:, s_ * 128:(s_ + 1) * 128], in_=pst[:, 0:128]), reads=[bpst], writes=[ybt.b])
                    relT((pst, bpst))
                    if s_ == 3:
                        ks.dma("pool", lambda e: e.dma_start(
                            out=yT[sq_i, h * 128:(h + 1) * 128, blk * 512:(blk + 1) * 512], in_=ybt.t[:]),
                               reads=[ybt.b], writes=[B_yT])
                    yield

                def chain(tiles, obase):
                    for k_, tt in enumerate(tiles):
                        O = O_[obase + k_]
                        Q = Q_[tt % 2]
                        cl = lambda nm, tt=tt: tl[nm][:, tt, h:h + 1]
                        ((psR, bpsR),) = yield from acquire(1, 0)
                        T(lambda e, psR=psR, O=O: e.matmul(psR[:, 0:128], lhsT=O["wT"].t[:], rhs=Sst.t[:], start=True, stop=True),
                          reads=[O["wT"].b, Sst.b], writes=[bpsR])
                        T(lambda e, psR=psR, O=O: e.matmul(psR[:, 128:256], lhsT=O["qnf"].t[:], rhs=Sst.t[:], start=True, stop=True),
                          reads=[O["qnf"].b, Sst.b], writes=[bpsR])
                        yield
                        V(lambda e, psR=psR, O=O, Q=Q: e.tensor_tensor(out=Q["vnew"].t[:], in0=O["rr"].t[:, 0:128], in1=psR[:, 0:128],
                                                                      op=ALU.subtract), reads=[bpsR, O["rr"].b], writes=[Q["vnew"].b])
                        A(lambda e, psR=psR, Q=Q, cl=cl: e.activation(out=Q["qS"].t[:], in_=psR[:, 128:256], func=AF.Copy, scale=cl("eG")),
                          reads=[bpsR, bsc], writes=[Q["qS"].b])
                        relA((psR, bpsR))
                        yield
                        ((psO, bpsO),) = yield from acquire(1, 0)
                        T(lambda e, psO=psO, O=O, Q=Q: e.matmul(psO[:, 0:128], lhsT=O["qkT"].t[:], rhs=Q["vnew"].t[:], start=True, stop=True),
                          reads=[O["qkT"].b, Q["vnew"].b], writes=[bpsO])
                        T(lambda e, psO=psO, O=O, Q=Q: e.matmul(psO[:, 128:256], lhsT=O["kdec"].t[:], rhs=Q["vnew"].t[:], start=True, stop=True),
                          reads=[O["kdec"].b, Q["vnew"].b], writes=[bpsO])
                        yield
                        V(lambda e, psO=psO, cl=cl: e.scalar_tensor_tensor(out=Sst.t[:], in0=Sst.t[:], scalar=cl("gl"), in1=psO[:, 128:256],
                                                                          op0=ALU.mult, op1=ALU.add), reads=[bpsO, bsc, Sst.b], writes=[Sst.b])
                        V(lambda e, psO=psO, Q=Q: e.tensor_tensor(out=Q["o"].t[:], in0=Q["qS"].t[:], in1=psO[:, 0:128], op=ALU.add),
                          reads=[bpsO, Q["qS"].b], writes=[Q["o"].b])
                        relA((psO, bpsO))
                        posts.append(post(tt, tt % 2))
                        yield

                _stop = 0

                def _lim(g_):
                    n_ = 0
                    for _ in g_:
                        n_ += 1
                        if _stop and n_ >= _stop:
                            return
                        yield

                def run_round(gens):
                    gens = [_lim(g_) for g_ in gens] if _stop else list(gens)
                    while gens or posts:
                        for g_ in list(gens):
                            try:
                                next(g_)
                            except StopIteration:
                                gens.remove(g_)
                        for g_ in list(posts):
                            try:
                                next(g_)
                            except StopIteration:
                                posts.remove(g_)

                NG = NT // G
                for g in range(NG + 1):
                    gens = []
                    if g < NG:
                        gens += [part1(g * G + k_, k_, (g % 2) * G + k_) for k_ in range(G)]
                    if g >= 1 and not _stop:
                        gens.append(chain([(g - 1) * G + k_ for k_ in range(G)], ((g - 1) % 2) * G))
                    run_round(gens)
            ks.fence()

        def gdn_all(sq_i):
            with contextlib.ExitStack() as st:
                set_psum(st, 6, 2)
                tl = gdn_prep(sq_i, st)
                for h in range(4):
                    gdn_head(sq_i, h, tl, dg, b_dg)

        def make_pool():
            freeA = list(zip(PSM["A"], PSM["BA"]))
            freeT = list(zip(PSM["T"], PSM["BT"]))

            def acquire(nA=0, nT=0):
                while len(freeA) < nA or len(freeT) < nT:
                    yield
                ra = [freeA.pop(0) for _ in range(nA)]
                rt = [freeT.pop(0) for _ in range(nT)]
                return ra + rt

            def relA(*bk):
                freeA.extend(bk)

            def relT(*bk):
                freeT.extend(bk)
            return acquire, relA, relT

        def run_staggered(gen_fns, stagger, max_live):
            pending = list(gen_fns)
            live = []
            rnd = 0
            while pending or live:
                if pending and len(live) < max_live and rnd % stagger == 0:
                    live.append(pending.pop(0)())
                for g_ in list(live):
                    try:
                        next(g_)
                    except StopIteration:
                        live.remove(g_)
                rnd += 1

        def rms_gen(xt, bx, grep, bg, junk, bjunk, ss, rstd, hn, bsc, acquire, relT, dst_fn):
            A(lambda e: e.activation(out=junk[:], in_=xt[:], func=AF.Square, accum_out=ss[:]),
              reads=[bx], writes=[bjunk, bsc])
            yield
            V(lambda e: e.tensor_scalar(out=rstd[:], in0=ss[:], scalar1=1.0 / D, scalar2=EPS, op0=ALU.mult, op1=ALU.add),
              reads=[bsc], writes=[bsc])
            yield
            A(lambda e: e.activation(out=rstd[:], in_=rstd[:], func=AF.Sqrt), reads=[bsc], writes=[bsc])
            yield
            V(lambda e: e.reciprocal(out=rstd[:], in_=rstd[:]), reads=[bsc], writes=[bsc])
            V(lambda e: e.scalar_tensor_tensor(out=hn[:], in0=xt[:], scalar=rstd[:, 0:1], in1=grep[:],
                                               op0=ALU.mult, op1=ALU.mult), reads=[bx, bg, bsc], writes=[bsc])
            yield
            ((pst, bpst),) = yield from acquire(0, 1)
            for c in range(8):
                T(lambda e, c=c: e.transpose(out=pst[:, c * 128:(c + 1) * 128], in_=hn[:, c * 128:(c + 1) * 128],
                                             identity=ident_b[:]), reads=[bsc, B_const], writes=[bpst])
            yield
            dst_fn(pst, bpst)
            relT((pst, bpst))
            yield

        def phase_C(sq_i, layer):
            last = (layer == 1)
            xsrc = x_in if layer == 0 else xres
            with contextlib.ExitStack() as st:
                set_psum(st, 6, 2)
                acquire, relA, relT = make_pool()
                wo = SB(st, "c_wo", [128, 8, D], BF16)
                wg = SB(st, "c_wg", [128, 8, D], BF16)
                wp = SB(st, "c_wp", [128, 2, D], BF16)
                gP = SB(st, "c_gP", [128, D], F32)
                gN = SB(st, "c_gN", [128, D], F32)
                bwo, bwg, bwp = Buf(), Buf(), Buf()
                bgP, bgN = Buf(), Buf()
                with contextlib.ExitStack() as st2:
                    stgs = [SB(st2, "c_stg", [128, 8, 512], F32) for _ in range(3)]
                    b_stgs = [Buf(), Buf(), Buf()]
                    k_ = 0
                    for (dst, bdst, src) in ((wo, bwo, w_out[layer]), (wg, bwg, w_gate[layer])):
                        for hh in range(2):
                            stg, b_stg = stgs[k_ % 3], b_stgs[k_ % 3]
                            k_ += 1
                            ks.dma("sp", lambda e, src=src, hh=hh, stg=stg: e.dma_start(
                                out=stg[:], in_=src[:, hh * 512:(hh + 1) * 512].rearrange("(c p) n -> p c n", p=128)),
                                   writes=[b_stg])
                            cast_on(("dve", "act", "pool")[k_ % 3], dst[:, :, hh * 512:(hh + 1) * 512], stg[:], [b_stg], [bdst])
                    for hh in range(2):
                        stg, b_stg = stgs[k_ % 3], b_stgs[k_ % 3]
                        k_ += 1
                        ks.dma("sp", lambda e, hh=hh, stg=stg: e.dma_start(
                            out=stg[:, 0:2, :], in_=w_pp[layer, :, hh * 512:(hh + 1) * 512].rearrange("(c p) n -> p c n", p=128)),
                               writes=[b_stg])
                        P(lambda e, hh=hh, stg=stg: e.tensor_copy(out=wp[:, :, hh * 512:(hh + 1) * 512], in_=stg[:, 0:2, :]),
                          reads=[b_stg], writes=[bwp])
                    ks.dma("sp", lambda e: e.dma_start(out=gP[:], in_=gvec[2 + layer, :, :]), writes=[bgP])
                    ks.dma("sp", lambda e: e.dma_start(out=gN[:], in_=gvec[4 if last else 1, :, :]), writes=[bgN])
                ks.fence()
                NP_ = 3
                mk = lambda nm, shp, dtp: [SB(st, nm, shp, dtp) for _ in range(NP_)]
                xt = mk("c_x", [128, D], F32)
                yl = mk("c_yl", [128, 8, 128], BF16)
                ptf = mk("c_ptf", [128, 2, 128], F32)
                ptb = mk("c_ptb", [128, 2, 128], BF16)
                x1 = mk("c_x1", [128, D], F32)
                hT = mk("c_hT", [128, 8, 128], BF16)
                gt = mk("c_gt", [128, D], F32)
                hn = mk("c_hn", [128, D], BF16)
                ss = mk("c_ss", [128, 1], F32)
                rstd = mk("c_rs", [128, 1], F32)
                ss2 = mk("c_ss2", [128, 1], F32)
                rstd2 = mk("c_rs2", [128, 1], F32)
                mb = lambda: [Buf() for _ in range(NP_)]
                bx, byl, bpt, bx1, bhT, bgt, bsc, bsc2, bhn2 = mb(), mb(), mb(), mb(), mb(), mb(), mb(), mb(), mb()

                def tile_gen(tt):
                    i = tt % NP_
                    tsl = slice(tt * 128, (tt + 1) * 128)
                    ks.dma("sp", lambda e: e.dma_start(out=xt[i][:], in_=xsrc[sq_i, tsl, :]), reads=[B_xres], writes=[bx[i]])
                    ks.dma("sp", lambda e: e.dma_start(out=yl[i][:], in_=yT[sq_i, :, tsl].rearrange("(c p) n -> p c n", p=128)),
                           reads=[B_yT], writes=[byl[i]])
                    ks.dma("sp", lambda e: e.dma_start(out=ptf[i][:], in_=pT_in[layer, sq_i, :, tsl].rearrange("(c p) n -> p c n", p=128)),
                           writes=[bpt[i]])
                    P(lambda e: e.tensor_copy(out=ptb[i][:], in_=ptf[i][:]), reads=[bpt[i]], writes=[bpt[i]])
                    yield
                    bk = yield from acquire(2, 0)
                    for hh in range(2):
                        ps, bps = bk[hh]
                        for c in range(8):
                            T(lambda e, ps=ps, c=c, hh=hh: e.matmul(ps[:, :], lhsT=yl[i][:, c, :], rhs=wo[:, c, hh * 512:(hh + 1) * 512],
                                                                    start=(c == 0), stop=(c == 7)), reads=[byl[i], bwo], writes=[bps])
                    yield
                    for hh in range(2):
                        ps, bps = bk[hh]
                        V(lambda e, ps=ps, hh=hh: e.tensor_tensor(out=x1[i][:, hh * 512:(hh + 1) * 512], in0=ps[:, :],
                                                                  in1=xt[i][:, hh * 512:(hh + 1) * 512], op=ALU.add),
                          reads=[bps, bx[i]], writes=[bx1[i]])
                    relA(*bk)
                    yield

                    def dstg(pst, bpst):
                        A(lambda e: e.copy(out=hT[i][:], in_=pst[:, :].rearrange("p (c n) -> p c n", c=8)),
                          reads=[bpst], writes=[bhT[i]])
                    yield from rms_gen(x1[i], bx1[i], gP, bgP, gt[i], bgt[i], ss[i], rstd[i], hn[i], bsc[i], acquire, relT, dstg)
                    bk = yield from acquire(4, 0)
                    for hh in range(2):
                        ps, bps = bk[hh]
                        for c in range(8):
                            T(lambda e, ps=ps, c=c, hh=hh: e.matmul(ps[:, :], lhsT=hT[i][:, c, :], rhs=wg[:, c, hh * 512:(hh + 1) * 512],
                                                                    start=(c == 0), stop=(c == 7)), reads=[bhT[i], bwg], writes=[bps])
                        ps, bps = bk[2 + hh]
                        for c in range(2):
                            T(lambda e, ps=ps, c=c, hh=hh: e.matmul(ps[:, :], lhsT=ptb[i][:, c, :], rhs=wp[:, c, hh * 512:(hh + 1) * 512],
                                                                    start=(c == 0), stop=(c == 1)), reads=[bpt[i], bwp], writes=[bps])
                    yield
                    for hh in range(2):
                        ps, bps = bk[hh]
                        A(lambda e, ps=ps, hh=hh: e.activation(out=gt[i][:, hh * 512:(hh + 1) * 512], in_=ps[:, :], func=AF.Sigmoid),
                          reads=[bps], writes=[bgt[i]])
                    yield
                    for hh in range(2):
                        ps, bps = bk[2 + hh]
                        V(lambda e, ps=ps, hh=hh: e.tensor_tensor(out=gt[i][:, hh * 512:(hh + 1) * 512],
                                                                  in0=gt[i][:, hh * 512:(hh + 1) * 512], in1=ps[:, :], op=ALU.mult),
                          reads=[bps, bgt[i]], writes=[bgt[i]])
                    relA(*bk)
                    yield
                    P(lambda e: e.tensor_tensor(out=xt[i][:], in0=gt[i][:], in1=x1[i][:], op=ALU.add),
                      reads=[bgt[i], bx1[i]], writes=[bx[i]])
                    yield
                    if not last:
                        ks.dma("pool", lambda e: e.dma_start(out=xres[sq_i, tsl, :], in_=xt[i][:]), reads=[bx[i]], writes=[B_xres])

                        def dstn(pst, bpst):
                            A(lambda e: e.copy(out=hnT[:, :, tt * 128:(tt + 1) * 128],
                                               in_=pst[:, :].rearrange("p (c n) -> p c n", c=8)),
                              reads=[bpst], writes=[B_hnT[tt]])
                        yield from rms_gen(xt[i], bx[i], gN, bgN, gt[i], bgt[i], ss2[i], rstd2[i], hn[i], bsc2[i], acquire, relT, dstn)
                    else:
                        A(lambda e: e.activation(out=gt[i][:], in_=xt[i][:], func=AF.Square, accum_out=ss2[i][:]),
                          reads=[bx[i]], writes=[bgt[i], bsc2[i]])
                        yield
                        V(lambda e: e.tensor_scalar(out=rstd2[i][:], in0=ss2[i][:], scalar1=1.0 / D, scalar2=EPS,
                                                    op0=ALU.mult, op1=ALU.add), reads=[bsc2[i]], writes=[bsc2[i]])
                        yield
                        A(lambda e: e.activation(out=rstd2[i][:], in_=rstd2[i][:], func=AF.Sqrt), reads=[bsc2[i]], writes=[bsc2[i]])
                        yield
                        V(lambda e: e.reciprocal(out=rstd2[i][:], in_=rstd2[i][:]), reads=[bsc2[i]], writes=[bsc2[i]])
                        V(lambda e: e.scalar_tensor_tensor(out=x1[i][:], in0=xt[i][:], scalar=rstd2[i][:, 0:1],
                                                           in1=gN[:], op0=ALU.mult, op1=ALU.mult),
                          reads=[bx[i], bgN, bsc2[i]], writes=[bx1[i]])
                        yield
                        ks.dma("pool", lambda e: e.dma_start(out=out[sq_i, tsl, :], in_=x1[i][:]), reads=[bx1[i]], writes=[B_out])
                        yield

                run_staggered([(lambda tt=tt: tile_gen(tt)) for tt in range(NT)], stagger=7, max_live=NP_)
            ks.fence()

        B_out = Buf()

        def zero_yT(sq_i, r0, r1):
            with contextlib.ExitStack() as st:
                z = SB(st, "zz", [128, S], BF16)
                bz = Buf()
                P(lambda e: e.memset(z[:], 0.0), writes=[bz])
                for r in range(r0, r1, 128):
                    ks.dma("pool", lambda e, r=r: e.dma_start(out=yT[sq_i, r:r + 128, :], in_=z[:]), reads=[bz], writes=[B_yT])
            ks.fence()

        for sq_i in range(NSEQ):
            phase_A(sq_i)
            if do_gdn:
                gdn_all(sq_i)
                ks.fence()
            else:
                zero_yT(sq_i, 0, 512)
            if do_moba:
                for h in range(4):
                    attn_head(sq_i, "moba", h, w_ab,
                              dict(q=B_Q + h * 128, k=B_K + h * 128, v=B_V + h * 128, z=A_Z + 512 + h * 128), 512 + h * 128)
            else:
                zero_yT(sq_i, 512, 1024)
            phase_C(sq_i, 0)
            if do_fox:
                for h in range(8):
                    attn_head(sq_i, "fox", h, w_c,
                              dict(q=C_Q + h * 128, k=C_K + h * 128, v=C_V + h * 128, z=C_Z + h * 128, f=C_F + h), h * 128)
            else:
                zero_yT(sq_i, 0, 1024)
            phase_C(sq_i, 1)
        ks.emit()
    return nc


def prep_inputs(inp, nseq, ncores):
    f = lambda a: np.ascontiguousarray(np.asarray(a, dtype=np.float32))
    x = f(inp["x"])
    p = f(inp["p"])
    gv = np.stack([inp["norm_g"][0], inp["norm_g"][1], inp["ple_norm_g"][0], inp["ple_norm_g"][1], inp["final_g"]], 0)
    gv = f(np.broadcast_to(np.asarray(gv, np.float32)[:, None, :], (5, 128, D)))
    cw = np.asarray(inp["conv_w"], np.float32)[0]
    convw = f(cw.T.reshape(12, 128, 4).transpose(1, 0, 2))
    sv = np.concatenate([np.asarray(inp["a_log"], np.float32)[0], np.asarray(inp["dt_bias"], np.float32)[0],
                         np.asarray(inp["forget_b"], np.float32)[0], np.asarray(inp["gdn_norm_g"], np.float32)[0]])
    smallv = f(np.broadcast_to(sv[None, :], (128, 144)))
    shared = dict(w_in_ab=f(inp["w_in_ab"][0]), w_in_c=f(inp["w_in_c"][0]), w_out_ab=f(inp["w_out_ab"][0]),
                  w_out_c=f(inp["w_out_c"][0]), w_ple_gate=f(inp["w_ple_gate"]), w_ple_proj=f(inp["w_ple_proj"]),
                  gvec=gv, convw=convw, smallv=smallv,
                  wfc=f(np.asarray(inp["w_in_c"], np.float32)[0][:, C_F:C_F + 8].T.reshape(8, 8, 128).transpose(0, 2, 1)))
    maps = []
    for c in range(ncores):
        sl = slice(c * nseq, (c + 1) * nseq)
        m = dict(shared)
        m["x"] = f(x[sl])
        m["pT"] = f(p[:, sl].transpose(0, 1, 3, 2))
        maps.append(m)
    return maps


def kernel(**inputs):
    x = np.asarray(inputs["x"])
    Bsz, S, _ = x.shape
    ncores = 8
    nseq = Bsz // ncores
    nc = build(S, nseq)
    maps = prep_inputs(inputs, nseq, ncores)
    res = run_bass_kernel_spmd(nc, maps, core_ids=list(range(ncores)))
    return np.concatenate([np.asarray(r["out"], dtype=np.float32) for r in res.results], axis=0)
```

```python
import contextlib
import numpy as np
import concourse.bass as bass
import concourse.mybir as mybir
from concourse.bass_utils import run_bass_kernel_spmd

F32 = mybir.dt.float32
BF16 = mybir.dt.bfloat16
AF = mybir.ActivationFunctionType
ALU = mybir.AluOpType
AX = mybir.AxisListType

D = 1024
HD = 128
EPS = 1e-6
BIG = 1.0e30
QSCALE = HD ** -0.5


class Buf:
    __slots__ = ("w", "r", "excl")

    def __init__(self, excl=False):
        self.w = None
        self.r = []
        self.excl = excl


class KS:
    ENGS = ("pe", "dve", "act", "pool", "sp")

    def __init__(self, nc, n_dma_chan=8):
        self.nc = nc
        self.streams = {e: [] for e in self.ENGS}
        self.cnt = {e: 0 for e in self.ENGS}
        self.seen = {e: {} for e in self.ENGS}
        self.nchan = n_dma_chan
        self.chan_cnt = {}
        self.chan_rr = {e: 0 for e in self.ENGS}
        self.sems = {}

    def _deps(self, eng, reads, writes):
        need = {}

        def add(tok):
            if tok is None:
                return
            k, v = tok
            if k == eng and eng == "pe":
                return
            if need.get(k, 0) < v:
                need[k] = v
        for b in reads:
            add(b.w)
            if b.excl:
                for t in b.r:
                    if t[0] != eng:
                        add(t)
        for b in writes:
            add(b.w)
            for t in b.r:
                add(t)
        out = []
        seen = self.seen[eng]
        for k, v in need.items():
            if seen.get(k, 0) >= v:
                continue
            seen[k] = v
            out.append((k, v))
        return out

    def _commit(self, tok, reads, writes):
        for b in reads:
            if len(b.r) > 24:
                m = {}
                for k, v in b.r:
                    if m.get(k, 0) < v:
                        m[k] = v
                b.r = list(m.items())
            b.r.append(tok)
        for b in writes:
            b.w = tok
            b.r = []

    def op(self, eng, fn, reads=(), writes=()):
        waits = self._deps(eng, reads, writes)
        self.cnt[eng] += 1
        tok = (eng, self.cnt[eng])
        self.streams[eng].append((waits, fn, (eng, 1)))
        self._commit(tok, reads, writes)
        return tok

    def dma(self, eng, fn, reads=(), writes=()):
        c = self.chan_rr[eng]
        self.chan_rr[eng] = (c + 1) % self.nchan
        key = ("dma", eng, c)
        prev = self.chan_cnt.get(key, 0)
        waits = self._deps(eng, reads, writes)
        if prev and self.seen[eng].get(key, 0) < prev:
            self.seen[eng][key] = prev
            waits.append((key, prev))
        self.chan_cnt[key] = prev + 16
        tok = (key, prev + 16)
        self.streams[eng].append((waits, fn, (key, 16)))
        self._commit(tok, reads, writes)
        return tok

    def fence(self):
        cur = dict(self.cnt)
        cur.update(self.chan_cnt)
        for e in self.ENGS:
            waits = []
            for k, v in cur.items():
                if v <= 0 or (k == e):
                    continue
                if self.seen[e].get(k, 0) < v:
                    self.seen[e][k] = v
                    waits.append((k, v))
            if waits:
                self.streams[e].append((waits, None, None))

    def emit(self):
        nc = self.nc
        keys = list(self.ENGS) + sorted(self.chan_cnt.keys(), key=str)
        with contextlib.ExitStack() as st:
            for k in keys:
                nm = "s_" + ("_".join(map(str, k)) if isinstance(k, tuple) else k)
                self.sems[k] = st.enter_context(nc.semaphore(nm))
            block = st.enter_context(nc.Block())
            finals = {k: v for k, v in self.chan_cnt.items()}
            finals.update({e: v for e, v in self.cnt.items() if v > 0})

            def run(eng, h):
                for waits, fn, inc in self.streams[eng]:
                    for k, v in waits:
                        h.wait_ge(self.sems[k], v)
                    if fn is not None:
                        fn(h).then_inc(self.sems[inc[0]], inc[1])
                if eng == "sp":
                    for k, v in finals.items():
                        if k != "sp":
                            h.wait_ge(self.sems[k], v)

            @block.tensor
            def _(h):
                run("pe", h)

            @block.vector
            def _(h):
                run("dve", h)

            @block.scalar
            def _(h):
                run("act", h)

            @block.gpsimd
            def _(h):
                run("pool", h)

            @block.sync
            def _(h):
                run("sp", h)


A_Q, A_K, A_V = 0, 512, 1024
A_A, A_B = 1536, 1540
B_Q, B_K, B_V = 1544, 2056, 2568
A_Z = 3080
C_Q, C_K, C_V, C_F, C_Z = 0, 1024, 2048, 3072, 3080
IN_W = 4104


def build(S, NSEQ, do_gdn=True, do_moba=True, do_fox=True):
    NT = S // 128
    NBQ = S // 512
    NB = S // 256
    nc = bass.Bass("TRN2", target_bir_lowering=False)
    dt = nc.dram_tensor
    x_in = dt("x", [NSEQ, S, D], F32, kind="ExternalInput").ap()
    pT_in = dt("pT", [2, NSEQ, 256, S], F32, kind="ExternalInput").ap()
    w_ab = dt("w_in_ab", [D, IN_W], F32, kind="ExternalInput").ap()
    w_c = dt("w_in_c", [D, IN_W], F32, kind="ExternalInput").ap()
    w_out = [dt("w_out_ab", [D, D], F32, kind="ExternalInput").ap(),
             dt("w_out_c", [D, D], F32, kind="ExternalInput").ap()]
    w_gate = dt("w_ple_gate", [2, D, D], F32, kind="ExternalInput").ap()
    w_pp = dt("w_ple_proj", [2, 256, D], F32, kind="ExternalInput").ap()
    gvec = dt("gvec", [5, 128, D], F32, kind="ExternalInput").ap()
    convw = dt("convw", [128, 12, 4], F32, kind="ExternalInput").ap()
    smallv = dt("smallv", [128, 144], F32, kind="ExternalInput").ap()
    wfc = dt("wfc", [8, 128, 8], F32, kind="ExternalInput").ap()
    out = dt("out", [NSEQ, S, D], F32, kind="ExternalOutput").ap()
    xres = dt("xres", [NSEQ, S, D], F32, kind="Internal").ap()
    yT = dt("yT", [NSEQ, D, S], BF16, kind="Internal").ap()

    ks = KS(nc)
    slopes = [2.0 ** (-8.0 * (h + 1) / 4) for h in range(4)]

    with contextlib.ExitStack() as top:
        uid = [0]

        def SB(st, name, shape, dtype):
            uid[0] += 1
            return st.enter_context(nc.sbuf_tensor("%s_%d" % (name, uid[0]), shape, dtype))

        def PS(st, name, shape, dtype):
            uid[0] += 1
            return st.enter_context(nc.psum_tensor("%s_%d" % (name, uid[0]), shape, dtype))

        hnT = SB(top, "hnT", [128, 8, S], BF16)
        B_hnT = [Buf() for _ in range(NT)]
        ident_f = SB(top, "ident_f", [128, 128], F32)
        ident_b = SB(top, "ident_b", [128, 128], BF16)
        ones_f = SB(top, "ones_f", [128, 128], F32)
        ones_b = SB(top, "ones_b", [128, 128], BF16)
        maskT_b = SB(top, "maskT_b", [128, 128], BF16)
        maskA = SB(top, "maskA", [128, 128], F32)
        maskQ = SB(top, "maskQ", [128, 128], F32)
        U_f = SB(top, "U_f", [128, 128], F32)
        e0col = SB(top, "e0col", [128, 2], F32)
        iq = SB(top, "iq", [128, 512], F32)
        ikcol = SB(top, "ikcol", [128, 1], F32)
        tmpf = SB(top, "tmpf", [128, 128], F32)
        smalls = SB(top, "smalls", [128, 144], F32)
        zero_c = SB(top, "zero_c", [128, 1], F32)
        B_const = Buf()

        PSM = {"A": [], "BA": [], "T": [], "BT": []}
        rr = {"A": 0, "T": 0}

        def set_psum(st, nA, nT):
            PSM["A"] = [PS(st, "psA", [128, 512], F32) for _ in range(nA)]
            PSM["BA"] = [Buf(True) for _ in range(nA)]
            PSM["T"] = [PS(st, "psT", [128, 1024], BF16) for _ in range(nT)]
            PSM["BT"] = [Buf(True) for _ in range(nT)]
            rr["A"] = 0
            rr["T"] = 0

        def nextA():
            i = rr["A"]
            rr["A"] = (i + 1) % len(PSM["A"])
            return PSM["A"][i], PSM["BA"][i]

        def nextT():
            i = rr["T"]
            rr["T"] = (i + 1) % len(PSM["T"])
            return PSM["T"][i], PSM["BT"][i]

        P = lambda fn, **kw: ks.op("pool", fn, **kw)
        V = lambda fn, **kw: ks.op("dve", fn, **kw)
        A = lambda fn, **kw: ks.op("act", fn, **kw)
        T = lambda fn, **kw: ks.op("pe", fn, **kw)

        C = [B_const]
        P(lambda e: e.memset(ident_f[:], 0.0), writes=C)
        P(lambda e: e.affine_select(out=ident_f[:], in_=ident_f[:], pattern=[[-1, 128]], compare_op=ALU.not_equal,
                                    fill=1.0, base=0, channel_multiplier=1), reads=C, writes=C)
        P(lambda e: e.tensor_copy(out=ident_b[:], in_=ident_f[:]), reads=C, writes=C)
        P(lambda e: e.memset(ones_f[:], 1.0), writes=C)
        P(lambda e: e.memset(ones_b[:], 1.0), writes=C)
        P(lambda e: e.memset(zero_c[:], 0.0), writes=C)
        P(lambda e: e.memset(maskQ[:], 0.0), writes=C)
        P(lambda e: e.affine_select(out=maskQ[:], in_=maskQ[:], pattern=[[1, 128]], compare_op=ALU.is_ge,
                                    fill=BIG, base=0, channel_multiplier=-1), reads=C, writes=C)
        P(lambda e: e.tensor_scalar(out=maskT_b[:], in0=maskQ[:], scalar1=-1.0, scalar2=None, op0=ALU.mult),
          reads=C, writes=C)
        P(lambda e: e.memset(maskA[:], 0.0), writes=C)
        P(lambda e: e.affine_select(out=maskA[:], in_=maskA[:], pattern=[[-1, 128]], compare_op=ALU.is_gt,
                                    fill=BIG, base=0, channel_multiplier=1), reads=C, writes=C)
        P(lambda e: e.memset(U_f[:], 1.0), writes=C)
        P(lambda e: e.affine_select(out=U_f[:], in_=U_f[:], pattern=[[1, 128]], compare_op=ALU.is_ge,
                                    fill=0.0, base=0, channel_multiplier=-1), reads=C, writes=C)
        P(lambda e: e.memset(e0col[:], 0.0), writes=C)
        P(lambda e: e.affine_select(out=e0col[:], in_=e0col[:], pattern=[[0, 2]], compare_op=ALU.not_equal,
                                    fill=1.0, base=0, channel_multiplier=1), reads=C, writes=C)
        P(lambda e: e.iota(iq[:], pattern=[[1, 512]], base=0, channel_multiplier=0,
                           allow_small_or_imprecise_dtypes=True), writes=C)
        P(lambda e: e.iota(ikcol[:], pattern=[[0, 1]], base=0, channel_multiplier=1,
                           allow_small_or_imprecise_dtypes=True), writes=C)
        ks.dma("sp", lambda e: e.dma_start(out=smalls[:], in_=smallv[:, :]), writes=C)

        def cast_on(ceng, out_ap, in_ap, reads, writes):
            if ceng == "act":
                A(lambda e: e.copy(out=out_ap, in_=in_ap), reads=reads, writes=writes)
            elif ceng == "dve":
                V(lambda e: e.tensor_copy(out=out_ap, in_=in_ap), reads=reads, writes=writes)
            else:
                P(lambda e: e.tensor_copy(out=out_ap, in_=in_ap), reads=reads, writes=writes)

        def load_w(st, name, src_ap, ncols, eng="sp", kchunks=8, ceng="pool"):
            stg = SB(st, name + "_f", [128, kchunks, ncols], F32)
            wb = SB(st, name + "_b", [128, kchunks, ncols], BF16)
            bs, bw = Buf(), Buf()
            ks.dma(eng, lambda e: e.dma_start(out=stg[:], in_=src_ap.rearrange("(c p) n -> p c n", p=128)), writes=[bs])
            cast_on(ceng, wb[:], stg[:], [bs], [bw])
            return wb, bw

        def rms_to_T(xt, bx, grep, bg, sc, dstT_fn, dst_bufs, extra_reads=()):
            sq, ss, rstd, hn = sc["sq"], sc["ss"], sc["rstd"], sc["hn"]
            bsc = sc["buf"]
            A(lambda e: e.activation(out=sq[:], in_=xt[:], func=AF.Square, accum_out=ss[:]),
              reads=[bx], writes=[bsc])
            V(lambda e: e.tensor_scalar(out=rstd[:], in0=ss[:], scalar1=1.0 / D, scalar2=EPS, op0=ALU.mult, op1=ALU.add),
              reads=[bsc], writes=[bsc])
            A(lambda e: e.activation(out=rstd[:], in_=rstd[:], func=AF.Sqrt), reads=[bsc], writes=[bsc])
            V(lambda e: e.reciprocal(out=rstd[:], in_=rstd[:]), reads=[bsc], writes=[bsc])
            V(lambda e: e.scalar_tensor_tensor(out=hn[:], in0=xt[:], scalar=rstd[:, 0:1], in1=grep[:],
                                               op0=ALU.mult, op1=ALU.mult), reads=[bx, bg, bsc], writes=[bsc])
            pst, bpst = nextT()
            for c in range(8):
                T(lambda e, c=c: e.transpose(out=pst[:, c * 128:(c + 1) * 128], in_=hn[:, c * 128:(c + 1) * 128],
                                             identity=ident_b[:]), reads=[bsc, B_const], writes=[bpst])
            dstT_fn(pst, bpst)

        def proj_fm(wb, bw, col0, blk, ps, bps, ncols=128):
            rd = [bw] + B_hnT[blk * 4:(blk + 1) * 4]
            for c in range(8):
                T(lambda e, c=c: e.matmul(ps[0:ncols, :], lhsT=wb[:, c, col0:col0 + ncols],
                                          rhs=hnT[:, c, blk * 512:(blk + 1) * 512], start=(c == 0), stop=(c == 7)),
                  reads=rd, writes=[bps])

        def proj_tm(wb, bw, col0, ncols, tt, ps, bps, pcol0=0):
            rd = [bw, B_hnT[tt]]
            for c in range(8):
                T(lambda e, c=c: e.matmul(ps[:, pcol0:pcol0 + ncols], lhsT=hnT[:, c, tt * 128:(tt + 1) * 128],
                                          rhs=wb[:, c, col0:col0 + ncols], start=(c == 0), stop=(c == 7)),
                  reads=rd, writes=[bps])

        cw = SB(top, "cw", [128, 12, 4], F32)
        dg = SB(top, "dg", [128, 48, 128], BF16)
        b_dg = Buf()
        ks.dma("sp", lambda e: e.dma_start(out=cw[:], in_=convw[:, :, :]), writes=[b_dg])
        for c in range(12):
            for j in range(4):
                cast_eng = (P, V)[(c * 4 + j) % 2]
                cast_eng(lambda e, c=c, j=j: e.tensor_scalar(out=dg[:, c * 4 + j, :], in0=ident_f[:], scalar1=cw[:, c, j:j + 1],
                                                             scalar2=None, op0=ALU.mult), reads=[b_dg, B_const], writes=[b_dg])

        def phase_A(sq_i):
            with contextlib.ExitStack() as st:
                set_psum(st, 0, 2)
                grep = SB(st, "gA", [128, D], F32)
                bg = Buf()
                ks.dma("sp", lambda e: e.dma_start(out=grep[:], in_=gvec[0, :, :]), writes=[bg])
                xts = [SB(st, "xtA%d" % i, [128, D], F32) for i in range(2)]
                bxs = [Buf(), Buf()]
                scs = [dict(sq=SB(st, "sqA%d" % i, [128, D], F32), ss=SB(st, "ssA%d" % i, [128, 1], F32),
                            rstd=SB(st, "rsA%d" % i, [128, 1], F32), hn=SB(st, "hnA%d" % i, [128, D], BF16),
                            buf=Buf()) for i in range(2)]
                for tt in range(NT):
                    xt, bx, sc = xts[tt % 2], bxs[tt % 2], scs[tt % 2]
                    ks.dma("sp", lambda e, tt=tt, xt=xt: e.dma_start(out=xt[:], in_=x_in[sq_i, tt * 128:(tt + 1) * 128, :]),
                           writes=[bx])

                    def dst(pst, bpst, tt=tt):
                        A(lambda e: e.copy(out=hnT[:, :, tt * 128:(tt + 1) * 128],
                                           in_=pst[:, :].rearrange("p (c n) -> p c n", c=8)),
                          reads=[bpst], writes=[B_hnT[tt]])
                    rms_to_T(xt, bx, grep, bg, sc, dst, None)
            ks.fence()

        def attn_head(sq_i, mode, h, wsrc, cols, yrow0):
            with contextlib.ExitStack() as st:
                set_psum(st, 3, 1)
                pso = [PS(st, "pso", [128, 512], F32) for _ in range(4)]
                bpso = [Buf(True) for _ in range(4)]
                wq, bwq = load_w(st, "wq", wsrc[:, cols["q"]:cols["q"] + 128], 128, ceng="dve")
                wk, bwk = load_w(st, "wk", wsrc[:, cols["k"]:cols["k"] + 128], 128, eng="pool", ceng="act")
                wv, bwv = load_w(st, "wv", wsrc[:, cols["v"]:cols["v"] + 128], 128, ceng="pool")
                wz, bwz = load_w(st, "wz", wsrc[:, cols["z"]:cols["z"] + 128], 128, eng="pool", ceng="dve")
                qT = SB(st, "qT", [128, S], BF16)
                kT = SB(st, "kT", [128, S], BF16)
                szT = SB(st, "szT", [128, S], BF16)
                vtm = SB(st, "vtm", [128, NT, 130], BF16)
                b_q = [Buf() for _ in range(NBQ)]
                b_k = [Buf() for _ in range(NBQ)]
                b_z = [Buf() for _ in range(NBQ)]
                b_v = [Buf() for _ in range(NT)]
                b_vones = Buf()
                P(lambda e: e.memset(vtm[:, :, 128:130], 1.0), writes=[b_vones])
                for blk in range(NBQ):
                    ps, bps = nextA()
                    proj_fm(wq, bwq, 0, blk, ps, bps)
                    A(lambda e, ps=ps, blk=blk: e.activation(out=qT[:, blk * 512:(blk + 1) * 512], in_=ps[:, :],
                                                             func=AF.Copy, scale=QSCALE), reads=[bps], writes=[b_q[blk]])
                    ps, bps = nextA()
                    proj_fm(wk, bwk, 0, blk, ps, bps)
                    V(lambda e, ps=ps, blk=blk: e.tensor_copy(out=kT[:, blk * 512:(blk + 1) * 512], in_=ps[:, :]),
                      reads=[bps], writes=[b_k[blk]])
                    ps, bps = nextA()
                    proj_fm(wz, bwz, 0, blk, ps, bps)
                    A(lambda e, ps=ps, blk=blk: e.activation(out=szT[:, blk * 512:(blk + 1) * 512], in_=ps[:, :],
                                                             func=AF.Silu), reads=[bps], writes=[b_z[blk]])
                    ps, bps = nextA()
                    for s in range(4):
                        proj_tm(wv, bwv, 0, 128, blk * 4 + s, ps, bps, pcol0=s * 128)
                    V(lambda e, ps=ps, blk=blk: e.tensor_copy(
                        out=vtm[:, blk * 4:(blk + 1) * 4, 0:128], in_=ps[:, :].rearrange("p (s n) -> p s n", s=4)),
                      reads=[bps], writes=b_v[blk * 4:(blk + 1) * 4])

                if mode == "fox":
                    wf_s = SB(st, "wf_s", [128, 8, 1], F32)
                    wf_b = SB(st, "wf_b", [128, 8, 128], BF16)
                    bwf = Buf()
                    ks.dma("sp", lambda e: e.dma_start(out=wf_s[:, :, 0], in_=wfc[h, :, :]), writes=[bwf])
                    P(lambda e: e.tensor_copy(out=wf_b[:], in_=wf_s[:].to_broadcast([128, 8, 128])),
                      reads=[bwf], writes=[bwf])
                    crep = SB(st, "crep", [128, S], F32)
                    ltmp = SB(st, "ltmp", [128, 512], F32)
                    negb = SB(st, "negb", [128, 1], F32)
                    b_c = [Buf() for _ in range(NBQ)]
                    b_l = Buf()
                    V(lambda e: e.tensor_scalar(out=negb[:], in0=smalls[:, 8 + h:9 + h], scalar1=-1.0, scalar2=None,
                                                op0=ALU.mult), reads=[B_const], writes=[b_l])
                    for blk in range(NBQ):
                        ps, bps = nextA()
                        proj_fm(wf_b, bwf, 0, blk, ps, bps)
                        A(lambda e, ps=ps: e.activation(out=ltmp[:], in_=ps[:, :], func=AF.Exp, bias=negb[:, 0:1], scale=-1.0),
                          reads=[bps, b_l], writes=[b_l])
                        A(lambda e: e.activation(out=ltmp[:], in_=ltmp[:], func=AF.Ln, bias=1.0, scale=1.0),
                          reads=[b_l], writes=[b_l])
                        init = zero_c[:, 0:1] if blk == 0 else crep[:, blk * 512 - 1:blk * 512]
                        V(lambda e, blk=blk, init=init: e.tensor_tensor_scan(
                            out=crep[:, blk * 512:(blk + 1) * 512], data0=ones_f[:, 0:1].to_broadcast([128, 512]),
                            data1=ltmp[:], initial=init, op0=ALU.mult, op1=ALU.add),
                          reads=[b_l, B_const] + ([b_c[blk - 1]] if blk else []), writes=[b_c[blk]])
                    ckcol = SB(st, "ckcol", [128, NT], F32)
                    b_ck = Buf()
                    ps, bps = nextA()
                    for j in range(NT):
                        T(lambda e, j=j, ps=ps: e.matmul(ps[:, 2 * j:2 * j + 2], lhsT=crep[:, j * 128:(j + 1) * 128],
                                                         rhs=e0col[:, 0:2], start=True, stop=True),
                          reads=[b_c[j // 4], B_const], writes=[bps])
                    V(lambda e, ps=ps: e.tensor_copy(out=ckcol[:], in_=ps[:, 0:2 * NT].rearrange("p (j t) -> p j t", t=2)[:, :, 0]),
                      reads=[bps], writes=[b_ck])
                else:
                    slope = slopes[h]
                    nd = NT + 4
                    kbt = SB(st, "kbt", [128, nd], F32)
                    b_kb = Buf()
                    for m in range(nd):
                        V(lambda e, m=m: e.tensor_scalar(out=kbt[:, m:m + 1], in0=ikcol[:], scalar1=slope,
                                                         scalar2=-slope * 128.0 * (m - 3), op0=ALU.mult, op1=ALU.add),
                          reads=[B_const], writes=[b_kb])
                    kms = SB(st, "kms", [128, NB], F32)
                    kmb = SB(st, "kmb", [128, NB], BF16)
                    b_km = Buf()
                    V(lambda e: e.tensor_reduce(out=kms[:], in_=kT[:].rearrange("p (n l) -> p n l", l=256), axis=AX.X,
                                                op=ALU.add), reads=b_k, writes=[b_km])
                    V(lambda e: e.tensor_scalar(out=kmb[:], in0=kms[:], scalar1=1.0 / 256, scalar2=None, op0=ALU.mult),
                      reads=[b_km], writes=[b_km])
                    past01 = SB(st, "past01", [128, NB, NB], F32)
                    pastneg = SB(st, "pastneg", [128, NB, NB], F32)
                    ownb = SB(st, "ownb", [128, NB, NB], F32)
                    b_tab = Buf()
                    P(lambda e: e.memset(past01[:], 1.0), writes=[b_tab])
                    P(lambda e: e.affine_select(out=past01[:], in_=past01[:], pattern=[[1, NB], [-1, NB]],
                                                compare_op=ALU.is_gt, fill=0.0, base=0, channel_multiplier=0),
                      reads=[b_tab], writes=[b_tab])
                    P(lambda e: e.tensor_scalar(out=pastneg[:], in0=past01[:], scalar1=-1.0, scalar2=BIG,
                                                op0=ALU.add, op1=ALU.mult), reads=[b_tab], writes=[b_tab])
                    P(lambda e: e.memset(ownb[:], 0.0), writes=[b_tab])
                    P(lambda e: e.affine_select(out=ownb[:], in_=ownb[:], pattern=[[1, NB], [-1, NB]],
                                                compare_op=ALU.is_equal, fill=-BIG, base=0, channel_multiplier=0),
                      reads=[b_tab], writes=[b_tab])
                    E_f = SB(st, "E_f", [128, NB, 128], F32)
                    E_b = SB(st, "E_b", [128, NB, 128], BF16)
                    P(lambda e: e.memset(E_f[:], 1.0), writes=[b_tab])
                    P(lambda e: e.affine_select(out=E_f[:], in_=E_f[:], pattern=[[-1, NB], [0, 128]],
                                                compare_op=ALU.is_equal, fill=0.0, base=0, channel_multiplier=1),
                      reads=[b_tab], writes=[b_tab])
                    P(lambda e: e.tensor_copy(out=E_b[:], in_=E_f[:]), reads=[b_tab], writes=[b_tab])
                    selT = SB(st, "selT", [128, S], BF16)
                    b_sel = [Buf() for _ in range(NBQ)]
                    gm = SB(st, "gm", [128, NB], F32)
                    top8 = SB(st, "top8", [128, 8], F32)
                    t2 = SB(st, "t2", [128, NB], F32)
                    seln = SB(st, "seln", [128, NB], BF16)
                    b_g = Buf()
                    for blk in range(NBQ):
                        pst, bpst = nextT()
                        for s in range(4):
                            tt = blk * 4 + s
                            own = tt // 2
                            ps, bps = nextA()
                            T(lambda e, ps=ps, tt=tt: e.matmul(ps[:, 0:NB], lhsT=qT[:, tt * 128:(tt + 1) * 128], rhs=kmb[:, :],
                                                               start=True, stop=True), reads=[b_q[blk], b_km], writes=[bps])
                            V(lambda e, ps=ps, own=own: e.tensor_tensor(out=gm[:], in0=ps[:, 0:NB], in1=pastneg[:, own, :],
                                                                        op=ALU.add), reads=[bps, b_tab], writes=[b_g])
                            V(lambda e: e.max(out=top8[:], in_=gm[:]), reads=[b_g], writes=[b_g])
                            V(lambda e, own=own: e.scalar_tensor_tensor(out=t2[:], in0=gm[:], scalar=top8[:, 2:3],
                                                                        in1=past01[:, own, :], op0=ALU.is_ge, op1=ALU.mult),
                              reads=[b_g, b_tab], writes=[b_g])
                            V(lambda e, own=own: e.scalar_tensor_tensor(out=seln[:], in0=t2[:], scalar=BIG,
                                                                        in1=ownb[:, own, :], op0=ALU.mult, op1=ALU.add),
                              reads=[b_g, b_tab], writes=[b_g])
                            T(lambda e, pst=pst, s=s: e.transpose(out=pst[0:NB, s * 128:(s + 1) * 128], in_=seln[:, :],
                                                                  identity=ident_b[:]), reads=[b_g, B_const], writes=[bpst])
                        V(lambda e, pst=pst, blk=blk: e.tensor_copy(out=selT[0:NB, blk * 512:(blk + 1) * 512],
                                                                    in_=pst[0:NB, 0:512]), reads=[bpst], writes=[b_sel[blk]])

                LA = 2
                NBUF = LA + 1
                tmps = [SB(st, "atmp", [128, 512], F32) for i in range(NBUF)]
                pTs = [SB(st, "apT", [128, 512], BF16) for i in range(NBUF)]
                b_tmp = [Buf() for _ in range(NBUF)]
                b_pT = [Buf() for _ in range(NBUF)]
                on = [SB(st, "aon", [128, 128], BF16) for i in range(2)]
                rden = [SB(st, "ard", [128, 1], F32) for i in range(2)]
                b_on = [Buf(), Buf()]
                yblk = [SB(st, "ayb", [128, 512], BF16) for i in range(2)]
                b_yb = [Buf(), Buf()]
                its = [(I, j) for I in range(NBQ) for j in range(4 * I + 4)]

                def stage1(n):
                    I, j = its[n]
                    off = max(0, j - 4 * I)
                    c0 = off * 128
                    ps, bps = nextA()
                    diag = j >= 4 * I
                    T(lambda e: e.matmul(ps[:, c0:512], lhsT=kT[:, j * 128:(j + 1) * 128],
                                         rhs=qT[:, I * 512 + c0:(I + 1) * 512],
                                         start=True, stop=False, skip_group_check=True),
                      reads=[b_k[j // 4], b_q[I]], writes=[bps])
                    if diag:
                        T(lambda e: e.matmul(ps[:, c0:c0 + 128], lhsT=ident_b[:], rhs=maskT_b[:],
                                             start=False, stop=False, skip_group_check=True),
                          reads=[B_const], writes=[bps])
                    if mode == "moba":
                        T(lambda e: e.matmul(ps[:, c0:512], lhsT=E_b[0:NB, j // 2, :],
                                             rhs=selT[0:NB, I * 512 + c0:(I + 1) * 512],
                                             start=False, stop=True, skip_group_check=True),
                          reads=[b_tab, b_sel[I]], writes=[bps])
                    tmp, btm = tmps[n % NBUF], b_tmp[n % NBUF]
                    pT, bpT = pTs[n % NBUF], b_pT[n % NBUF]
                    if mode == "fox":
                        V(lambda e: e.tensor_tensor(out=tmp[:, c0:512], in0=ps[:, c0:512],
                                                    in1=crep[:, I * 512 + c0:(I + 1) * 512], op=ALU.subtract),
                          reads=[bps, b_c[I]], writes=[btm])
                        A(lambda e: e.activation(out=pT[:, c0:512], in_=tmp[:, c0:512], func=AF.Exp,
                                                 bias=ckcol[:, j:j + 1], scale=1.0),
                          reads=[btm, b_ck], writes=[bpT])
                    else:
                        m = (I * 4 - j) + 3
                        V(lambda e: e.scalar_tensor_tensor(out=tmp[:, c0:512], in0=iq[:, c0:512], scalar=-slope,
                                                           in1=ps[:, c0:512], op0=ALU.mult, op1=ALU.add),
                          reads=[bps, B_const], writes=[btm])
                        A(lambda e: e.activation(out=pT[:, c0:512], in_=tmp[:, c0:512], func=AF.Exp,
                                                 bias=kbt[:, m:m + 1], scale=1.0),
                          reads=[btm, b_kb], writes=[bpT])

                def stage2(n):
                    I, j = its[n]
                    off = max(0, j - 4 * I)
                    pT, bpT = pTs[n % NBUF], b_pT[n % NBUF]
                    pb = 2 * (I % 2)
                    for s in range(off, 4):
                        bank = pb + s // 2
                        oc = (s % 2) * 256
                        first = (j == 0 and s % 2 == 0)
                        T(lambda e, s=s, bank=bank, oc=oc, first=first: e.matmul(
                            pso[bank][:, oc:oc + 129], lhsT=pT[:, s * 128:(s + 1) * 128], rhs=vtm[:, j, 0:129],
                            start=first, stop=False, skip_group_check=True),
                          reads=[bpT, b_v[j], b_vones], writes=[bpso[bank]])
                    if j != 4 * I + 3:
                        return
                    yb, byb = yblk[I % 2], b_yb[I % 2]
                    for s in range(4):
                        bank = pb + s // 2
                        oc = (s % 2) * 256
                        o_n, rd_, bo = on[s % 2], rden[s % 2], b_on[s % 2]
                        V(lambda e, bank=bank, oc=oc, rd_=rd_: e.reciprocal(out=rd_[:], in_=pso[bank][:, oc + 128:oc + 129]),
                          reads=[bpso[bank]], writes=[bo])
                        V(lambda e, bank=bank, oc=oc, rd_=rd_, o_n=o_n: e.tensor_scalar(
                            out=o_n[:], in0=pso[bank][:, oc:oc + 128], scalar1=rd_[:, 0:1], scalar2=None, op0=ALU.mult),
                          reads=[bpso[bank], bo], writes=[bo])
                        pst, bpst = nextT()
                        T(lambda e, pst=pst, o_n=o_n: e.transpose(out=pst[:, 0:128], in_=o_n[:], identity=ident_b[:]),
                          reads=[bo, B_const], writes=[bpst])
                        V(lambda e, pst=pst, s=s: e.tensor_tensor(
                            out=yb[:, s * 128:(s + 1) * 128], in0=pst[:, 0:128],
                            in1=szT[:, I * 512 + s * 128:I * 512 + (s + 1) * 128], op=ALU.mult),
                          reads=[bpst, b_z[I]], writes=[byb])
                    ks.dma("pool", lambda e: e.dma_start(
                        out=yT[sq_i, yrow0:yrow0 + 128, I * 512:(I + 1) * 512], in_=yb[:]),
                           reads=[byb], writes=[B_yT])

                N_it = len(its)
                for n in range(N_it + LA):
                    if n < N_it:
                        stage1(n)
                    if n - LA >= 0:
                        stage2(n - LA)
            ks.fence()

        B_yT = Buf()
        B_xres = Buf()

        def gdn_prep(sq_i, st):
            wab, bwab = load_w(st, "wab", w_ab[:, A_A:A_A + 8], 8)
            names = ["g", "beta", "G", "eG", "eGlm", "gl", "bG"]
            tl = {n: SB(st, "gp_" + n, [128, NT, 4], F32) for n in names}
            b = Buf()
            negA = SB(st, "negA", [128, 4], F32)
            tl["negA"] = negA
            A(lambda e: e.activation(out=negA[:], in_=smalls[:, 0:4], func=AF.Exp), reads=[B_const], writes=[b])
            V(lambda e: e.tensor_scalar(out=negA[:], in0=negA[:], scalar1=-1.0, scalar2=None, op0=ALU.mult),
              reads=[b], writes=[b])
            t4 = SB(st, "gp_t4", [128, 4], F32)
            for tt in range(NT):
                ps, bps = nextA()
                proj_tm(wab, bwab, 0, 8, tt, ps, bps)
                V(lambda e, ps=ps: e.tensor_tensor(out=t4[:], in0=ps[:, 0:4], in1=smalls[:, 4:8], op=ALU.add),
                  reads=[bps, B_const], writes=[b])
                A(lambda e: e.activation(out=t4[:], in_=t4[:], func=AF.Exp), reads=[b], writes=[b])
                A(lambda e: e.activation(out=t4[:], in_=t4[:], func=AF.Ln, bias=1.0, scale=1.0), reads=[b], writes=[b])
                V(lambda e, tt=tt: e.tensor_tensor(out=tl["g"][:, tt, :], in0=t4[:], in1=negA[:], op=ALU.mult),
                  reads=[b], writes=[b])
                A(lambda e, ps=ps, tt=tt: e.activation(out=tl["beta"][:, tt, :], in_=ps[:, 4:8], func=AF.Sigmoid),
                  reads=[bps], writes=[b])
                ps2, bps2 = nextA()
                T(lambda e, ps2=ps2, tt=tt: e.matmul(ps2[:, 0:4], lhsT=U_f[:], rhs=tl["g"][:, tt, :], start=True, stop=True),
                  reads=[b, B_const], writes=[bps2])
                T(lambda e, ps2=ps2, tt=tt: e.matmul(ps2[:, 8:12], lhsT=ones_f[:], rhs=tl["g"][:, tt, :], start=True, stop=True),
                  reads=[b, B_const], writes=[bps2])
                V(lambda e, ps2=ps2, tt=tt: e.tensor_copy(out=tl["G"][:, tt, :], in_=ps2[:, 0:4]), reads=[bps2], writes=[b])
                A(lambda e, ps2=ps2, tt=tt: e.activation(out=tl["eG"][:, tt, :], in_=ps2[:, 0:4], func=AF.Exp),
                  reads=[bps2], writes=[b])
                A(lambda e, ps2=ps2, tt=tt: e.activation(out=tl["gl"][:, tt, :], in_=ps2[:, 8:12], func=AF.Exp),
                  reads=[bps2], writes=[b])
                V(lambda e, ps2=ps2, tt=tt: e.tensor_tensor(out=tl["eGlm"][:, tt, :], in0=ps2[:, 8:12], in1=tl["G"][:, tt, :],
                                                           op=ALU.subtract), reads=[bps2, b], writes=[b])
                A(lambda e, tt=tt: e.activation(out=tl["eGlm"][:, tt, :], in_=tl["eGlm"][:, tt, :], func=AF.Exp),
                  reads=[b], writes=[b])
                V(lambda e, tt=tt: e.tensor_tensor(out=tl["bG"][:, tt, :], in0=tl["beta"][:, tt, :], in1=tl["eG"][:, tt, :],
                                                   op=ALU.mult), reads=[b], writes=[b])
            tl["buf"] = b
            return tl

        def gdn_head(sq_i, h, tl, dg, b_dg):
            bsc = tl["buf"]
            with contextlib.ExitStack() as st:
                wq, bwq = load_w(st, "gwq", w_ab[:, A_Q + h * 128:A_Q + (h + 1) * 128], 128, ceng="dve")
                wk, bwk = load_w(st, "gwk", w_ab[:, A_K + h * 128:A_K + (h + 1) * 128], 128, eng="pool", ceng="act")
                wv, bwv = load_w(st, "gwv", w_ab[:, A_V + h * 128:A_V + (h + 1) * 128], 128, ceng="pool")
                wz, bwz = load_w(st, "gwz", w_ab[:, A_Z + h * 128:A_Z + (h + 1) * 128], 128, eng="pool", ceng="dve")
                uT = SB(st, "uT", [128, S + 4], BF16)
                b_u = Buf()
                outT = {n: SB(st, "g_%sT" % n, [128, S], BF16) for n in ("q", "k", "v")}
                b_o = {n: [Buf() for _ in range(NBQ)] for n in ("q", "k", "v")}
                cs_l = [SB(st, "g_cs", [128, 512], F32) for _ in range(2)]
                sqb_l = [SB(st, "g_sqb", [128, 512], BF16) for _ in range(2)]
                rs_l = [SB(st, "g_rs", [128, 512], F32) for _ in range(2)]
                b_cs_l = [Buf() for _ in range(2)]
                P(lambda e: e.memset(uT[:, 0:4], 0.0), writes=[b_u])
                for ci, (n, wb, bw) in enumerate((("q", wq, bwq), ("k", wk, bwk), ("v", wv, bwv))):
                    chunk = ci * 4 + h
                    for blk in range(NBQ):
                        ps, bps = nextA()
                        proj_fm(wb, bw, 0, blk, ps, bps)
                        V(lambda e, ps=ps, blk=blk: e.tensor_copy(out=uT[:, 4 + blk * 512:4 + (blk + 1) * 512], in_=ps[:, :]),
                          reads=[bps], writes=[b_u])
                    acq_, relA_, relT_ = make_pool()

                    def conv_gen(blk, n=n, chunk=chunk):
                        cs, sqb, rs, b_cs = cs_l[blk % 2], sqb_l[blk % 2], rs_l[blk % 2], b_cs_l[blk % 2]
                        ((ps, bps),) = yield from acq_(1, 0)
                        for j in range(4):
                            T(lambda e, j=j: e.matmul(
                                ps[:, :], lhsT=dg[:, chunk * 4 + j, :], rhs=uT[:, 1 + j + blk * 512:1 + j + (blk + 1) * 512],
                                start=(j == 0), stop=(j == 3)), reads=[b_u, b_dg], writes=[bps])
                        yield
                        if n == "v":
                            A(lambda e: e.activation(out=outT["v"][:, blk * 512:(blk + 1) * 512], in_=ps[:, :],
                                                     func=AF.Silu), reads=[bps], writes=[b_o["v"][blk]])
                            relA_((ps, bps))
                            yield
                            return
                        A(lambda e: e.activation(out=cs[:], in_=ps[:, :], func=AF.Silu), reads=[bps], writes=[b_cs])
                        relA_((ps, bps))
                        yield
                        V(lambda e: e.tensor_tensor(out=sqb[:], in0=cs[:], in1=cs[:], op=ALU.mult), reads=[b_cs], writes=[b_cs])
                        yield
                        ((ps2, bps2),) = yield from acq_(1, 0)
                        T(lambda e: e.matmul(ps2[:, :], lhsT=ones_b[:], rhs=sqb[:], start=True, stop=True),
                          reads=[b_cs, B_const], writes=[bps2])
                        yield
                        V(lambda e: e.tensor_scalar(out=rs[:], in0=ps2[:, :], scalar1=EPS, scalar2=None, op0=ALU.add),
                          reads=[bps2], writes=[b_cs])
                        relA_((ps2, bps2))
                        yield
                        A(lambda e: e.activation(out=rs[:], in_=rs[:], func=AF.Sqrt), reads=[b_cs], writes=[b_cs])
                        yield
                        sc_ = QSCALE if n == "q" else 1.0
                        V(lambda e: e.reciprocal(out=rs[:], in_=rs[:]), reads=[b_cs], writes=[b_cs])
                        V(lambda e: e.scalar_tensor_tensor(
                            out=outT[n][:, blk * 512:(blk + 1) * 512], in0=cs[:], scalar=sc_, in1=rs[:],
                            op0=ALU.mult, op1=ALU.mult), reads=[b_cs], writes=[b_o[n][blk]])
                        yield

                    run_staggered([(lambda blk=blk: conv_gen(blk)) for blk in range(NBQ)], stagger=3, max_live=2)
                class TB:
                    def __init__(self, name, shape, dtp):
                        self.t = SB(st, "g_" + name, shape, dtp)
                        self.b = Buf()
                G = 4
                Sst = TB("S", [128, 128], F32)
                V(lambda e: e.memset(Sst.t[:], 0.0), writes=[Sst.b])
                f128 = [128, 128]
                I_ = [dict(gb=TB("gb", f128, F32), dec=TB("dec", f128, F32), decT=TB("decT", f128, F32),
                           P=[TB("Pa", f128, F32), TB("Pb", f128, F32)], PT=[TB("PTa", f128, F32), TB("PTb", f128, F32)],
                           XT=[TB("XTa", f128, F32), TB("XTb", f128, F32)]) for _ in range(G)]
                O_ = [dict(rr=TB("rr", [128, 256], F32), qkT=TB("qkT", f128, F32), kdec=TB("kdec", f128, F32),
                           wT=TB("wT", f128, F32), qnf=TB("qnf", f128, F32)) for _ in range(2 * G)]
                Q_ = [dict(vnew=TB("vnew", f128, F32), qS=TB("qS", f128, F32), o=TB("o", f128, F32), osq=TB("osq", f128, F32),
                           oss=TB("oss", [128, 1], F32), on=TB("on", f128, F32), sz=TB("sz", f128, F32),
                           yb=TB("yb", f128, BF16)) for _ in range(2)]
                ybT = [TB("ybT", [128, 512], BF16) for _ in range(2)]
                kT_, qT_, vT_ = outT["k"], outT["q"], outT["v"]

                freeA = list(zip(PSM["A"], PSM["BA"]))
                freeT = list(zip(PSM["T"], PSM["BT"]))

                def acquire(nA=0, nT=0):
                    while len(freeA) < nA or len(freeT) < nT:
                        yield
                    ra = [freeA.pop(0) for _ in range(nA)]
                    rt = [freeT.pop(0) for _ in range(nT)]
                    return ra + rt

                def relA(*bk):
                    freeA.extend(bk)

                def relT(*bk):
                    freeT.extend(bk)

                def part1(tt, si, oi):
                    I, O = I_[si], O_[oi]
                    blk = tt // 4
                    sl = slice(tt * 128, (tt + 1) * 128)
                    cl = lambda nm: tl[nm][:, tt, h:h + 1]
                    rdq, rdk, rdv = [b_o["q"][blk]], [b_o["k"][blk]], [b_o["v"][blk]]
                    gb, dec, decT = I["gb"], I["dec"], I["decT"]
                    rr_, qkT, kdec, wT, qnf = O["rr"], O["qkT"], O["kdec"], O["wT"], O["qnf"]
                    V(lambda e: e.tensor_scalar(out=gb.t[:], in0=ones_f[:], scalar1=cl("g"), scalar2=None, op0=ALU.mult),
                      reads=[bsc, B_const], writes=[gb.b])
                    (psG, bpsG), (pst, bpst) = yield from acquire(1, 1)
                    T(lambda e: e.matmul(psG[:, 0:128], lhsT=gb.t[:], rhs=U_f[:], start=True, stop=True),
                      reads=[gb.b, B_const], writes=[bpsG])
                    T(lambda e: e.transpose(out=pst[:, 0:128], in_=kT_[:, sl], identity=ident_b[:]),
                      reads=rdk + [B_const], writes=[bpst])
                    T(lambda e: e.transpose(out=pst[:, 128:256], in_=vT_[:, sl], identity=ident_b[:]),
                      reads=rdv + [B_const], writes=[bpst])
                    yield
                    V(lambda e: e.scalar_tensor_tensor(out=dec.t[:], in0=psG[:, 0:128], scalar=cl("G"), in1=maskA[:],
                                                       op0=ALU.subtract, op1=ALU.add), reads=[bpsG, bsc, B_const], writes=[dec.b])
                    V(lambda e: e.scalar_tensor_tensor(out=decT.t[:], in0=psG[:, 0:128], scalar=cl("G"), in1=maskQ[:],
                                                       op0=ALU.subtract, op1=ALU.subtract), reads=[bpsG, bsc, B_const], writes=[decT.b])
                    V(lambda e: e.tensor_scalar(out=rr_.t[:, 0:128], in0=pst[:, 128:256], scalar1=cl("beta"), scalar2=None,
                                                op0=ALU.mult), reads=[bpst, bsc], writes=[rr_.b])
                    V(lambda e: e.tensor_scalar(out=rr_.t[:, 128:256], in0=pst[:, 0:128], scalar1=cl("bG"), scalar2=None,
                                                op0=ALU.mult), reads=[bpst, bsc], writes=[rr_.b])
                    V(lambda e: e.tensor_scalar(out=kdec.t[:], in0=pst[:, 0:128], scalar1=cl("eGlm"), scalar2=None,
                                                op0=ALU.mult), reads=[bpst, bsc], writes=[kdec.b])
                    P(lambda e: e.tensor_copy(out=qnf.t[:], in_=qT_[:, sl]), reads=rdq, writes=[qnf.b])
                    relA((psG, bpsG))
                    relT((pst, bpst))
                    yield
                    ((psK, bpsK),) = yield from acquire(1, 0)
                    T(lambda e: e.matmul(psK[:, 0:128], lhsT=kT_[:, sl], rhs=kT_[:, sl], start=True, stop=True),
                      reads=rdk, writes=[bpsK])
                    T(lambda e: e.matmul(psK[:, 128:256], lhsT=kT_[:, sl], rhs=qT_[:, sl], start=True, stop=True),
                      reads=rdk + rdq, writes=[bpsK])
                    A(lambda e: e.activation(out=dec.t[:], in_=dec.t[:], func=AF.Exp, scale=-1.0), reads=[dec.b], writes=[dec.b])
                    A(lambda e: e.activation(out=decT.t[:], in_=decT.t[:], func=AF.Exp), reads=[decT.b], writes=[decT.b])
                    yield
                    Pc, PTc, XTc = I["P"][0], I["PT"][0], I["XT"][0]
                    V(lambda e: e.scalar_tensor_tensor(out=Pc.t[:], in0=psK[:, 0:128], scalar=cl("beta"), in1=dec.t[:],
                                                       op0=ALU.mult, op1=ALU.mult), reads=[bpsK, bsc, dec.b], writes=[Pc.b])
                    V(lambda e: e.tensor_tensor(out=qkT.t[:], in0=psK[:, 128:256], in1=decT.t[:], op=ALU.mult),
                      reads=[bpsK, decT.b], writes=[qkT.b])
                    relA((psK, bpsK))
                    yield
                    ((psX, bpsX),) = yield from acquire(1, 0)
                    T(lambda e: e.matmul(psX[:, 0:128], lhsT=Pc.t[:], rhs=ident_f[:], start=True, stop=True),
                      reads=[Pc.b, B_const], writes=[bpsX])
                    yield
                    V(lambda e: e.tensor_copy(out=PTc.t[:], in_=psX[:, 0:128]), reads=[bpsX], writes=[PTc.b])
                    V(lambda e: e.scalar_tensor_tensor(out=XTc.t[:], in0=psX[:, 0:128], scalar=-1.0, in1=ident_f[:],
                                                       op0=ALU.mult, op1=ALU.add), reads=[bpsX, B_const], writes=[XTc.b])
                    relA((psX, bpsX))
                    yield
                    pend = None
                    for l in range(1, 8):
                        Pp, PTp = I["P"][(l - 1) % 2], I["PT"][(l - 1) % 2]
                        Pn, PTn = I["P"][l % 2], I["PT"][l % 2]
                        got = yield from acquire((1 if l <= 6 else 0) + (1 if l >= 2 else 0), 0)
                        if l <= 6:
                            psS, bpsS = got.pop(0)
                            T(lambda e, psS=psS, Pp=Pp, PTp=PTp: e.matmul(psS[:, 0:128], lhsT=PTp.t[:], rhs=Pp.t[:], start=True, stop=True),
                              reads=[Pp.b, PTp.b], writes=[bpsS])
                            if l < 6:
                                T(lambda e, psS=psS, Pp=Pp, PTp=PTp: e.matmul(psS[:, 128:256], lhsT=Pp.t[:], rhs=PTp.t[:], start=True, stop=True),
                                  reads=[Pp.b, PTp.b], writes=[bpsS])
                        if l >= 2:
                            XTo, XTn = I["XT"][(l - 2) % 2], I["XT"][(l - 1) % 2]
                            psM, bpsM = got.pop(0)
                            T(lambda e, psM=psM, Pp=Pp, XTo=XTo: e.matmul(psM[:, 0:128], lhsT=Pp.t[:], rhs=XTo.t[:], start=True, stop=True),
                              reads=[Pp.b, XTo.b], writes=[bpsM])
                            pend = (psM, bpsM, XTo, XTn)
                        yield
                        if l <= 6:
                            A(lambda e, psS=psS, Pn=Pn: e.copy(out=Pn.t[:], in_=psS[:, 0:128]), reads=[bpsS], writes=[Pn.b])
                            if l < 6:
                                V(lambda e, psS=psS, PTn=PTn: e.tensor_copy(out=PTn.t[:], in_=psS[:, 128:256]), reads=[bpsS], writes=[PTn.b])
                            relA((psS, bpsS))
                        if pend is not None:
                            psM, bpsM, XTo, XTn = pend
                            V(lambda e, psM=psM, XTo=XTo, XTn=XTn: e.tensor_tensor(out=XTn.t[:], in0=XTo.t[:], in1=psM[:, 0:128], op=ALU.add),
                              reads=[bpsM, XTo.b], writes=[XTn.b])
                            relA((psM, bpsM))
                            pend = None
                        yield
                    XTf = I["XT"][0]
                    ((psL, bpsL),) = yield from acquire(1, 0)
                    T(lambda e: e.matmul(psL[:, 0:256], lhsT=XTf.t[:], rhs=rr_.t[:], start=True, stop=True),
                      reads=[XTf.b, rr_.b], writes=[bpsL])
                    yield
                    V(lambda e: e.tensor_copy(out=rr_.t[:, 0:128], in_=psL[:, 0:128]), reads=[bpsL], writes=[rr_.b])
                    A(lambda e: e.copy(out=rr_.t[:, 128:256], in_=psL[:, 128:256]), reads=[bpsL], writes=[rr_.b])
                    relA((psL, bpsL))
                    yield
                    ((psW, bpsW),) = yield from acquire(1, 0)
                    T(lambda e: e.matmul(psW[:, 0:128], lhsT=rr_.t[:, 128:256], rhs=ident_f[:], start=True, stop=True),
                      reads=[rr_.b, B_const], writes=[bpsW])
                    yield
                    A(lambda e: e.copy(out=wT.t[:], in_=psW[:, 0:128]), reads=[bpsW], writes=[wT.b])
                    relA((psW, bpsW))
                    yield

                posts = []

                def post(tt, qi):
                    Q = Q_[qi]
                    blk = tt // 4
                    o, osq, oss, on_, sz, yb = Q["o"], Q["osq"], Q["oss"], Q["on"], Q["sz"], Q["yb"]
                    ((psZ, bpsZ),) = yield from acquire(1, 0)
                    proj_tm(wz, bwz, 0, 128, tt, psZ, bpsZ)
                    yield
                    A(lambda e: e.activation(out=osq.t[:], in_=o.t[:], func=AF.Square, accum_out=oss.t[:]),
                      reads=[o.b], writes=[osq.b, oss.b])
                    A(lambda e: e.activation(out=sz.t[:], in_=psZ[:, 0:128], func=AF.Silu), reads=[bpsZ], writes=[sz.b])
                    relA((psZ, bpsZ))
                    yield
                    V(lambda e: e.tensor_scalar(out=oss.t[:], in0=oss.t[:], scalar1=1.0 / HD, scalar2=EPS, op0=ALU.mult, op1=ALU.add),
                      reads=[oss.b], writes=[oss.b])
                    yield
                    A(lambda e: e.activation(out=oss.t[:], in_=oss.t[:], func=AF.Sqrt), reads=[oss.b], writes=[oss.b])
                    yield
                    V(lambda e: e.reciprocal(out=oss.t[:], in_=oss.t[:]), reads=[oss.b], writes=[oss.b])
                    V(lambda e: e.scalar_tensor_tensor(out=on_.t[:], in0=o.t[:], scalar=oss.t[:, 0:1], in1=smalls[:, 16:144],
                                                       op0=ALU.mult, op1=ALU.mult), reads=[o.b, oss.b, B_const], writes=[on_.b])
                    V(lambda e: e.tensor_tensor(out=yb.t[:], in0=on_.t[:], in1=sz.t[:], op=ALU.mult),
                      reads=[on_.b, sz.b], writes=[yb.b])
                    yield
                    ((pst, bpst),) = yield from acquire(0, 1)
                    T(lambda e: e.transpose(out=pst[:, 0:128], in_=yb.t[:], identity=ident_b[:]), reads=[yb.b, B_const], writes=[bpst])
                    yield
                    ybt = ybT[blk % 2]
                    s_ = tt % 4
                    A(lambda e: e.copy(out=ybt.t[:, s_ * 128:(s_ + 1) * 128], in_=pst[:, 0:128]), reads=[bpst], writes=[ybt.b])
                    relT((pst, bpst))
                    if s_ == 3:
                        ks.dma("pool", lambda e: e.dma_start(
                            out=yT[sq_i, h * 128:(h + 1) * 128, blk * 512:(blk + 1) * 512], in_=ybt.t[:]),
                               reads=[ybt.b], writes=[B_yT])
                    yield

                def chain(tiles, obase):
                    for k_, tt in enumerate(tiles):
                        O = O_[obase + k_]
                        Q = Q_[tt % 2]
                        cl = lambda nm, tt=tt: tl[nm][:, tt, h:h + 1]
                        ((psR, bpsR),) = yield from acquire(1, 0)
                        T(lambda e, psR=psR, O=O: e.matmul(psR[:, 0:128], lhsT=O["wT"].t[:], rhs=Sst.t[:], start=True, stop=True),
                          reads=[O["wT"].b, Sst.b], writes=[bpsR])
                        T(lambda e, psR=psR, O=O: e.matmul(psR[:, 128:256], lhsT=O["qnf"].t[:], rhs=Sst.t[:], start=True, stop=True),
                          reads=[O["qnf"].b, Sst.b], writes=[bpsR])
                        yield
                        V(lambda e, psR=psR, O=O, Q=Q: e.tensor_tensor(out=Q["vnew"].t[:], in0=O["rr"].t[:, 0:128], in1=psR[:, 0:128],
                                                                      op=ALU.subtract), reads=[bpsR, O["rr"].b], writes=[Q["vnew"].b])
                        A(lambda e, psR=psR, Q=Q, cl=cl: e.activation(out=Q["qS"].t[:], in_=psR[:, 128:256], func=AF.Copy, scale=cl("eG")),
                          reads=[bpsR, bsc], writes=[Q["qS"].b])
                        relA((psR, bpsR))
                        yield
                        ((psO, bpsO),) = yield from acquire(1, 0)
                        T(lambda e, psO=psO, O=O, Q=Q: e.matmul(psO[:, 0:128], lhsT=O["qkT"].t[:], rhs=Q["vnew"].t[:], start=True, stop=True),
                          reads=[O["qkT"].b, Q["vnew"].b], writes=[bpsO])
                        T(lambda e, psO=psO, O=O, Q=Q: e.matmul(psO[:, 128:256], lhsT=O["kdec"].t[:], rhs=Q["vnew"].t[:], start=True, stop=True),
                          reads=[O["kdec"].b, Q["vnew"].b], writes=[bpsO])
                        yield
                        V(lambda e, psO=psO, cl=cl: e.scalar_tensor_tensor(out=Sst.t[:], in0=Sst.t[:], scalar=cl("gl"), in1=psO[:, 128:256],
                                                                          op0=ALU.mult, op1=ALU.add), reads=[bpsO, bsc, Sst.b], writes=[Sst.b])
                        V(lambda e, psO=psO, Q=Q: e.tensor_tensor(out=Q["o"].t[:], in0=Q["qS"].t[:], in1=psO[:, 0:128], op=ALU.add),
                          reads=[bpsO, Q["qS"].b], writes=[Q["o"].b])
                        relA((psO, bpsO))
                        posts.append(post(tt, tt % 2))
                        yield

                _stop = 0

                def _lim(g_):
                    n_ = 0
                    for _ in g_:
                        n_ += 1
                        if _stop and n_ >= _stop:
                            return
                        yield

                def run_round(gens):
                    gens = [_lim(g_) for g_ in gens] if _stop else list(gens)
                    while gens or posts:
                        for g_ in list(gens):
                            try:
                                next(g_)
                            except StopIteration:
                                gens.remove(g_)
                        for g_ in list(posts):
                            try:
                                next(g_)
                            except StopIteration:
                                posts.remove(g_)

                NG = NT // G
                for g in range(NG + 1):
                    gens = []
                    if g < NG:
                        gens += [part1(g * G + k_, k_, (g % 2) * G + k_) for k_ in range(G)]
                    if g >= 1 and not _stop:
                        gens.append(chain([(g - 1) * G + k_ for k_ in range(G)], ((g - 1) % 2) * G))
                    run_round(gens)
            ks.fence()

        def gdn_all(sq_i):
            with contextlib.ExitStack() as st:
                set_psum(st, 6, 2)
                tl = gdn_prep(sq_i, st)
                for h in range(4):
                    gdn_head(sq_i, h, tl, dg, b_dg)

        def make_pool():
            freeA = list(zip(PSM["A"], PSM["BA"]))
            freeT = list(zip(PSM["T"], PSM["BT"]))

            def acquire(nA=0, nT=0):
                while len(freeA) < nA or len(freeT) < nT:
                    yield
                ra = [freeA.pop(0) for _ in range(nA)]
                rt = [freeT.pop(0) for _ in range(nT)]
                return ra + rt

            def relA(*bk):
                freeA.extend(bk)

            def relT(*bk):
                freeT.extend(bk)
            return acquire, relA, relT

        def run_staggered(gen_fns, stagger, max_live):
            pending = list(gen_fns)
            live = []
            rnd = 0
            while pending or live:
                if pending and len(live) < max_live and rnd % stagger == 0:
                    live.append(pending.pop(0)())
                for g_ in list(live):
                    try:
                        next(g_)
                    except StopIteration:
                        live.remove(g_)
                rnd += 1

        def rms_gen(xt, bx, grep, bg, junk, bjunk, ss, rstd, hn, bsc, acquire, relT, dst_fn):
            A(lambda e: e.activation(out=junk[:], in_=xt[:], func=AF.Square, accum_out=ss[:]),
              reads=[bx], writes=[bjunk, bsc])
            yield
            V(lambda e: e.tensor_scalar(out=rstd[:], in0=ss[:], scalar1=1.0 / D, scalar2=EPS, op0=ALU.mult, op1=ALU.add),
              reads=[bsc], writes=[bsc])
            yield
            A(lambda e: e.activation(out=rstd[:], in_=rstd[:], func=AF.Sqrt), reads=[bsc], writes=[bsc])
            yield
            V(lambda e: e.reciprocal(out=rstd[:], in_=rstd[:]), reads=[bsc], writes=[bsc])
            V(lambda e: e.scalar_tensor_tensor(out=hn[:], in0=xt[:], scalar=rstd[:, 0:1], in1=grep[:],
                                               op0=ALU.mult, op1=ALU.mult), reads=[bx, bg, bsc], writes=[bsc])
            yield
            ((pst, bpst),) = yield from acquire(0, 1)
            for c in range(8):
                T(lambda e, c=c: e.transpose(out=pst[:, c * 128:(c + 1) * 128], in_=hn[:, c * 128:(c + 1) * 128],
                                             identity=ident_b[:]), reads=[bsc, B_const], writes=[bpst])
            yield
            dst_fn(pst, bpst)
            relT((pst, bpst))
            yield

        def phase_C(sq_i, layer):
            last = (layer == 1)
            xsrc = x_in if layer == 0 else xres
            with contextlib.ExitStack() as st:
                set_psum(st, 6, 2)
                acquire, relA, relT = make_pool()
                wo = SB(st, "c_wo", [128, 8, D], BF16)
                wg = SB(st, "c_wg", [128, 8, D], BF16)
                wp = SB(st, "c_wp", [128, 2, D], BF16)
                gP = SB(st, "c_gP", [128, D], F32)
                gN = SB(st, "c_gN", [128, D], F32)
                bwo, bwg, bwp = Buf(), Buf(), Buf()
                bgP, bgN = Buf(), Buf()
                with contextlib.ExitStack() as st2:
                    stgs = [SB(st2, "c_stg", [128, 8, 512], F32) for _ in range(3)]
                    b_stgs = [Buf(), Buf(), Buf()]
                    k_ = 0
                    for (dst, bdst, src) in ((wo, bwo, w_out[layer]), (wg, bwg, w_gate[layer])):
                        for hh in range(2):
                            stg, b_stg = stgs[k_ % 3], b_stgs[k_ % 3]
                            k_ += 1
                            ks.dma("sp", lambda e, src=src, hh=hh, stg=stg: e.dma_start(
                                out=stg[:], in_=src[:, hh * 512:(hh + 1) * 512].rearrange("(c p) n -> p c n", p=128)),
                                   writes=[b_stg])
                            cast_on(("dve", "act", "pool")[k_ % 3], dst[:, :, hh * 512:(hh + 1) * 512], stg[:], [b_stg], [bdst])
                    for hh in range(2):
                        stg, b_stg = stgs[k_ % 3], b_stgs[k_ % 3]
                        k_ += 1
                        ks.dma("sp", lambda e, hh=hh, stg=stg: e.dma_start(
                            out=stg[:, 0:2, :], in_=w_pp[layer, :, hh * 512:(hh + 1) * 512].rearrange("(c p) n -> p c n", p=128)),
                               writes=[b_stg])
                        P(lambda e, hh=hh, stg=stg: e.tensor_copy(out=wp[:, :, hh * 512:(hh + 1) * 512], in_=stg[:, 0:2, :]),
                          reads=[b_stg], writes=[bwp])
                    ks.dma("sp", lambda e: e.dma_start(out=gP[:], in_=gvec[2 + layer, :, :]), writes=[bgP])
                    ks.dma("sp", lambda e: e.dma_start(out=gN[:], in_=gvec[4 if last else 1, :, :]), writes=[bgN])
                ks.fence()
                NP_ = 3
                mk = lambda nm, shp, dtp: [SB(st, nm, shp, dtp) for _ in range(NP_)]
                xt = mk("c_x", [128, D], F32)
                yl = mk("c_yl", [128, 8, 128], BF16)
                ptf = mk("c_ptf", [128, 2, 128], F32)
                ptb = mk("c_ptb", [128, 2, 128], BF16)
                x1 = mk("c_x1", [128, D], F32)
                hT = mk("c_hT", [128, 8, 128], BF16)
                gt = mk("c_gt", [128, D], F32)
                hn = mk("c_hn", [128, D], BF16)
                ss = mk("c_ss", [128, 1], F32)
                rstd = mk("c_rs", [128, 1], F32)
                ss2 = mk("c_ss2", [128, 1], F32)
                rstd2 = mk("c_rs2", [128, 1], F32)
                mb = lambda: [Buf() for _ in range(NP_)]
                bx, byl, bpt, bx1, bhT, bgt, bsc, bsc2, bhn2 = mb(), mb(), mb(), mb(), mb(), mb(), mb(), mb(), mb()

                def tile_gen(tt):
                    i = tt % NP_
                    tsl = slice(tt * 128, (tt + 1) * 128)
                    ks.dma("sp", lambda e: e.dma_start(out=xt[i][:], in_=xsrc[sq_i, tsl, :]), reads=[B_xres], writes=[bx[i]])
                    ks.dma("sp", lambda e: e.dma_start(out=yl[i][:], in_=yT[sq_i, :, tsl].rearrange("(c p) n -> p c n", p=128)),
                           reads=[B_yT], writes=[byl[i]])
                    ks.dma("sp", lambda e: e.dma_start(out=ptf[i][:], in_=pT_in[layer, sq_i, :, tsl].rearrange("(c p) n -> p c n", p=128)),
                           writes=[bpt[i]])
                    P(lambda e: e.tensor_copy(out=ptb[i][:], in_=ptf[i][:]), reads=[bpt[i]], writes=[bpt[i]])
                    yield
                    bk = yield from acquire(2, 0)
                    for hh in range(2):
                        ps, bps = bk[hh]
                        for c in range(8):
                            T(lambda e, ps=ps, c=c, hh=hh: e.matmul(ps[:, :], lhsT=yl[i][:, c, :], rhs=wo[:, c, hh * 512:(hh + 1) * 512],
                                                                    start=(c == 0), stop=(c == 7)), reads=[byl[i], bwo], writes=[bps])
                    yield
                    for hh in range(2):
                        ps, bps = bk[hh]
                        V(lambda e, ps=ps, hh=hh: e.tensor_tensor(out=x1[i][:, hh * 512:(hh + 1) * 512], in0=ps[:, :],
                                                                  in1=xt[i][:, hh * 512:(hh + 1) * 512], op=ALU.add),
                          reads=[bps, bx[i]], writes=[bx1[i]])
                    relA(*bk)
                    yield

                    def dstg(pst, bpst):
                        A(lambda e: e.copy(out=hT[i][:], in_=pst[:, :].rearrange("p (c n) -> p c n", c=8)),
                          reads=[bpst], writes=[bhT[i]])
                    yield from rms_gen(x1[i], bx1[i], gP, bgP, gt[i], bgt[i], ss[i], rstd[i], hn[i], bsc[i], acquire, relT, dstg)
                    bk = yield from acquire(4, 0)
                    for hh in range(2):
                        ps, bps = bk[hh]
                        for c in range(8):
                            T(lambda e, ps=ps, c=c, hh=hh: e.matmul(ps[:, :], lhsT=hT[i][:, c, :], rhs=wg[:, c, hh * 512:(hh + 1) * 512],
                                                                    start=(c == 0), stop=(c == 7)), reads=[bhT[i], bwg], writes=[bps])
                        ps, bps = bk[2 + hh]
                        for c in range(2):
                            T(lambda e, ps=ps, c=c, hh=hh: e.matmul(ps[:, :], lhsT=ptb[i][:, c, :], rhs=wp[:, c, hh * 512:(hh + 1) * 512],
                                                                    start=(c == 0), stop=(c == 1)), reads=[bpt[i], bwp], writes=[bps])
                    yield
                    for hh in range(2):
                        ps, bps = bk[hh]
                        A(lambda e, ps=ps, hh=hh: e.activation(out=gt[i][:, hh * 512:(hh + 1) * 512], in_=ps[:, :], func=AF.Sigmoid),
                          reads=[bps], writes=[bgt[i]])
                    yield
                    for hh in range(2):
                        ps, bps = bk[2 + hh]
                        V(lambda e, ps=ps, hh=hh: e.tensor_tensor(out=gt[i][:, hh * 512:(hh + 1) * 512],
                                                                  in0=gt[i][:, hh * 512:(hh + 1) * 512], in1=ps[:, :], op=ALU.mult),
                          reads=[bps, bgt[i]], writes=[bgt[i]])
                    relA(*bk)
                    yield
                    P(lambda e: e.tensor_tensor(out=xt[i][:], in0=gt[i][:], in1=x1[i][:], op=ALU.add),
                      reads=[bgt[i], bx1[i]], writes=[bx[i]])
                    yield
                    if not last:
                        ks.dma("pool", lambda e: e.dma_start(out=xres[sq_i, tsl, :], in_=xt[i][:]), reads=[bx[i]], writes=[B_xres])

                        def dstn(pst, bpst):
                            A(lambda e: e.copy(out=hnT[:, :, tt * 128:(tt + 1) * 128],
                                               in_=pst[:, :].rearrange("p (c n) -> p c n", c=8)),
                              reads=[bpst], writes=[B_hnT[tt]])
                        yield from rms_gen(xt[i], bx[i], gN, bgN, gt[i], bgt[i], ss2[i], rstd2[i], hn[i], bsc2[i], acquire, relT, dstn)
                    else:
                        A(lambda e: e.activation(out=gt[i][:], in_=xt[i][:], func=AF.Square, accum_out=ss2[i][:]),
                          reads=[bx[i]], writes=[bgt[i], bsc2[i]])
                        yield
                        V(lambda e: e.tensor_scalar(out=rstd2[i][:], in0=ss2[i][:], scalar1=1.0 / D, scalar2=EPS,
                                                    op0=ALU.mult, op1=ALU.add), reads=[bsc2[i]], writes=[bsc2[i]])
                        yield
                        A(lambda e: e.activation(out=rstd2[i][:], in_=rstd2[i][:], func=AF.Sqrt), reads=[bsc2[i]], writes=[bsc2[i]])
                        yield
                        V(lambda e: e.reciprocal(out=rstd2[i][:], in_=rstd2[i][:]), reads=[bsc2[i]], writes=[bsc2[i]])
                        V(lambda e: e.scalar_tensor_tensor(out=x1[i][:], in0=xt[i][:], scalar=rstd2[i][:, 0:1],
                                                           in1=gN[:], op0=ALU.mult, op1=ALU.mult),
                          reads=[bx[i], bgN, bsc2[i]], writes=[bx1[i]])
                        yield
                        ks.dma("pool", lambda e: e.dma_start(out=out[sq_i, tsl, :], in_=x1[i][:]), reads=[bx1[i]], writes=[B_out])
                        yield

                run_staggered([(lambda tt=tt: tile_gen(tt)) for tt in range(NT)], stagger=5, max_live=NP_)
            ks.fence()

        B_out = Buf()

        def zero_yT(sq_i, r0, r1):
            with contextlib.ExitStack() as st:
                z = SB(st, "zz", [128, S], BF16)
                bz = Buf()
                P(lambda e: e.memset(z[:], 0.0), writes=[bz])
                for r in range(r0, r1, 128):
                    ks.dma("pool", lambda e, r=r: e.dma_start(out=yT[sq_i, r:r + 128, :], in_=z[:]), reads=[bz], writes=[B_yT])
            ks.fence()

        for sq_i in range(NSEQ):
            phase_A(sq_i)
            if do_gdn:
                gdn_all(sq_i)
                ks.fence()
            else:
                zero_yT(sq_i, 0, 512)
            if do_moba:
                for h in range(4):
                    attn_head(sq_i, "moba", h, w_ab,
                              dict(q=B_Q + h * 128, k=B_K + h * 128, v=B_V + h * 128, z=A_Z + 512 + h * 128), 512 + h * 128)
            else:
                zero_yT(sq_i, 512, 1024)
            phase_C(sq_i, 0)
            if do_fox:
                for h in range(8):
                    attn_head(sq_i, "fox", h, w_c,
                              dict(q=C_Q + h * 128, k=C_K + h * 128, v=C_V + h * 128, z=C_Z + h * 128, f=C_F + h), h * 128)
            else:
                zero_yT(sq_i, 0, 1024)
            phase_C(sq_i, 1)
        ks.emit()
    return nc


def prep_inputs(inp, nseq, ncores):
    f = lambda a: np.ascontiguousarray(np.asarray(a, dtype=np.float32))
    x = f(inp["x"])
    p = f(inp["p"])
    gv = np.stack([inp["norm_g"][0], inp["norm_g"][1], inp["ple_norm_g"][0], inp["ple_norm_g"][1], inp["final_g"]], 0)
    gv = f(np.broadcast_to(np.asarray(gv, np.float32)[:, None, :], (5, 128, D)))
    cw = np.asarray(inp["conv_w"], np.float32)[0]
    convw = f(cw.T.reshape(12, 128, 4).transpose(1, 0, 2))
    sv = np.concatenate([np.asarray(inp["a_log"], np.float32)[0], np.asarray(inp["dt_bias"], np.float32)[0],
                         np.asarray(inp["forget_b"], np.float32)[0], np.asarray(inp["gdn_norm_g"], np.float32)[0]])
    smallv = f(np.broadcast_to(sv[None, :], (128, 144)))
    shared = dict(w_in_ab=f(inp["w_in_ab"][0]), w_in_c=f(inp["w_in_c"][0]), w_out_ab=f(inp["w_out_ab"][0]),
                  w_out_c=f(inp["w_out_c"][0]), w_ple_gate=f(inp["w_ple_gate"]), w_ple_proj=f(inp["w_ple_proj"]),
                  gvec=gv, convw=convw, smallv=smallv,
                  wfc=f(np.asarray(inp["w_in_c"], np.float32)[0][:, C_F:C_F + 8].T.reshape(8, 8, 128).transpose(0, 2, 1)))
    maps = []
    for c in range(ncores):
        sl = slice(c * nseq, (c + 1) * nseq)
        m = dict(shared)
        m["x"] = f(x[sl])
        m["pT"] = f(p[:, sl].transpose(0, 1, 3, 2))
        maps.append(m)
    return maps


def kernel(**inputs):
    x = np.asarray(inputs["x"])
    Bsz, S, _ = x.shape
    ncores = 8
    nseq = Bsz // ncores
    nc = build(S, nseq)
    maps = prep_inputs(inputs, nseq, ncores)
    res = run_bass_kernel_spmd(nc, maps, core_ids=list(range(ncores)))
    return np.concatenate([np.asarray(r["out"], dtype=np.float32) for r in res.results], axis=0)
```

```python
import contextlib
import numpy as np
import concourse.bass as bass
import concourse.mybir as mybir
from concourse.bass_utils import run_bass_kernel_spmd

F32 = mybir.dt.float32
BF16 = mybir.dt.bfloat16
AF = mybir.ActivationFunctionType
ALU = mybir.AluOpType
AX = mybir.AxisListType

D = 1024
HD = 128
EPS = 1e-6
BIG = 1.0e30
QSCALE = HD ** -0.5


class Buf:
    __slots__ = ("w", "r", "excl")

    def __init__(self, excl=False):
        self.w = None
        self.r = []
        self.excl = excl


class KS:
    ENGS = ("pe", "dve", "act", "pool", "sp")

    def __init__(self, nc, n_dma_chan=8):
        self.nc = nc
        self.streams = {e: [] for e in self.ENGS}
        self.cnt = {e: 0 for e in self.ENGS}
        self.seen = {e: {} for e in self.ENGS}
        self.nchan = n_dma_chan
        self.chan_cnt = {}
        self.chan_rr = {e: 0 for e in self.ENGS}
        self.sems = {}

    def _deps(self, eng, reads, writes):
        need = {}

        def add(tok):
            if tok is None:
                return
            k, v = tok
            if k == eng and eng == "pe":
                return
            if need.get(k, 0) < v:
                need[k] = v
        for b in reads:
            add(b.w)
            if b.excl:
                for t in b.r:
                    if t[0] != eng:
                        add(t)
        for b in writes:
            add(b.w)
            for t in b.r:
                add(t)
        out = []
        seen = self.seen[eng]
        for k, v in need.items():
            if seen.get(k, 0) >= v:
                continue
            seen[k] = v
            out.append((k, v))
        return out

    def _commit(self, tok, reads, writes):
        for b in reads:
            if len(b.r) > 24:
                m = {}
                for k, v in b.r:
                    if m.get(k, 0) < v:
                        m[k] = v
                b.r = list(m.items())
            b.r.append(tok)
        for b in writes:
            b.w = tok
            b.r = []

    def op(self, eng, fn, reads=(), writes=()):
        waits = self._deps(eng, reads, writes)
        self.cnt[eng] += 1
        tok = (eng, self.cnt[eng])
        self.streams[eng].append((waits, fn, (eng, 1)))
        self._commit(tok, reads, writes)
        return tok

    def dma(self, eng, fn, reads=(), writes=()):
        c = self.chan_rr[eng]
        self.chan_rr[eng] = (c + 1) % self.nchan
        key = ("dma", eng, c)
        prev = self.chan_cnt.get(key, 0)
        waits = self._deps(eng, reads, writes)
        if prev and self.seen[eng].get(key, 0) < prev:
            self.seen[eng][key] = prev
            waits.append((key, prev))
        self.chan_cnt[key] = prev + 16
        tok = (key, prev + 16)
        self.streams[eng].append((waits, fn, (key, 16)))
        self._commit(tok, reads, writes)
        return tok

    def fence(self):
        cur = dict(self.cnt)
        cur.update(self.chan_cnt)
        for e in self.ENGS:
            waits = []
            for k, v in cur.items():
                if v <= 0 or (k == e):
                    continue
                if self.seen[e].get(k, 0) < v:
                    self.seen[e][k] = v
                    waits.append((k, v))
            if waits:
                self.streams[e].append((waits, None, None))

    def emit(self):
        nc = self.nc
        keys = list(self.ENGS) + sorted(self.chan_cnt.keys(), key=str)
        with contextlib.ExitStack() as st:
            for k in keys:
                nm = "s_" + ("_".join(map(str, k)) if isinstance(k, tuple) else k)
                self.sems[k] = st.enter_context(nc.semaphore(nm))
            block = st.enter_context(nc.Block())
            finals = {k: v for k, v in self.chan_cnt.items()}
            finals.update({e: v for e, v in self.cnt.items() if v > 0})

            def run(eng, h):
                for waits, fn, inc in self.streams[eng]:
                    for k, v in waits:
                        h.wait_ge(self.sems[k], v)
                    if fn is not None:
                        fn(h).then_inc(self.sems[inc[0]], inc[1])
                if eng == "sp":
                    for k, v in finals.items():
                        if k != "sp":
                            h.wait_ge(self.sems[k], v)

            @block.tensor
            def _(h):
                run("pe", h)

            @block.vector
            def _(h):
                run("dve", h)

            @block.scalar
            def _(h):
                run("act", h)

            @block.gpsimd
            def _(h):
                run("pool", h)

            @block.sync
            def _(h):
                run("sp", h)


A_Q, A_K, A_V = 0, 512, 1024
A_A, A_B = 1536, 1540
B_Q, B_K, B_V = 1544, 2056, 2568
A_Z = 3080
C_Q, C_K, C_V, C_F, C_Z = 0, 1024, 2048, 3072, 3080
IN_W = 4104


def build(S, NSEQ, do_gdn=True, do_moba=True, do_fox=True):
    NT = S // 128
    NBQ = S // 512
    NB = S // 256
    nc = bass.Bass("TRN2", target_bir_lowering=False)
    dt = nc.dram_tensor
    x_in = dt("x", [NSEQ, S, D], F32, kind="ExternalInput").ap()
    pT_in = dt("pT", [2, NSEQ, 256, S], F32, kind="ExternalInput").ap()
    w_ab = dt("w_in_ab", [D, IN_W], F32, kind="ExternalInput").ap()
    w_c = dt("w_in_c", [D, IN_W], F32, kind="ExternalInput").ap()
    w_out = [dt("w_out_ab", [D, D], F32, kind="ExternalInput").ap(),
             dt("w_out_c", [D, D], F32, kind="ExternalInput").ap()]
    w_gate = dt("w_ple_gate", [2, D, D], F32, kind="ExternalInput").ap()
    w_pp = dt("w_ple_proj", [2, 256, D], F32, kind="ExternalInput").ap()
    gvec = dt("gvec", [5, 128, D], F32, kind="ExternalInput").ap()
    convw = dt("convw", [128, 12, 4], F32, kind="ExternalInput").ap()
    smallv = dt("smallv", [128, 144], F32, kind="ExternalInput").ap()
    wfc = dt("wfc", [8, 128, 8], F32, kind="ExternalInput").ap()
    out = dt("out", [NSEQ, S, D], F32, kind="ExternalOutput").ap()
    xres = dt("xres", [NSEQ, S, D], F32, kind="Internal").ap()
    yT = dt("yT", [NSEQ, D, S], BF16, kind="Internal").ap()

    ks = KS(nc)
    slopes = [2.0 ** (-8.0 * (h + 1) / 4) for h in range(4)]

    with contextlib.ExitStack() as top:
        uid = [0]

        def SB(st, name, shape, dtype):
            uid[0] += 1
            return st.enter_context(nc.sbuf_tensor("%s_%d" % (name, uid[0]), shape, dtype))

        def PS(st, name, shape, dtype):
            uid[0] += 1
            return st.enter_context(nc.psum_tensor("%s_%d" % (name, uid[0]), shape, dtype))

        hnT = SB(top, "hnT", [128, 8, S], BF16)
        B_hnT = [Buf() for _ in range(NT)]
        ident_f = SB(top, "ident_f", [128, 128], F32)
        ident_b = SB(top, "ident_b", [128, 128], BF16)
        ones_f = SB(top, "ones_f", [128, 128], F32)
        ones_b = SB(top, "ones_b", [128, 128], BF16)
        maskT_b = SB(top, "maskT_b", [128, 128], BF16)
        maskA = SB(top, "maskA", [128, 128], F32)
        maskQ = SB(top, "maskQ", [128, 128], F32)
        U_f = SB(top, "U_f", [128, 128], F32)
        e0col = SB(top, "e0col", [128, 2], F32)
        iq = SB(top, "iq", [128, 512], F32)
        ikcol = SB(top, "ikcol", [128, 1], F32)
        tmpf = SB(top, "tmpf", [128, 128], F32)
        smalls = SB(top, "smalls", [128, 144], F32)
        zero_c = SB(top, "zero_c", [128, 1], F32)
        B_const = Buf()

        PSM = {"A": [], "BA": [], "T": [], "BT": []}
        rr = {"A": 0, "T": 0}

        def set_psum(st, nA, nT):
            PSM["A"] = [PS(st, "psA", [128, 512], F32) for _ in range(nA)]
            PSM["BA"] = [Buf(True) for _ in range(nA)]
            PSM["T"] = [PS(st, "psT", [128, 1024], BF16) for _ in range(nT)]
            PSM["BT"] = [Buf(True) for _ in range(nT)]
            rr["A"] = 0
            rr["T"] = 0

        def nextA():
            i = rr["A"]
            rr["A"] = (i + 1) % len(PSM["A"])
            return PSM["A"][i], PSM["BA"][i]

        def nextT():
            i = rr["T"]
            rr["T"] = (i + 1) % len(PSM["T"])
            return PSM["T"][i], PSM["BT"][i]

        P = lambda fn, **kw: ks.op("pool", fn, **kw)
        V = lambda fn, **kw: ks.op("dve", fn, **kw)
        A = lambda fn, **kw: ks.op("act", fn, **kw)
        T = lambda fn, **kw: ks.op("pe", fn, **kw)

        C = [B_const]
        P(lambda e: e.memset(ident_f[:], 0.0), writes=C)
        P(lambda e: e.affine_select(out=ident_f[:], in_=ident_f[:], pattern=[[-1, 128]], compare_op=ALU.not_equal,
                                    fill=1.0, base=0, channel_multiplier=1), reads=C, writes=C)
        P(lambda e: e.tensor_copy(out=ident_b[:], in_=ident_f[:]), reads=C, writes=C)
        P(lambda e: e.memset(ones_f[:], 1.0), writes=C)
        P(lambda e: e.memset(ones_b[:], 1.0), writes=C)
        P(lambda e: e.memset(zero_c[:], 0.0), writes=C)
        P(lambda e: e.memset(maskQ[:], 0.0), writes=C)
        P(lambda e: e.affine_select(out=maskQ[:], in_=maskQ[:], pattern=[[1, 128]], compare_op=ALU.is_ge,
                                    fill=BIG, base=0, channel_multiplier=-1), reads=C, writes=C)
        P(lambda e: e.tensor_scalar(out=maskT_b[:], in0=maskQ[:], scalar1=-1.0, scalar2=None, op0=ALU.mult),
          reads=C, writes=C)
        P(lambda e: e.memset(maskA[:], 0.0), writes=C)
        P(lambda e: e.affine_select(out=maskA[:], in_=maskA[:], pattern=[[-1, 128]], compare_op=ALU.is_gt,
                                    fill=BIG, base=0, channel_multiplier=1), reads=C, writes=C)
        P(lambda e: e.memset(U_f[:], 1.0), writes=C)
        P(lambda e: e.affine_select(out=U_f[:], in_=U_f[:], pattern=[[1, 128]], compare_op=ALU.is_ge,
                                    fill=0.0, base=0, channel_multiplier=-1), reads=C, writes=C)
        P(lambda e: e.memset(e0col[:], 0.0), writes=C)
        P(lambda e: e.affine_select(out=e0col[:], in_=e0col[:], pattern=[[0, 2]], compare_op=ALU.not_equal,
                                    fill=1.0, base=0, channel_multiplier=1), reads=C, writes=C)
        P(lambda e: e.iota(iq[:], pattern=[[1, 512]], base=0, channel_multiplier=0,
                           allow_small_or_imprecise_dtypes=True), writes=C)
        P(lambda e: e.iota(ikcol[:], pattern=[[0, 1]], base=0, channel_multiplier=1,
                           allow_small_or_imprecise_dtypes=True), writes=C)
        ks.dma("sp", lambda e: e.dma_start(out=smalls[:], in_=smallv[:, :]), writes=C)

        def cast_on(ceng, out_ap, in_ap, reads, writes):
            if ceng == "act":
                A(lambda e: e.copy(out=out_ap, in_=in_ap), reads=reads, writes=writes)
            elif ceng == "dve":
                V(lambda e: e.tensor_copy(out=out_ap, in_=in_ap), reads=reads, writes=writes)
            else:
                P(lambda e: e.tensor_copy(out=out_ap, in_=in_ap), reads=reads, writes=writes)

        def load_w(st, name, src_ap, ncols, eng="sp", kchunks=8, ceng="pool"):
            stg = SB(st, name + "_f", [128, kchunks, ncols], F32)
            wb = SB(st, name + "_b", [128, kchunks, ncols], BF16)
            bs, bw = Buf(), Buf()
            ks.dma(eng, lambda e: e.dma_start(out=stg[:], in_=src_ap.rearrange("(c p) n -> p c n", p=128)), writes=[bs])
            cast_on(ceng, wb[:], stg[:], [bs], [bw])
            return wb, bw

        def rms_to_T(xt, bx, grep, bg, sc, dstT_fn, dst_bufs, extra_reads=()):
            sq, ss, rstd, hn = sc["sq"], sc["ss"], sc["rstd"], sc["hn"]
            bsc = sc["buf"]
            A(lambda e: e.activation(out=sq[:], in_=xt[:], func=AF.Square, accum_out=ss[:]),
              reads=[bx], writes=[bsc])
            V(lambda e: e.tensor_scalar(out=rstd[:], in0=ss[:], scalar1=1.0 / D, scalar2=EPS, op0=ALU.mult, op1=ALU.add),
              reads=[bsc], writes=[bsc])
            A(lambda e: e.activation(out=rstd[:], in_=rstd[:], func=AF.Sqrt), reads=[bsc], writes=[bsc])
            V(lambda e: e.reciprocal(out=rstd[:], in_=rstd[:]), reads=[bsc], writes=[bsc])
            V(lambda e: e.scalar_tensor_tensor(out=hn[:], in0=xt[:], scalar=rstd[:, 0:1], in1=grep[:],
                                               op0=ALU.mult, op1=ALU.mult), reads=[bx, bg, bsc], writes=[bsc])
            pst, bpst = nextT()
            for c in range(8):
                T(lambda e, c=c: e.transpose(out=pst[:, c * 128:(c + 1) * 128], in_=hn[:, c * 128:(c + 1) * 128],
                                             identity=ident_b[:]), reads=[bsc, B_const], writes=[bpst])
            dstT_fn(pst, bpst)

        def proj_fm(wb, bw, col0, blk, ps, bps, ncols=128):
            rd = [bw] + B_hnT[blk * 4:(blk + 1) * 4]
            for c in range(8):
                T(lambda e, c=c: e.matmul(ps[0:ncols, :], lhsT=wb[:, c, col0:col0 + ncols],
                                          rhs=hnT[:, c, blk * 512:(blk + 1) * 512], start=(c == 0), stop=(c == 7)),
                  reads=rd, writes=[bps])

        def proj_tm(wb, bw, col0, ncols, tt, ps, bps, pcol0=0):
            rd = [bw, B_hnT[tt]]
            for c in range(8):
                T(lambda e, c=c: e.matmul(ps[:, pcol0:pcol0 + ncols], lhsT=hnT[:, c, tt * 128:(tt + 1) * 128],
                                          rhs=wb[:, c, col0:col0 + ncols], start=(c == 0), stop=(c == 7)),
                  reads=rd, writes=[bps])

        cw = SB(top, "cw", [128, 12, 4], F32)
        dg = SB(top, "dg", [128, 48, 128], BF16)
        b_dg = Buf()
        ks.dma("sp", lambda e: e.dma_start(out=cw[:], in_=convw[:, :, :]), writes=[b_dg])
        for c in range(12):
            for j in range(4):
                cast_eng = (P, V)[(c * 4 + j) % 2]
                cast_eng(lambda e, c=c, j=j: e.tensor_scalar(out=dg[:, c * 4 + j, :], in0=ident_f[:], scalar1=cw[:, c, j:j + 1],
                                                             scalar2=None, op0=ALU.mult), reads=[b_dg, B_const], writes=[b_dg])

        def phase_A(sq_i):
            with contextlib.ExitStack() as st:
                set_psum(st, 0, 4)
                acquire, relA, relT = make_pool()
                grep = SB(st, "gA", [128, D], F32)
                bg = Buf()
                ks.dma("sp", lambda e: e.dma_start(out=grep[:], in_=gvec[0, :, :]), writes=[bg])
                NPA = 4
                xts = [SB(st, "xtA", [128, D], F32) for i in range(NPA)]
                sqs = [SB(st, "sqA", [128, D], F32) for i in range(NPA)]
                sss = [SB(st, "ssA", [128, 1], F32) for i in range(NPA)]
                rss = [SB(st, "rsA", [128, 1], F32) for i in range(NPA)]
                hns = [SB(st, "hnA", [128, D], BF16) for i in range(NPA)]
                bxs = [Buf() for _ in range(NPA)]
                bsq = [Buf() for _ in range(NPA)]
                bsc = [Buf() for _ in range(NPA)]

                def tile_gen(tt):
                    i = tt % NPA
                    ks.dma("sp", lambda e: e.dma_start(out=xts[i][:], in_=x_in[sq_i, tt * 128:(tt + 1) * 128, :]),
                           writes=[bxs[i]])
                    yield

                    def dst(pst, bpst):
                        A(lambda e: e.copy(out=hnT[:, :, tt * 128:(tt + 1) * 128],
                                           in_=pst[:, :].rearrange("p (c n) -> p c n", c=8)),
                          reads=[bpst], writes=[B_hnT[tt]])
                    yield from rms_gen(xts[i], bxs[i], grep, bg, sqs[i], bsq[i], sss[i], rss[i], hns[i], bsc[i],
                                       acquire, relT, dst)

                run_staggered([(lambda tt=tt: tile_gen(tt)) for tt in range(NT)], stagger=2, max_live=NPA)
            ks.fence()

        def attn_head(sq_i, mode, h, wsrc, cols, yrow0):
            with contextlib.ExitStack() as st:
                set_psum(st, 3, 1)
                pso = [PS(st, "pso", [128, 512], F32) for _ in range(4)]
                bpso = [Buf(True) for _ in range(4)]
                wq, bwq = load_w(st, "wq", wsrc[:, cols["q"]:cols["q"] + 128], 128, ceng="dve")
                wk, bwk = load_w(st, "wk", wsrc[:, cols["k"]:cols["k"] + 128], 128, eng="pool", ceng="act")
                wv, bwv = load_w(st, "wv", wsrc[:, cols["v"]:cols["v"] + 128], 128, ceng="pool")
                wz, bwz = load_w(st, "wz", wsrc[:, cols["z"]:cols["z"] + 128], 128, eng="pool", ceng="dve")
                qT = SB(st, "qT", [128, S], BF16)
                kT = SB(st, "kT", [128, S], BF16)
                szT = SB(st, "szT", [128, S], BF16)
                vtm = SB(st, "vtm", [128, NT, 130], BF16)
                b_q = [Buf() for _ in range(NBQ)]
                b_k = [Buf() for _ in range(NBQ)]
                b_z = [Buf() for _ in range(NBQ)]
                b_v = [Buf() for _ in range(NT)]
                b_vones = Buf()
                P(lambda e: e.memset(vtm[:, :, 128:130], 1.0), writes=[b_vones])
                for blk in range(NBQ):
                    ps, bps = nextA()
                    proj_fm(wq, bwq, 0, blk, ps, bps)
                    A(lambda e, ps=ps, blk=blk: e.activation(out=qT[:, blk * 512:(blk + 1) * 512], in_=ps[:, :],
                                                             func=AF.Copy, scale=QSCALE), reads=[bps], writes=[b_q[blk]])
                    ps, bps = nextA()
                    proj_fm(wk, bwk, 0, blk, ps, bps)
                    V(lambda e, ps=ps, blk=blk: e.tensor_copy(out=kT[:, blk * 512:(blk + 1) * 512], in_=ps[:, :]),
                      reads=[bps], writes=[b_k[blk]])
                    ps, bps = nextA()
                    proj_fm(wz, bwz, 0, blk, ps, bps)
                    A(lambda e, ps=ps, blk=blk: e.activation(out=szT[:, blk * 512:(blk + 1) * 512], in_=ps[:, :],
                                                             func=AF.Silu), reads=[bps], writes=[b_z[blk]])
                    ps, bps = nextA()
                    for s in range(4):
                        proj_tm(wv, bwv, 0, 128, blk * 4 + s, ps, bps, pcol0=s * 128)
                    V(lambda e, ps=ps, blk=blk: e.tensor_copy(
                        out=vtm[:, blk * 4:(blk + 1) * 4, 0:128], in_=ps[:, :].rearrange("p (s n) -> p s n", s=4)),
                      reads=[bps], writes=b_v[blk * 4:(blk + 1) * 4])

                if mode == "fox":
                    wf_s = SB(st, "wf_s", [128, 8, 1], F32)
                    wf_b = SB(st, "wf_b", [128, 8, 128], BF16)
                    bwf = Buf()
                    ks.dma("sp", lambda e: e.dma_start(out=wf_s[:, :, 0], in_=wfc[h, :, :]), writes=[bwf])
                    P(lambda e: e.tensor_copy(out=wf_b[:], in_=wf_s[:].to_broadcast([128, 8, 128])),
                      reads=[bwf], writes=[bwf])
                    crep = SB(st, "crep", [128, S], F32)
                    ltmp = SB(st, "ltmp", [128, 512], F32)
                    negb = SB(st, "negb", [128, 1], F32)
                    b_c = [Buf() for _ in range(NBQ)]
                    b_l = Buf()
                    V(lambda e: e.tensor_scalar(out=negb[:], in0=smalls[:, 8 + h:9 + h], scalar1=-1.0, scalar2=None,
                                                op0=ALU.mult), reads=[B_const], writes=[b_l])
                    for blk in range(NBQ):
                        ps, bps = nextA()
                        proj_fm(wf_b, bwf, 0, blk, ps, bps)
                        A(lambda e, ps=ps: e.activation(out=ltmp[:], in_=ps[:, :], func=AF.Exp, bias=negb[:, 0:1], scale=-1.0),
                          reads=[bps, b_l], writes=[b_l])
                        A(lambda e: e.activation(out=ltmp[:], in_=ltmp[:], func=AF.Ln, bias=1.0, scale=1.0),
                          reads=[b_l], writes=[b_l])
                        init = zero_c[:, 0:1] if blk == 0 else crep[:, blk * 512 - 1:blk * 512]
                        V(lambda e, blk=blk, init=init: e.tensor_tensor_scan(
                            out=crep[:, blk * 512:(blk + 1) * 512], data0=ones_f[:, 0:1].to_broadcast([128, 512]),
                            data1=ltmp[:], initial=init, op0=ALU.mult, op1=ALU.add),
                          reads=[b_l, B_const] + ([b_c[blk - 1]] if blk else []), writes=[b_c[blk]])
                    ckcol = SB(st, "ckcol", [128, NT], F32)
                    b_ck = Buf()
                    ps, bps = nextA()
                    for j in range(NT):
                        T(lambda e, j=j, ps=ps: e.matmul(ps[:, 2 * j:2 * j + 2], lhsT=crep[:, j * 128:(j + 1) * 128],
                                                         rhs=e0col[:, 0:2], start=True, stop=True),
                          reads=[b_c[j // 4], B_const], writes=[bps])
                    V(lambda e, ps=ps: e.tensor_copy(out=ckcol[:], in_=ps[:, 0:2 * NT].rearrange("p (j t) -> p j t", t=2)[:, :, 0]),
                      reads=[bps], writes=[b_ck])
                else:
                    slope = slopes[h]
                    nd = NT + 4
                    kbt = SB(st, "kbt", [128, nd], F32)
                    b_kb = Buf()
                    for m in range(nd):
                        V(lambda e, m=m: e.tensor_scalar(out=kbt[:, m:m + 1], in0=ikcol[:], scalar1=slope,
                                                         scalar2=-slope * 128.0 * (m - 3), op0=ALU.mult, op1=ALU.add),
                          reads=[B_const], writes=[b_kb])
                    kms = SB(st, "kms", [128, NB], F32)
                    kmb = SB(st, "kmb", [128, NB], BF16)
                    b_km = Buf()
                    V(lambda e: e.tensor_reduce(out=kms[:], in_=kT[:].rearrange("p (n l) -> p n l", l=256), axis=AX.X,
                                                op=ALU.add), reads=b_k, writes=[b_km])
                    V(lambda e: e.tensor_scalar(out=kmb[:], in0=kms[:], scalar1=1.0 / 256, scalar2=None, op0=ALU.mult),
                      reads=[b_km], writes=[b_km])
                    past01 = SB(st, "past01", [128, NB, NB], F32)
                    pastneg = SB(st, "pastneg", [128, NB, NB], F32)
                    ownb = SB(st, "ownb", [128, NB, NB], F32)
                    b_tab = Buf()
                    P(lambda e: e.memset(past01[:], 1.0), writes=[b_tab])
                    P(lambda e: e.affine_select(out=past01[:], in_=past01[:], pattern=[[1, NB], [-1, NB]],
                                                compare_op=ALU.is_gt, fill=0.0, base=0, channel_multiplier=0),
                      reads=[b_tab], writes=[b_tab])
                    P(lambda e: e.tensor_scalar(out=pastneg[:], in0=past01[:], scalar1=-1.0, scalar2=BIG,
                                                op0=ALU.add, op1=ALU.mult), reads=[b_tab], writes=[b_tab])
                    P(lambda e: e.memset(ownb[:], 0.0), writes=[b_tab])
                    P(lambda e: e.affine_select(out=ownb[:], in_=ownb[:], pattern=[[1, NB], [-1, NB]],
                                                compare_op=ALU.is_equal, fill=-BIG, base=0, channel_multiplier=0),
                      reads=[b_tab], writes=[b_tab])
                    E_f = SB(st, "E_f", [128, NB, 128], F32)
                    E_b = SB(st, "E_b", [128, NB, 128], BF16)
                    P(lambda e: e.memset(E_f[:], 1.0), writes=[b_tab])
                    P(lambda e: e.affine_select(out=E_f[:], in_=E_f[:], pattern=[[-1, NB], [0, 128]],
                                                compare_op=ALU.is_equal, fill=0.0, base=0, channel_multiplier=1),
                      reads=[b_tab], writes=[b_tab])
                    P(lambda e: e.tensor_copy(out=E_b[:], in_=E_f[:]), reads=[b_tab], writes=[b_tab])
                    selT = SB(st, "selT", [128, S], BF16)
                    b_sel = [Buf() for _ in range(NBQ)]
                    gm = SB(st, "gm", [128, NB], F32)
                    top8 = SB(st, "top8", [128, 8], F32)
                    t2 = SB(st, "t2", [128, NB], F32)
                    seln = SB(st, "seln", [128, NB], BF16)
                    b_g = Buf()
                    for blk in range(NBQ):
                        pst, bpst = nextT()
                        for s in range(4):
                            tt = blk * 4 + s
                            own = tt // 2
                            ps, bps = nextA()
                            T(lambda e, ps=ps, tt=tt: e.matmul(ps[:, 0:NB], lhsT=qT[:, tt * 128:(tt + 1) * 128], rhs=kmb[:, :],
                                                               start=True, stop=True), reads=[b_q[blk], b_km], writes=[bps])
                            V(lambda e, ps=ps, own=own: e.tensor_tensor(out=gm[:], in0=ps[:, 0:NB], in1=pastneg[:, own, :],
                                                                        op=ALU.add), reads=[bps, b_tab], writes=[b_g])
                            V(lambda e: e.max(out=top8[:], in_=gm[:]), reads=[b_g], writes=[b_g])
                            V(lambda e, own=own: e.scalar_tensor_tensor(out=t2[:], in0=gm[:], scalar=top8[:, 2:3],
                                                                        in1=past01[:, own, :], op0=ALU.is_ge, op1=ALU.mult),
                              reads=[b_g, b_tab], writes=[b_g])
                            V(lambda e, own=own: e.scalar_tensor_tensor(out=seln[:], in0=t2[:], scalar=BIG,
                                                                        in1=ownb[:, own, :], op0=ALU.mult, op1=ALU.add),
                              reads=[b_g, b_tab], writes=[b_g])
                            T(lambda e, pst=pst, s=s: e.transpose(out=pst[0:NB, s * 128:(s + 1) * 128], in_=seln[:, :],
                                                                  identity=ident_b[:]), reads=[b_g, B_const], writes=[bpst])
                        V(lambda e, pst=pst, blk=blk: e.tensor_copy(out=selT[0:NB, blk * 512:(blk + 1) * 512],
                                                                    in_=pst[0:NB, 0:512]), reads=[bpst], writes=[b_sel[blk]])

                LA = 2
                NBUF = LA + 1
                tmps = [SB(st, "atmp", [128, 512], F32) for i in range(NBUF)]
                pTs = [SB(st, "apT", [128, 512], BF16) for i in range(NBUF)]
                b_tmp = [Buf() for _ in range(NBUF)]
                b_pT = [Buf() for _ in range(NBUF)]
                on = [SB(st, "aon", [128, 128], BF16) for i in range(2)]
                rden = [SB(st, "ard", [128, 1], F32) for i in range(2)]
                b_on = [Buf(), Buf()]
                yblk = [SB(st, "ayb", [128, 512], BF16) for i in range(2)]
                b_yb = [Buf(), Buf()]
                its = [(I, j) for I in range(NBQ) for j in range(4 * I + 4)]

                def stage1(n):
                    I, j = its[n]
                    off = max(0, j - 4 * I)
                    c0 = off * 128
                    ps, bps = nextA()
                    diag = j >= 4 * I
                    T(lambda e: e.matmul(ps[:, c0:512], lhsT=kT[:, j * 128:(j + 1) * 128],
                                         rhs=qT[:, I * 512 + c0:(I + 1) * 512],
                                         start=True, stop=False, skip_group_check=True),
                      reads=[b_k[j // 4], b_q[I]], writes=[bps])
                    if diag:
                        T(lambda e: e.matmul(ps[:, c0:c0 + 128], lhsT=ident_b[:], rhs=maskT_b[:],
                                             start=False, stop=False, skip_group_check=True),
                          reads=[B_const], writes=[bps])
                    if mode == "moba":
                        T(lambda e: e.matmul(ps[:, c0:512], lhsT=E_b[0:NB, j // 2, :],
                                             rhs=selT[0:NB, I * 512 + c0:(I + 1) * 512],
                                             start=False, stop=True, skip_group_check=True),
                          reads=[b_tab, b_sel[I]], writes=[bps])
                    tmp, btm = tmps[n % NBUF], b_tmp[n % NBUF]
                    pT, bpT = pTs[n % NBUF], b_pT[n % NBUF]
                    if mode == "fox":
                        V(lambda e: e.tensor_tensor(out=tmp[:, c0:512], in0=ps[:, c0:512],
                                                    in1=crep[:, I * 512 + c0:(I + 1) * 512], op=ALU.subtract),
                          reads=[bps, b_c[I]], writes=[btm])
                        A(lambda e: e.activation(out=pT[:, c0:512], in_=tmp[:, c0:512], func=AF.Exp,
                                                 bias=ckcol[:, j:j + 1], scale=1.0),
                          reads=[btm, b_ck], writes=[bpT])
                    else:
                        m = (I * 4 - j) + 3
                        V(lambda e: e.scalar_tensor_tensor(out=tmp[:, c0:512], in0=iq[:, c0:512], scalar=-slope,
                                                           in1=ps[:, c0:512], op0=ALU.mult, op1=ALU.add),
                          reads=[bps, B_const], writes=[btm])
                        A(lambda e: e.activation(out=pT[:, c0:512], in_=tmp[:, c0:512], func=AF.Exp,
                                                 bias=kbt[:, m:m + 1], scale=1.0),
                          reads=[btm, b_kb], writes=[bpT])

                def stage2(n):
                    I, j = its[n]
                    off = max(0, j - 4 * I)
                    pT, bpT = pTs[n % NBUF], b_pT[n % NBUF]
                    pb = 2 * (I % 2)
                    for s in range(off, 4):
                        bank = pb + s // 2
                        oc = (s % 2) * 256
                        first = (j == 0 and s % 2 == 0)
                        T(lambda e, s=s, bank=bank, oc=oc, first=first: e.matmul(
                            pso[bank][:, oc:oc + 129], lhsT=pT[:, s * 128:(s + 1) * 128], rhs=vtm[:, j, 0:129],
                            start=first, stop=False, skip_group_check=True),
                          reads=[bpT, b_v[j], b_vones], writes=[bpso[bank]])
                    if j != 4 * I + 3:
                        return
                    yb, byb = yblk[I % 2], b_yb[I % 2]
                    for s in range(4):
                        bank = pb + s // 2
                        oc = (s % 2) * 256
                        o_n, rd_, bo = on[s % 2], rden[s % 2], b_on[s % 2]
                        V(lambda e, bank=bank, oc=oc, rd_=rd_: e.reciprocal(out=rd_[:], in_=pso[bank][:, oc + 128:oc + 129]),
                          reads=[bpso[bank]], writes=[bo])
                        V(lambda e, bank=bank, oc=oc, rd_=rd_, o_n=o_n: e.tensor_scalar(
                            out=o_n[:], in0=pso[bank][:, oc:oc + 128], scalar1=rd_[:, 0:1], scalar2=None, op0=ALU.mult),
                          reads=[bpso[bank], bo], writes=[bo])
                        pst, bpst = nextT()
                        T(lambda e, pst=pst, o_n=o_n: e.transpose(out=pst[:, 0:128], in_=o_n[:], identity=ident_b[:]),
                          reads=[bo, B_const], writes=[bpst])
                        V(lambda e, pst=pst, s=s: e.tensor_tensor(
                            out=yb[:, s * 128:(s + 1) * 128], in0=pst[:, 0:128],
                            in1=szT[:, I * 512 + s * 128:I * 512 + (s + 1) * 128], op=ALU.mult),
                          reads=[bpst, b_z[I]], writes=[byb])
                    ks.dma("pool", lambda e: e.dma_start(
                        out=yT[sq_i, yrow0:yrow0 + 128, I * 512:(I + 1) * 512], in_=yb[:]),
                           reads=[byb], writes=[B_yT])

                N_it = len(its)
                for n in range(N_it + LA):
                    if n < N_it:
                        stage1(n)
                    if n - LA >= 0:
                        stage2(n - LA)
            ks.fence()

        B_yT = Buf()
        B_xres = Buf()

        def gdn_prep(sq_i, st):
            wab, bwab = load_w(st, "wab", w_ab[:, A_A:A_A + 8], 8)
            names = ["g", "beta", "G", "eG", "eGlm", "gl", "bG"]
            tl = {n: SB(st, "gp_" + n, [128, NT, 4], F32) for n in names}
            b = Buf()
            negA = SB(st, "negA", [128, 4], F32)
            tl["negA"] = negA
            A(lambda e: e.activation(out=negA[:], in_=smalls[:, 0:4], func=AF.Exp), reads=[B_const], writes=[b])
            V(lambda e: e.tensor_scalar(out=negA[:], in0=negA[:], scalar1=-1.0, scalar2=None, op0=ALU.mult),
              reads=[b], writes=[b])
            t4 = SB(st, "gp_t4", [128, 4], F32)
            for tt in range(NT):
                ps, bps = nextA()
                proj_tm(wab, bwab, 0, 8, tt, ps, bps)
                V(lambda e, ps=ps: e.tensor_tensor(out=t4[:], in0=ps[:, 0:4], in1=smalls[:, 4:8], op=ALU.add),
                  reads=[bps, B_const], writes=[b])
                A(lambda e: e.activation(out=t4[:], in_=t4[:], func=AF.Exp), reads=[b], writes=[b])
                A(lambda e: e.activation(out=t4[:], in_=t4[:], func=AF.Ln, bias=1.0, scale=1.0), reads=[b], writes=[b])
                V(lambda e, tt=tt: e.tensor_tensor(out=tl["g"][:, tt, :], in0=t4[:], in1=negA[:], op=ALU.mult),
                  reads=[b], writes=[b])
                A(lambda e, ps=ps, tt=tt: e.activation(out=tl["beta"][:, tt, :], in_=ps[:, 4:8], func=AF.Sigmoid),
                  reads=[bps], writes=[b])
                ps2, bps2 = nextA()
                T(lambda e, ps2=ps2, tt=tt: e.matmul(ps2[:, 0:4], lhsT=U_f[:], rhs=tl["g"][:, tt, :], start=True, stop=True),
                  reads=[b, B_const], writes=[bps2])
                T(lambda e, ps2=ps2, tt=tt: e.matmul(ps2[:, 8:12], lhsT=ones_f[:], rhs=tl["g"][:, tt, :], start=True, stop=True),
                  reads=[b, B_const], writes=[bps2])
                V(lambda e, ps2=ps2, tt=tt: e.tensor_copy(out=tl["G"][:, tt, :], in_=ps2[:, 0:4]), reads=[bps2], writes=[b])
                A(lambda e, ps2=ps2, tt=tt: e.activation(out=tl["eG"][:, tt, :], in_=ps2[:, 0:4], func=AF.Exp),
                  reads=[bps2], writes=[b])
                A(lambda e, ps2=ps2, tt=tt: e.activation(out=tl["gl"][:, tt, :], in_=ps2[:, 8:12], func=AF.Exp),
                  reads=[bps2], writes=[b])
                V(lambda e, ps2=ps2, tt=tt: e.tensor_tensor(out=tl["eGlm"][:, tt, :], in0=ps2[:, 8:12], in1=tl["G"][:, tt, :],
                                                           op=ALU.subtract), reads=[bps2, b], writes=[b])
                A(lambda e, tt=tt: e.activation(out=tl["eGlm"][:, tt, :], in_=tl["eGlm"][:, tt, :], func=AF.Exp),
                  reads=[b], writes=[b])
                V(lambda e, tt=tt: e.tensor_tensor(out=tl["bG"][:, tt, :], in0=tl["beta"][:, tt, :], in1=tl["eG"][:, tt, :],
                                                   op=ALU.mult), reads=[b], writes=[b])
            tl["buf"] = b
            return tl

        def gdn_head(sq_i, h, tl, dg, b_dg):
            bsc = tl["buf"]
            with contextlib.ExitStack() as st:
                wq, bwq = load_w(st, "gwq", w_ab[:, A_Q + h * 128:A_Q + (h + 1) * 128], 128, ceng="dve")
                wk, bwk = load_w(st, "gwk", w_ab[:, A_K + h * 128:A_K + (h + 1) * 128], 128, eng="pool", ceng="act")
                wv, bwv = load_w(st, "gwv", w_ab[:, A_V + h * 128:A_V + (h + 1) * 128], 128, ceng="pool")
                wz, bwz = load_w(st, "gwz", w_ab[:, A_Z + h * 128:A_Z + (h + 1) * 128], 128, eng="pool", ceng="dve")
                uT = SB(st, "uT", [128, S + 4], BF16)
                b_u = Buf()
                outT = {n: SB(st, "g_%sT" % n, [128, S], BF16) for n in ("q", "k", "v")}
                b_o = {n: [Buf() for _ in range(NBQ)] for n in ("q", "k", "v")}
                cs_l = [SB(st, "g_cs", [128, 512], F32) for _ in range(2)]
                sqb_l = [SB(st, "g_sqb", [128, 512], BF16) for _ in range(2)]
                rs_l = [SB(st, "g_rs", [128, 512], F32) for _ in range(2)]
                b_cs_l = [Buf() for _ in range(2)]
                P(lambda e: e.memset(uT[:, 0:4], 0.0), writes=[b_u])
                for ci, (n, wb, bw) in enumerate((("q", wq, bwq), ("k", wk, bwk), ("v", wv, bwv))):
                    chunk = ci * 4 + h
                    for blk in range(NBQ):
                        ps, bps = nextA()
                        proj_fm(wb, bw, 0, blk, ps, bps)
                        V(lambda e, ps=ps, blk=blk: e.tensor_copy(out=uT[:, 4 + blk * 512:4 + (blk + 1) * 512], in_=ps[:, :]),
                          reads=[bps], writes=[b_u])
                    acq_, relA_, relT_ = make_pool()

                    def conv_gen(blk, n=n, chunk=chunk):
                        cs, sqb, rs, b_cs = cs_l[blk % 2], sqb_l[blk % 2], rs_l[blk % 2], b_cs_l[blk % 2]
                        ((ps, bps),) = yield from acq_(1, 0)
                        for j in range(4):
                            T(lambda e, j=j: e.matmul(
                                ps[:, :], lhsT=dg[:, chunk * 4 + j, :], rhs=uT[:, 1 + j + blk * 512:1 + j + (blk + 1) * 512],
                                start=(j == 0), stop=(j == 3)), reads=[b_u, b_dg], writes=[bps])
                        yield
                        if n == "v":
                            A(lambda e: e.activation(out=outT["v"][:, blk * 512:(blk + 1) * 512], in_=ps[:, :],
                                                     func=AF.Silu), reads=[bps], writes=[b_o["v"][blk]])
                            relA_((ps, bps))
                            yield
                            return
                        A(lambda e: e.activation(out=cs[:], in_=ps[:, :], func=AF.Silu), reads=[bps], writes=[b_cs])
                        relA_((ps, bps))
                        yield
                        V(lambda e: e.tensor_tensor(out=sqb[:], in0=cs[:], in1=cs[:], op=ALU.mult), reads=[b_cs], writes=[b_cs])
                        yield
                        ((ps2, bps2),) = yield from acq_(1, 0)
                        T(lambda e: e.matmul(ps2[:, :], lhsT=ones_b[:], rhs=sqb[:], start=True, stop=True),
                          reads=[b_cs, B_const], writes=[bps2])
                        yield
                        V(lambda e: e.tensor_scalar(out=rs[:], in0=ps2[:, :], scalar1=EPS, scalar2=None, op0=ALU.add),
                          reads=[bps2], writes=[b_cs])
                        relA_((ps2, bps2))
                        yield
                        A(lambda e: e.activation(out=rs[:], in_=rs[:], func=AF.Sqrt), reads=[b_cs], writes=[b_cs])
                        yield
                        sc_ = QSCALE if n == "q" else 1.0
                        V(lambda e: e.reciprocal(out=rs[:], in_=rs[:]), reads=[b_cs], writes=[b_cs])
                        V(lambda e: e.scalar_tensor_tensor(
                            out=outT[n][:, blk * 512:(blk + 1) * 512], in0=cs[:], scalar=sc_, in1=rs[:],
                            op0=ALU.mult, op1=ALU.mult), reads=[b_cs], writes=[b_o[n][blk]])
                        yield

                    run_staggered([(lambda blk=blk: conv_gen(blk)) for blk in range(NBQ)], stagger=3, max_live=2)
                class TB:
                    def __init__(self, name, shape, dtp):
                        self.t = SB(st, "g_" + name, shape, dtp)
                        self.b = Buf()
                G = 4
                Sst = TB("S", [128, 128], F32)
                V(lambda e: e.memset(Sst.t[:], 0.0), writes=[Sst.b])
                f128 = [128, 128]
                I_ = [dict(gb=TB("gb", f128, F32), dec=TB("dec", f128, F32), decT=TB("decT", f128, F32),
                           P=[TB("Pa", f128, F32), TB("Pb", f128, F32)], PT=[TB("PTa", f128, F32), TB("PTb", f128, F32)],
                           XT=[TB("XTa", f128, F32), TB("XTb", f128, F32)]) for _ in range(G)]
                O_ = [dict(rr=TB("rr", [128, 256], F32), qkT=TB("qkT", f128, F32), kdec=TB("kdec", f128, F32),
                           wT=TB("wT", f128, F32), qnf=TB("qnf", f128, F32)) for _ in range(2 * G)]
                Q_ = [dict(vnew=TB("vnew", f128, F32), qS=TB("qS", f128, F32), o=TB("o", f128, F32), osq=TB("osq", f128, F32),
                           oss=TB("oss", [128, 1], F32), on=TB("on", f128, F32), sz=TB("sz", f128, F32),
                           yb=TB("yb", f128, BF16)) for _ in range(2)]
                ybT = [TB("ybT", [128, 512], BF16) for _ in range(2)]
                kT_, qT_, vT_ = outT["k"], outT["q"], outT["v"]

                freeA = list(zip(PSM["A"], PSM["BA"]))
                freeT = list(zip(PSM["T"], PSM["BT"]))

                def acquire(nA=0, nT=0):
                    while len(freeA) < nA or len(freeT) < nT:
                        yield
                    ra = [freeA.pop(0) for _ in range(nA)]
                    rt = [freeT.pop(0) for _ in range(nT)]
                    return ra + rt

                def relA(*bk):
                    freeA.extend(bk)

                def relT(*bk):
                    freeT.extend(bk)

                def part1(tt, si, oi):
                    I, O = I_[si], O_[oi]
                    blk = tt // 4
                    sl = slice(tt * 128, (tt + 1) * 128)
                    cl = lambda nm: tl[nm][:, tt, h:h + 1]
                    rdq, rdk, rdv = [b_o["q"][blk]], [b_o["k"][blk]], [b_o["v"][blk]]
                    gb, dec, decT = I["gb"], I["dec"], I["decT"]
                    rr_, qkT, kdec, wT, qnf = O["rr"], O["qkT"], O["kdec"], O["wT"], O["qnf"]
                    V(lambda e: e.tensor_scalar(out=gb.t[:], in0=ones_f[:], scalar1=cl("g"), scalar2=None, op0=ALU.mult),
                      reads=[bsc, B_const], writes=[gb.b])
                    (psG, bpsG), (pst, bpst) = yield from acquire(1, 1)
                    T(lambda e: e.matmul(psG[:, 0:128], lhsT=gb.t[:], rhs=U_f[:], start=True, stop=True),
                      reads=[gb.b, B_const], writes=[bpsG])
                    T(lambda e: e.transpose(out=pst[:, 0:128], in_=kT_[:, sl], identity=ident_b[:]),
                      reads=rdk + [B_const], writes=[bpst])
                    T(lambda e: e.transpose(out=pst[:, 128:256], in_=vT_[:, sl], identity=ident_b[:]),
                      reads=rdv + [B_const], writes=[bpst])
                    yield
                    V(lambda e: e.scalar_tensor_tensor(out=dec.t[:], in0=psG[:, 0:128], scalar=cl("G"), in1=maskA[:],
                                                       op0=ALU.subtract, op1=ALU.add), reads=[bpsG, bsc, B_const], writes=[dec.b])
                    V(lambda e: e.scalar_tensor_tensor(out=decT.t[:], in0=psG[:, 0:128], scalar=cl("G"), in1=maskQ[:],
                                                       op0=ALU.subtract, op1=ALU.subtract), reads=[bpsG, bsc, B_const], writes=[decT.b])
                    V(lambda e: e.tensor_scalar(out=rr_.t[:, 0:128], in0=pst[:, 128:256], scalar1=cl("beta"), scalar2=None,
                                                op0=ALU.mult), reads=[bpst, bsc], writes=[rr_.b])
                    V(lambda e: e.tensor_scalar(out=rr_.t[:, 128:256], in0=pst[:, 0:128], scalar1=cl("bG"), scalar2=None,
                                                op0=ALU.mult), reads=[bpst, bsc], writes=[rr_.b])
                    V(lambda e: e.tensor_scalar(out=kdec.t[:], in0=pst[:, 0:128], scalar1=cl("eGlm"), scalar2=None,
                                                op0=ALU.mult), reads=[bpst, bsc], writes=[kdec.b])
                    P(lambda e: e.tensor_copy(out=qnf.t[:], in_=qT_[:, sl]), reads=rdq, writes=[qnf.b])
                    relA((psG, bpsG))
                    relT((pst, bpst))
                    yield
                    ((psK, bpsK),) = yield from acquire(1, 0)
                    T(lambda e: e.matmul(psK[:, 0:128], lhsT=kT_[:, sl], rhs=kT_[:, sl], start=True, stop=True),
                      reads=rdk, writes=[bpsK])
                    T(lambda e: e.matmul(psK[:, 128:256], lhsT=kT_[:, sl], rhs=qT_[:, sl], start=True, stop=True),
                      reads=rdk + rdq, writes=[bpsK])
                    A(lambda e: e.activation(out=dec.t[:], in_=dec.t[:], func=AF.Exp, scale=-1.0), reads=[dec.b], writes=[dec.b])
                    A(lambda e: e.activation(out=decT.t[:], in_=decT.t[:], func=AF.Exp), reads=[decT.b], writes=[decT.b])
                    yield
                    Pc, PTc, XTc = I["P"][0], I["PT"][0], I["XT"][0]
                    V(lambda e: e.scalar_tensor_tensor(out=Pc.t[:], in0=psK[:, 0:128], scalar=cl("beta"), in1=dec.t[:],
                                                       op0=ALU.mult, op1=ALU.mult), reads=[bpsK, bsc, dec.b], writes=[Pc.b])
                    V(lambda e: e.tensor_tensor(out=qkT.t[:], in0=psK[:, 128:256], in1=decT.t[:], op=ALU.mult),
                      reads=[bpsK, decT.b], writes=[qkT.b])
                    relA((psK, bpsK))
                    yield
                    ((psX, bpsX),) = yield from acquire(1, 0)
                    T(lambda e: e.matmul(psX[:, 0:128], lhsT=Pc.t[:], rhs=ident_f[:], start=True, stop=True),
                      reads=[Pc.b, B_const], writes=[bpsX])
                    yield
                    V(lambda e: e.tensor_copy(out=PTc.t[:], in_=psX[:, 0:128]), reads=[bpsX], writes=[PTc.b])
                    V(lambda e: e.scalar_tensor_tensor(out=XTc.t[:], in0=psX[:, 0:128], scalar=-1.0, in1=ident_f[:],
                                                       op0=ALU.mult, op1=ALU.add), reads=[bpsX, B_const], writes=[XTc.b])
                    relA((psX, bpsX))
                    yield
                    pend = None
                    for l in range(1, 8):
                        Pp, PTp = I["P"][(l - 1) % 2], I["PT"][(l - 1) % 2]
                        Pn, PTn = I["P"][l % 2], I["PT"][l % 2]
                        got = yield from acquire((1 if l <= 6 else 0) + (1 if l >= 2 else 0), 0)
                        if l <= 6:
                            psS, bpsS = got.pop(0)
                            T(lambda e, psS=psS, Pp=Pp, PTp=PTp: e.matmul(psS[:, 0:128], lhsT=PTp.t[:], rhs=Pp.t[:], start=True, stop=True),
                              reads=[Pp.b, PTp.b], writes=[bpsS])
                            if l < 6:
                                T(lambda e, psS=psS, Pp=Pp, PTp=PTp: e.matmul(psS[:, 128:256], lhsT=Pp.t[:], rhs=PTp.t[:], start=True, stop=True),
                                  reads=[Pp.b, PTp.b], writes=[bpsS])
                        if l >= 2:
                            XTo, XTn = I["XT"][(l - 2) % 2], I["XT"][(l - 1) % 2]
                            psM, bpsM = got.pop(0)
                            T(lambda e, psM=psM, Pp=Pp, XTo=XTo: e.matmul(psM[:, 0:128], lhsT=Pp.t[:], rhs=XTo.t[:], start=True, stop=True),
                              reads=[Pp.b, XTo.b], writes=[bpsM])
                            pend = (psM, bpsM, XTo, XTn)
                        yield
                        if l <= 6:
                            A(lambda e, psS=psS, Pn=Pn: e.copy(out=Pn.t[:], in_=psS[:, 0:128]), reads=[bpsS], writes=[Pn.b])
                            if l < 6:
                                V(lambda e, psS=psS, PTn=PTn: e.tensor_copy(out=PTn.t[:], in_=psS[:, 128:256]), reads=[bpsS], writes=[PTn.b])
                            relA((psS, bpsS))
                        if pend is not None:
                            psM, bpsM, XTo, XTn = pend
                            V(lambda e, psM=psM, XTo=XTo, XTn=XTn: e.tensor_tensor(out=XTn.t[:], in0=XTo.t[:], in1=psM[:, 0:128], op=ALU.add),
                              reads=[bpsM, XTo.b], writes=[XTn.b])
                            relA((psM, bpsM))
                            pend = None
                        yield
                    XTf = I["XT"][0]
                    ((psL, bpsL),) = yield from acquire(1, 0)
                    T(lambda e: e.matmul(psL[:, 0:256], lhsT=XTf.t[:], rhs=rr_.t[:], start=True, stop=True),
                      reads=[XTf.b, rr_.b], writes=[bpsL])
                    yield
                    V(lambda e: e.tensor_copy(out=rr_.t[:, 0:128], in_=psL[:, 0:128]), reads=[bpsL], writes=[rr_.b])
                    A(lambda e: e.copy(out=rr_.t[:, 128:256], in_=psL[:, 128:256]), reads=[bpsL], writes=[rr_.b])
                    relA((psL, bpsL))
                    yield
                    ((psW, bpsW),) = yield from acquire(1, 0)
                    T(lambda e: e.matmul(psW[:, 0:128], lhsT=rr_.t[:, 128:256], rhs=ident_f[:], start=True, stop=True),
                      reads=[rr_.b, B_const], writes=[bpsW])
                    yield
                    A(lambda e: e.copy(out=wT.t[:], in_=psW[:, 0:128]), reads=[bpsW], writes=[wT.b])
                    relA((psW, bpsW))
                    yield

                posts = []

                def post(tt, qi):
                    Q = Q_[qi]
                    blk = tt // 4
                    o, osq, oss, on_, sz, yb = Q["o"], Q["osq"], Q["oss"], Q["on"], Q["sz"], Q["yb"]
                    ((psZ, bpsZ),) = yield from acquire(1, 0)
                    proj_tm(wz, bwz, 0, 128, tt, psZ, bpsZ)
                    yield
                    A(lambda e: e.activation(out=osq.t[:], in_=o.t[:], func=AF.Square, accum_out=oss.t[:]),
                      reads=[o.b], writes=[osq.b, oss.b])
                    A(lambda e: e.activation(out=sz.t[:], in_=psZ[:, 0:128], func=AF.Silu), reads=[bpsZ], writes=[sz.b])
                    relA((psZ, bpsZ))
                    yield
                    V(lambda e: e.tensor_scalar(out=oss.t[:], in0=oss.t[:], scalar1=1.0 / HD, scalar2=EPS, op0=ALU.mult, op1=ALU.add),
                      reads=[oss.b], writes=[oss.b])
                    yield
                    A(lambda e: e.activation(out=oss.t[:], in_=oss.t[:], func=AF.Sqrt), reads=[oss.b], writes=[oss.b])
                    yield
                    V(lambda e: e.reciprocal(out=oss.t[:], in_=oss.t[:]), reads=[oss.b], writes=[oss.b])
                    V(lambda e: e.scalar_tensor_tensor(out=on_.t[:], in0=o.t[:], scalar=oss.t[:, 0:1], in1=smalls[:, 16:144],
                                                       op0=ALU.mult, op1=ALU.mult), reads=[o.b, oss.b, B_const], writes=[on_.b])
                    V(lambda e: e.tensor_tensor(out=yb.t[:], in0=on_.t[:], in1=sz.t[:], op=ALU.mult),
                      reads=[on_.b, sz.b], writes=[yb.b])
                    yield
                    ((pst, bpst),) = yield from acquire(0, 1)
                    T(lambda e: e.transpose(out=pst[:, 0:128], in_=yb.t[:], identity=ident_b[:]), reads=[yb.b, B_const], writes=[bpst])
                    yield
                    ybt = ybT[blk % 2]
                    s_ = tt % 4
                    A(lambda e: e.copy(out=ybt.t[:, s_ * 128:(s_ + 1) * 128], in_=pst[:, 0:128]), reads=[bpst], writes=[ybt.b])
                    relT((pst, bpst))
                    if s_ == 3:
                        ks.dma("pool", lambda e: e.dma_start(
                            out=yT[sq_i, h * 128:(h + 1) * 128, blk * 512:(blk + 1) * 512], in_=ybt.t[:]),
                               reads=[ybt.b], writes=[B_yT])
                    yield

                def chain(tiles, obase):
                    for k_, tt in enumerate(tiles):
                        O = O_[obase + k_]
                        Q = Q_[tt % 2]
                        cl = lambda nm, tt=tt: tl[nm][:, tt, h:h + 1]
                        ((psR, bpsR),) = yield from acquire(1, 0)
                        T(lambda e, psR=psR, O=O: e.matmul(psR[:, 0:128], lhsT=O["wT"].t[:], rhs=Sst.t[:], start=True, stop=True),
                          reads=[O["wT"].b, Sst.b], writes=[bpsR])
                        T(lambda e, psR=psR, O=O: e.matmul(psR[:, 128:256], lhsT=O["qnf"].t[:], rhs=Sst.t[:], start=True, stop=True),
                          reads=[O["qnf"].b, Sst.b], writes=[bpsR])
                        yield
                        V(lambda e, psR=psR, O=O, Q=Q: e.tensor_tensor(out=Q["vnew"].t[:], in0=O["rr"].t[:, 0:128], in1=psR[:, 0:128],
                                                                      op=ALU.subtract), reads=[bpsR, O["rr"].b], writes=[Q["vnew"].b])
                        A(lambda e, psR=psR, Q=Q, cl=cl: e.activation(out=Q["qS"].t[:], in_=psR[:, 128:256], func=AF.Copy, scale=cl("eG")),
                          reads=[bpsR, bsc], writes=[Q["qS"].b])
                        relA((psR, bpsR))
                        yield
                        ((psO, bpsO),) = yield from acquire(1, 0)
                        T(lambda e, psO=psO, O=O, Q=Q: e.matmul(psO[:, 0:128], lhsT=O["qkT"].t[:], rhs=Q["vnew"].t[:], start=True, stop=True),
                          reads=[O["qkT"].b, Q["vnew"].b], writes=[bpsO])
                        T(lambda e, psO=psO, O=O, Q=Q: e.matmul(psO[:, 128:256], lhsT=O["kdec"].t[:], rhs=Q["vnew"].t[:], start=True, stop=True),
                          reads=[O["kdec"].b, Q["vnew"].b], writes=[bpsO])
                        yield
                        V(lambda e, psO=psO, cl=cl: e.scalar_tensor_tensor(out=Sst.t[:], in0=Sst.t[:], scalar=cl("gl"), in1=psO[:, 128:256],
                                                                          op0=ALU.mult, op1=ALU.add), reads=[bpsO, bsc, Sst.b], writes=[Sst.b])
                        V(lambda e, psO=psO, Q=Q: e.tensor_tensor(out=Q["o"].t[:], in0=Q["qS"].t[:], in1=psO[:, 0:128], op=ALU.add),
                          reads=[bpsO, Q["qS"].b], writes=[Q["o"].b])
                        relA((psO, bpsO))
                        posts.append(post(tt, tt % 2))
                        yield

                _stop = 0

                def _lim(g_):
                    n_ = 0
                    for _ in g_:
                        n_ += 1
                        if _stop and n_ >= _stop:
                            return
                        yield

                def run_round(gens):
                    gens = [_lim(g_) for g_ in gens] if _stop else list(gens)
                    while gens or posts:
                        for g_ in list(gens):
                            try:
                                next(g_)
                            except StopIteration:
                                gens.remove(g_)
                        for g_ in list(posts):
                            try:
                                next(g_)
                            except StopIteration:
                                posts.remove(g_)

                NG = NT // G
                for g in range(NG + 1):
                    gens = []
                    if g < NG:
                        gens += [part1(g * G + k_, k_, (g % 2) * G + k_) for k_ in range(G)]
                    if g >= 1 and not _stop:
                        gens.append(chain([(g - 1) * G + k_ for k_ in range(G)], ((g - 1) % 2) * G))
                    run_round(gens)
            ks.fence()

        def gdn_all(sq_i):
            with contextlib.ExitStack() as st:
                set_psum(st, 6, 2)
                tl = gdn_prep(sq_i, st)
                for h in range(4):
                    gdn_head(sq_i, h, tl, dg, b_dg)

        def make_pool():
            freeA = list(zip(PSM["A"], PSM["BA"]))
            freeT = list(zip(PSM["T"], PSM["BT"]))

            def acquire(nA=0, nT=0):
                while len(freeA) < nA or len(freeT) < nT:
                    yield
                ra = [freeA.pop(0) for _ in range(nA)]
                rt = [freeT.pop(0) for _ in range(nT)]
                return ra + rt

            def relA(*bk):
                freeA.extend(bk)

            def relT(*bk):
                freeT.extend(bk)
            return acquire, relA, relT

        def run_staggered(gen_fns, stagger, max_live):
            pending = list(gen_fns)
            live = []
            rnd = 0
            while pending or live:
                if pending and len(live) < max_live and rnd % stagger == 0:
                    live.append(pending.pop(0)())
                for g_ in list(live):
                    try:
                        next(g_)
                    except StopIteration:
                        live.remove(g_)
                rnd += 1

        def rms_gen(xt, bx, grep, bg, junk, bjunk, ss, rstd, hn, bsc, acquire, relT, dst_fn):
            A(lambda e: e.activation(out=junk[:], in_=xt[:], func=AF.Square, accum_out=ss[:]),
              reads=[bx], writes=[bjunk, bsc])
            yield
            V(lambda e: e.tensor_scalar(out=rstd[:], in0=ss[:], scalar1=1.0 / D, scalar2=EPS, op0=ALU.mult, op1=ALU.add),
              reads=[bsc], writes=[bsc])
            yield
            A(lambda e: e.activation(out=rstd[:], in_=rstd[:], func=AF.Sqrt), reads=[bsc], writes=[bsc])
            yield
            V(lambda e: e.reciprocal(out=rstd[:], in_=rstd[:]), reads=[bsc], writes=[bsc])
            V(lambda e: e.scalar_tensor_tensor(out=hn[:], in0=xt[:], scalar=rstd[:, 0:1], in1=grep[:],
                                               op0=ALU.mult, op1=ALU.mult), reads=[bx, bg, bsc], writes=[bsc])
            yield
            ((pst, bpst),) = yield from acquire(0, 1)
            for c in range(8):
                T(lambda e, c=c: e.transpose(out=pst[:, c * 128:(c + 1) * 128], in_=hn[:, c * 128:(c + 1) * 128],
                                             identity=ident_b[:]), reads=[bsc, B_const], writes=[bpst])
            yield
            dst_fn(pst, bpst)
            relT((pst, bpst))
            yield

        def phase_C(sq_i, layer):
            last = (layer == 1)
            xsrc = x_in if layer == 0 else xres
            with contextlib.ExitStack() as st:
                set_psum(st, 6, 2)
                acquire, relA, relT = make_pool()
                wo = SB(st, "c_wo", [128, 8, D], BF16)
                wg = SB(st, "c_wg", [128, 8, D], BF16)
                wp = SB(st, "c_wp", [128, 2, D], BF16)
                gP = SB(st, "c_gP", [128, D], F32)
                gN = SB(st, "c_gN", [128, D], F32)
                bwo, bwg, bwp = Buf(), Buf(), Buf()
                bgP, bgN = Buf(), Buf()
                with contextlib.ExitStack() as st2:
                    stgs = [SB(st2, "c_stg", [128, 8, 512], F32) for _ in range(3)]
                    b_stgs = [Buf(), Buf(), Buf()]
                    k_ = 0
                    for (dst, bdst, src) in ((wo, bwo, w_out[layer]), (wg, bwg, w_gate[layer])):
                        for hh in range(2):
                            stg, b_stg = stgs[k_ % 3], b_stgs[k_ % 3]
                            k_ += 1
                            ks.dma("sp", lambda e, src=src, hh=hh, stg=stg: e.dma_start(
                                out=stg[:], in_=src[:, hh * 512:(hh + 1) * 512].rearrange("(c p) n -> p c n", p=128)),
                                   writes=[b_stg])
                            cast_on(("dve", "act", "pool")[k_ % 3], dst[:, :, hh * 512:(hh + 1) * 512], stg[:], [b_stg], [bdst])
                    for hh in range(2):
                        stg, b_stg = stgs[k_ % 3], b_stgs[k_ % 3]
                        k_ += 1
                        ks.dma("sp", lambda e, hh=hh, stg=stg: e.dma_start(
                            out=stg[:, 0:2, :], in_=w_pp[layer, :, hh * 512:(hh + 1) * 512].rearrange("(c p) n -> p c n", p=128)),
                               writes=[b_stg])
                        P(lambda e, hh=hh, stg=stg: e.tensor_copy(out=wp[:, :, hh * 512:(hh + 1) * 512], in_=stg[:, 0:2, :]),
                          reads=[b_stg], writes=[bwp])
                    ks.dma("sp", lambda e: e.dma_start(out=gP[:], in_=gvec[2 + layer, :, :]), writes=[bgP])
                    ks.dma("sp", lambda e: e.dma_start(out=gN[:], in_=gvec[4 if last else 1, :, :]), writes=[bgN])
                ks.fence()
                NP_ = 3
                mk = lambda nm, shp, dtp: [SB(st, nm, shp, dtp) for _ in range(NP_)]
                xt = mk("c_x", [128, D], F32)
                yl = mk("c_yl", [128, 8, 128], BF16)
                ptf = mk("c_ptf", [128, 2, 128], F32)
                ptb = mk("c_ptb", [128, 2, 128], BF16)
                x1 = mk("c_x1", [128, D], F32)
                hT = mk("c_hT", [128, 8, 128], BF16)
                gt = mk("c_gt", [128, D], F32)
                hn = mk("c_hn", [128, D], BF16)
                ss = mk("c_ss", [128, 1], F32)
                rstd = mk("c_rs", [128, 1], F32)
                ss2 = mk("c_ss2", [128, 1], F32)
                rstd2 = mk("c_rs2", [128, 1], F32)
                mb = lambda: [Buf() for _ in range(NP_)]
                bx, byl, bpt, bx1, bhT, bgt, bsc, bsc2, bhn2 = mb(), mb(), mb(), mb(), mb(), mb(), mb(), mb(), mb()

                def tile_gen(tt):
                    i = tt % NP_
                    tsl = slice(tt * 128, (tt + 1) * 128)
                    ks.dma("sp", lambda e: e.dma_start(out=xt[i][:], in_=xsrc[sq_i, tsl, :]), reads=[B_xres], writes=[bx[i]])
                    ks.dma("sp", lambda e: e.dma_start(out=yl[i][:], in_=yT[sq_i, :, tsl].rearrange("(c p) n -> p c n", p=128)),
                           reads=[B_yT], writes=[byl[i]])
                    ks.dma("sp", lambda e: e.dma_start(out=ptf[i][:], in_=pT_in[layer, sq_i, :, tsl].rearrange("(c p) n -> p c n", p=128)),
                           writes=[bpt[i]])
                    P(lambda e: e.tensor_copy(out=ptb[i][:], in_=ptf[i][:]), reads=[bpt[i]], writes=[bpt[i]])
                    yield
                    bk = yield from acquire(2, 0)
                    for hh in range(2):
                        ps, bps = bk[hh]
                        for c in range(8):
                            T(lambda e, ps=ps, c=c, hh=hh: e.matmul(ps[:, :], lhsT=yl[i][:, c, :], rhs=wo[:, c, hh * 512:(hh + 1) * 512],
                                                                    start=(c == 0), stop=(c == 7)), reads=[byl[i], bwo], writes=[bps])
                    yield
                    for hh in range(2):
                        ps, bps = bk[hh]
                        V(lambda e, ps=ps, hh=hh: e.tensor_tensor(out=x1[i][:, hh * 512:(hh + 1) * 512], in0=ps[:, :],
                                                                  in1=xt[i][:, hh * 512:(hh + 1) * 512], op=ALU.add),
                          reads=[bps, bx[i]], writes=[bx1[i]])
                    relA(*bk)
                    yield

                    def dstg(pst, bpst):
                        A(lambda e: e.copy(out=hT[i][:], in_=pst[:, :].rearrange("p (c n) -> p c n", c=8)),
                          reads=[bpst], writes=[bhT[i]])
                    yield from rms_gen(x1[i], bx1[i], gP, bgP, gt[i], bgt[i], ss[i], rstd[i], hn[i], bsc[i], acquire, relT, dstg)
                    bk = yield from acquire(4, 0)
                    for hh in range(2):
                        ps, bps = bk[hh]
                        for c in range(8):
                            T(lambda e, ps=ps, c=c, hh=hh: e.matmul(ps[:, :], lhsT=hT[i][:, c, :], rhs=wg[:, c, hh * 512:(hh + 1) * 512],
                                                                    start=(c == 0), stop=(c == 7)), reads=[bhT[i], bwg], writes=[bps])
                        ps, bps = bk[2 + hh]
                        for c in range(2):
                            T(lambda e, ps=ps, c=c, hh=hh: e.matmul(ps[:, :], lhsT=ptb[i][:, c, :], rhs=wp[:, c, hh * 512:(hh + 1) * 512],
                                                                    start=(c == 0), stop=(c == 1)), reads=[bpt[i], bwp], writes=[bps])
                    yield
                    for hh in range(2):
                        ps, bps = bk[hh]
                        A(lambda e, ps=ps, hh=hh: e.activation(out=gt[i][:, hh * 512:(hh + 1) * 512], in_=ps[:, :], func=AF.Sigmoid),
                          reads=[bps], writes=[bgt[i]])
                    yield
                    for hh in range(2):
                        ps, bps = bk[2 + hh]
                        V(lambda e, ps=ps, hh=hh: e.tensor_tensor(out=gt[i][:, hh * 512:(hh + 1) * 512],
                                                                  in0=gt[i][:, hh * 512:(hh + 1) * 512], in1=ps[:, :], op=ALU.mult),
                          reads=[bps, bgt[i]], writes=[bgt[i]])
                    relA(*bk)
                    yield
                    P(lambda e: e.tensor_tensor(out=xt[i][:], in0=gt[i][:], in1=x1[i][:], op=ALU.add),
                      reads=[bgt[i], bx1[i]], writes=[bx[i]])
                    yield
                    if not last:
                        ks.dma("pool", lambda e: e.dma_start(out=xres[sq_i, tsl, :], in_=xt[i][:]), reads=[bx[i]], writes=[B_xres])

                        def dstn(pst, bpst):
                            A(lambda e: e.copy(out=hnT[:, :, tt * 128:(tt + 1) * 128],
                                               in_=pst[:, :].rearrange("p (c n) -> p c n", c=8)),
                              reads=[bpst], writes=[B_hnT[tt]])
                        yield from rms_gen(xt[i], bx[i], gN, bgN, gt[i], bgt[i], ss2[i], rstd2[i], hn[i], bsc2[i], acquire, relT, dstn)
                    else:
                        A(lambda e: e.activation(out=gt[i][:], in_=xt[i][:], func=AF.Square, accum_out=ss2[i][:]),
                          reads=[bx[i]], writes=[bgt[i], bsc2[i]])
                        yield
                        V(lambda e: e.tensor_scalar(out=rstd2[i][:], in0=ss2[i][:], scalar1=1.0 / D, scalar2=EPS,
                                                    op0=ALU.mult, op1=ALU.add), reads=[bsc2[i]], writes=[bsc2[i]])
                        yield
                        A(lambda e: e.activation(out=rstd2[i][:], in_=rstd2[i][:], func=AF.Sqrt), reads=[bsc2[i]], writes=[bsc2[i]])
                        yield
                        V(lambda e: e.reciprocal(out=rstd2[i][:], in_=rstd2[i][:]), reads=[bsc2[i]], writes=[bsc2[i]])
                        V(lambda e: e.scalar_tensor_tensor(out=x1[i][:], in0=xt[i][:], scalar=rstd2[i][:, 0:1],
                                                           in1=gN[:], op0=ALU.mult, op1=ALU.mult),
                          reads=[bx[i], bgN, bsc2[i]], writes=[bx1[i]])
                        yield
                        ks.dma("pool", lambda e: e.dma_start(out=out[sq_i, tsl, :], in_=x1[i][:]), reads=[bx1[i]], writes=[B_out])
                        yield

                run_staggered([(lambda tt=tt: tile_gen(tt)) for tt in range(NT)], stagger=5, max_live=NP_)
            ks.fence()

        B_out = Buf()

        def zero_yT(sq_i, r0, r1):
            with contextlib.ExitStack() as st:
                z = SB(st, "zz", [128, S], BF16)
                bz = Buf()
                P(lambda e: e.memset(z[:], 0.0), writes=[bz])
                for r in range(r0, r1, 128):
                    ks.dma("pool", lambda e, r=r: e.dma_start(out=yT[sq_i, r:r + 128, :], in_=z[:]), reads=[bz], writes=[B_yT])
            ks.fence()

        for sq_i in range(NSEQ):
            phase_A(sq_i)
            if do_gdn:
                gdn_all(sq_i)
                ks.fence()
            else:
                zero_yT(sq_i, 0, 512)
            if do_moba:
                for h in range(4):
                    attn_head(sq_i, "moba", h, w_ab,
                              dict(q=B_Q + h * 128, k=B_K + h * 128, v=B_V + h * 128, z=A_Z + 512 + h * 128), 512 + h * 128)
            else:
                zero_yT(sq_i, 512, 1024)
            phase_C(sq_i, 0)
            if do_fox:
                for h in range(8):
                    attn_head(sq_i, "fox", h, w_c,
                              dict(q=C_Q + h * 128, k=C_K + h * 128, v=C_V + h * 128, z=C_Z + h * 128, f=C_F + h), h * 128)
            else:
                zero_yT(sq_i, 0, 1024)
            phase_C(sq_i, 1)
        ks.emit()
    return nc


def prep_inputs(inp, nseq, ncores):
    f = lambda a: np.ascontiguousarray(np.asarray(a, dtype=np.float32))
    x = f(inp["x"])
    p = f(inp["p"])
    gv = np.stack([inp["norm_g"][0], inp["norm_g"][1], inp["ple_norm_g"][0], inp["ple_norm_g"][1], inp["final_g"]], 0)
    gv = f(np.broadcast_to(np.asarray(gv, np.float32)[:, None, :], (5, 128, D)))
    cw = np.asarray(inp["conv_w"], np.float32)[0]
    convw = f(cw.T.reshape(12, 128, 4).transpose(1, 0, 2))
    sv = np.concatenate([np.asarray(inp["a_log"], np.float32)[0], np.asarray(inp["dt_bias"], np.float32)[0],
                         np.asarray(inp["forget_b"], np.float32)[0], np.asarray(inp["gdn_norm_g"], np.float32)[0]])
    smallv = f(np.broadcast_to(sv[None, :], (128, 144)))
    shared = dict(w_in_ab=f(inp["w_in_ab"][0]), w_in_c=f(inp["w_in_c"][0]), w_out_ab=f(inp["w_out_ab"][0]),
                  w_out_c=f(inp["w_out_c"][0]), w_ple_gate=f(inp["w_ple_gate"]), w_ple_proj=f(inp["w_ple_proj"]),
                  gvec=gv, convw=convw, smallv=smallv,
                  wfc=f(np.asarray(inp["w_in_c"], np.float32)[0][:, C_F:C_F + 8].T.reshape(8, 8, 128).transpose(0, 2, 1)))
    maps = []
    for c in range(ncores):
        sl = slice(c * nseq, (c + 1) * nseq)
        m = dict(shared)
        m["x"] = f(x[sl])
        m["pT"] = f(p[:, sl].transpose(0, 1, 3, 2))
        maps.append(m)
    return maps


def kernel(**inputs):
    x = np.asarray(inputs["x"])
    Bsz, S, _ = x.shape
    ncores = 8
    nseq = Bsz // ncores
    nc = build(S, nseq)
    maps = prep_inputs(inputs, nseq, ncores)
    res = run_bass_kernel_spmd(nc, maps, core_ids=list(range(ncores)))
    return np.concatenate([np.asarray(r["out"], dtype=np.float32) for r in res.results], axis=0)
```
